# Optimizing a Trainium2 kernel written in Bass

```python
import math
import jax
import jax.numpy as jnp
from jax import lax
import numpy as np

D_MODEL = 1024
BATCH = 16
SEQ = 2048
DEPTH = 2

GRID_W = 64
CTX_LEN = 256
EPS = 1e-6

W_MIX = 512
N_BRANCH = 3
CHUNK = 128
SGU_HEADS = 4
CONV_WIDTH = 3
S5_GROUP_CH = 16
S5_GROUPS = W_MIX // S5_GROUP_CH
S5_STATE = 64
DT_MIN = 0.01
DT_MAX = 0.1
A_U0 = 0
A_V0 = A_U0 + W_MIX
B_B0 = A_V0 + W_MIX
B_C0 = B_B0 + W_MIX
B_H0 = B_C0 + W_MIX
S5_COL0 = B_H0 + W_MIX
GATE0 = S5_COL0 + W_MIX
D_IN = GATE0 + N_BRANCH * D_MODEL
N_EXPERTS = 16
N_EXPERT_GROUPS = 4
EXPERTS_PER_GROUP = N_EXPERTS // N_EXPERT_GROUPS
TOPK_GROUPS = 1
GROUP_SCORE_TOPK = 2
TOP_K = 2
D_FF_EXPERT = 512

kernel_name = "hybrid_sgu_conv_s5_moe_dit"


def _rmsnorm(x, g):
    xf = x.astype(jnp.float32)
    y = xf * lax.rsqrt(jnp.mean(xf * xf, axis=-1, keepdims=True) + EPS)
    return (y * g.astype(jnp.float32)).astype(x.dtype)


def _layernorm(x, g):
    xf = x.astype(jnp.float32)
    xc = xf - jnp.mean(xf, axis=-1, keepdims=True)
    y = xc * lax.rsqrt(jnp.mean(xc * xc, axis=-1, keepdims=True) + EPS)
    return (y * g.astype(jnp.float32)).astype(x.dtype)


def _adaln(cvec, w, b):
    m = jax.nn.silu(cvec) @ w + b
    return jnp.split(m[..., None, :], 6, axis=-1)


def _modulate(h, shift, scale):
    return h * (1.0 + scale) + shift


def _chunk_sgu(u, v, norm_g, w_s, b_s):
    u = jax.nn.gelu(u)
    v = _layernorm(jax.nn.gelu(v), norm_g)
    bsz, n, w = v.shape
    vc = v.reshape(bsz, n // CHUNK, CHUNK, SGU_HEADS, w // SGU_HEADS)
    mixed = jnp.einsum("hqk,bnkhc->bnqhc", w_s, vc) + b_s.T[:, :, None]
    return u * mixed.reshape(bsz, n, w)


def _conv3_axis(h, w, axis):
    n = h.shape[axis]
    pad = [(0, 0)] * h.ndim
    pad[axis] = (1, 1)
    hp = jnp.pad(h, pad)
    prev = lax.slice_in_dim(hp, 0, n, axis=axis)
    nxt = lax.slice_in_dim(hp, 2, n + 2, axis=axis)
    return w[0] * prev + w[1] * h + w[2] * nxt


def _short_conv(h, w, grid):
    if grid:
        bsz, n, ch = h.shape
        rows = n // GRID_W
        hg = h.reshape(bsz, rows, GRID_W, ch)
        return _conv3_axis(hg, w, 2).reshape(bsz, n, ch)
    return _conv3_axis(h, w, 1)


def _cmul(ar, ai, br, bi):
    return ar * br - ai * bi, ar * bi + ai * br


def _ssm_combine(e1, e2):
    a1r, a1i, b1r, b1i = e1
    a2r, a2i, b2r, b2i = e2
    ar, ai = _cmul(a2r, a2i, a1r, a1i)
    br, bi = _cmul(a2r, a2i, b1r, b1i)
    return ar, ai, br + b2r, bi + b2i


def _s5_discretise(lam_re, lam_im, log_dt, b_re, b_im):
    lam_re = lam_re.astype(jnp.float32)
    lam_im = lam_im.astype(jnp.float32)
    dt = jnp.exp(log_dt.astype(jnp.float32))[:, None]
    mag = jnp.exp(lam_re * dt)
    ab_re = mag * jnp.cos(lam_im * dt)
    ab_im = mag * jnp.sin(lam_im * dt)
    den = lam_re * lam_re + lam_im * lam_im
    nr = ab_re - 1.0
    f_re = (nr * lam_re + ab_im * lam_im) / den
    f_im = (ab_im * lam_re - nr * lam_im) / den
    bb_re, bb_im = _cmul(f_re[..., None], f_im[..., None], b_re.astype(jnp.float32), b_im.astype(jnp.float32))
    return ab_re, ab_im, bb_re, bb_im


def _s5_states(u, p, h0s):
    bsz, n, _ = u.shape
    ug = u.reshape(bsz, n, S5_GROUPS, S5_GROUP_CH).astype(jnp.float32)
    states = []
    for d in range(2):
        reverse = d == 1
        ab_re, ab_im, bb_re, bb_im = _s5_discretise(
            p["s5_lam_re"][d], p["s5_lam_im"][d], p["s5_log_dt"][d], p["s5_b_re"][d], p["s5_b_im"][d])
        bu_re = jnp.einsum("gph,blgh->blgp", bb_re, ug)
        bu_im = jnp.einsum("gph,blgh->blgp", bb_im, ug)
        if h0s is not None:
            i0 = n - 1 if reverse else 0
            c_re, c_im = _cmul(ab_re, ab_im, h0s[d][0], h0s[d][1])
            bu_re = bu_re.at[:, i0].add(c_re)
            bu_im = bu_im.at[:, i0].add(c_im)
        a_re = jnp.broadcast_to(ab_re, (1, n) + ab_re.shape)
        a_im = jnp.broadcast_to(ab_im, (1, n) + ab_im.shape)
        _, _, s_re, s_im = lax.associative_scan(
            _ssm_combine, (a_re, a_im, bu_re, bu_im), reverse=reverse, axis=1)
        states.append((s_re, s_im))
    return states


def _final_states(states):
    (f_re, f_im), (b_re, b_im) = states
    return ((f_re[:, -1], f_im[:, -1]), (b_re[:, 0], b_im[:, 0]))


def _s5_readout(u, states, p):
    bsz, n, w = u.shape
    ug = u.reshape(bsz, n, S5_GROUPS, S5_GROUP_CH).astype(jnp.float32)
    y = ug * p["s5_d"].astype(jnp.float32).reshape(S5_GROUPS, S5_GROUP_CH)
    for d in range(2):
        s_re, s_im = states[d]
        y = (y + jnp.einsum("ghp,blgp->blgh", p["s5_c_re"][d].astype(jnp.float32), s_re)
             - jnp.einsum("ghp,blgp->blgh", p["s5_c_im"][d].astype(jnp.float32), s_im))
    z = jax.nn.gelu(y.reshape(bsz, n, w))
    out = z * jax.nn.sigmoid(z @ p["glu_w"].astype(jnp.float32) + p["glu_b"].astype(jnp.float32))
    return out.astype(u.dtype)


def _token_mix(hn, p, grid, s5_h0):
    proj = hn @ p["w_in"]
    a_u, a_v, b_b, b_c, b_h, c_u, gates = jnp.split(
        proj, [A_V0, B_B0, B_C0, B_H0, S5_COL0, GATE0], axis=-1)
    y_a = _chunk_sgu(a_u, a_v, p["sgu_norm_g"], p["sgu_w"], p["sgu_b"])
    y_b = b_b * _short_conv(b_c * b_h, p["conv_w"], grid)
    states = _s5_states(c_u, p, s5_h0)
    y_c = _s5_readout(c_u, states, p)
    g = jax.nn.sigmoid(gates.astype(jnp.float32)).astype(hn.dtype)
    merged = None
    for i, y in enumerate((y_a, y_b, y_c)):
        term = g[..., i * D_MODEL:(i + 1) * D_MODEL] * (y @ p["w_branch"][i])
        merged = term if merged is None else merged + term
    return merged @ p["w_out"], states


def _moe(h, router_w, router_b, w_gate, w_up, w_down):
    bsz, n, d = h.shape
    t = h.reshape(bsz * n, d)
    scores = jax.nn.sigmoid((t @ router_w).astype(jnp.float32))
    biased = (scores + router_b.astype(jnp.float32)).reshape(-1, N_EXPERT_GROUPS, EXPERTS_PER_GROUP)
    group_score = jnp.sum(lax.top_k(biased, GROUP_SCORE_TOPK)[0], axis=-1)
    _, g_idx = lax.top_k(group_score, TOPK_GROUPS)
    g_mask = jnp.sum(jax.nn.one_hot(g_idx, N_EXPERT_GROUPS, dtype=jnp.float32), axis=1) > 0
    masked = jnp.where(g_mask[:, :, None], biased, -jnp.inf).reshape(-1, N_EXPERTS)
    _, e_idx = lax.top_k(masked, TOP_K)
    w_sel = jnp.take_along_axis(scores, e_idx, axis=1)
    w_sel = w_sel / jnp.sum(w_sel, axis=-1, keepdims=True)
    gates = jnp.sum(jax.nn.one_hot(e_idx, N_EXPERTS, dtype=jnp.float32) * w_sel[..., None], axis=1)
    gates = gates.astype(h.dtype)
    out = jnp.zeros_like(t)
    for e in range(N_EXPERTS):
        he = jax.nn.silu(t @ w_gate[e]) * (t @ w_up[e])
        out = out + gates[:, e:e + 1] * (he @ w_down[e])
    return out.reshape(bsz, n, d)


def setup_inputs(seed: int = 0) -> dict:
    key = jax.random.key(seed)
    ks = jax.random.split(key, 32)
    f32 = jnp.float32

    def nrm(k, shape, scale):
        return jax.random.normal(k, shape, f32) * scale

    G, P, H = S5_GROUPS, S5_STATE, S5_GROUP_CH
    lam_im_base = jnp.pi * jnp.arange(P, dtype=f32)
    return {
        "x": nrm(ks[0], (BATCH, SEQ, D_MODEL), 1.0),
        "c": nrm(ks[1], (BATCH, D_MODEL), 1.0),
        "ctx": nrm(ks[2], (BATCH, CTX_LEN, D_MODEL), 1.0),
        "c_ctx": nrm(ks[3], (D_MODEL,), 1.0),
        "w_mod": nrm(ks[4], (DEPTH, D_MODEL, 6 * D_MODEL), 0.5 * D_MODEL ** -0.5),
        "b_mod": nrm(ks[5], (DEPTH, 6 * D_MODEL), 0.02),
        "norm1_g": 1.0 + nrm(ks[6], (DEPTH, D_MODEL), 0.02),
        "norm2_g": 1.0 + nrm(ks[7], (DEPTH, D_MODEL), 0.02),
        "w_in": nrm(ks[8], (DEPTH, D_MODEL, D_IN), D_MODEL ** -0.5),
        "sgu_norm_g": 1.0 + nrm(ks[9], (DEPTH, W_MIX), 0.02),
        "sgu_w": nrm(ks[10], (DEPTH, SGU_HEADS, CHUNK, CHUNK), CHUNK ** -0.5),
        "sgu_b": nrm(ks[11], (DEPTH, SGU_HEADS, CHUNK), 0.02),
        "conv_w": nrm(ks[12], (DEPTH, CONV_WIDTH, W_MIX), CONV_WIDTH ** -0.5),
        "s5_lam_re": -0.5 + nrm(ks[13], (DEPTH, 2, G, P), 0.01),
        "s5_lam_im": lam_im_base + nrm(ks[14], (DEPTH, 2, G, P), 0.01),
        "s5_log_dt": jax.random.uniform(ks[15], (DEPTH, 2, G), f32, math.log(DT_MIN), math.log(DT_MAX)),
        "s5_b_re": nrm(ks[16], (DEPTH, 2, G, P, H), (2 * H) ** -0.5),
        "s5_b_im": nrm(ks[17], (DEPTH, 2, G, P, H), (2 * H) ** -0.5),
        "s5_c_re": nrm(ks[18], (DEPTH, 2, G, H, P), P ** -0.5),
        "s5_c_im": nrm(ks[19], (DEPTH, 2, G, H, P), P ** -0.5),
        "s5_d": nrm(ks[20], (DEPTH, W_MIX), 1.0),
        "glu_w": nrm(ks[21], (DEPTH, W_MIX, W_MIX), W_MIX ** -0.5),
        "glu_b": nrm(ks[22], (DEPTH, W_MIX), 0.02),
        "w_branch": nrm(ks[23], (DEPTH, N_BRANCH, W_MIX, D_MODEL), W_MIX ** -0.5),
        "w_out": nrm(ks[24], (DEPTH, D_MODEL, D_MODEL), D_MODEL ** -0.5),
        "router_w": nrm(ks[25], (D_MODEL, N_EXPERTS), D_MODEL ** -0.5),
        "router_b": nrm(ks[26], (N_EXPERTS,), 0.01),
        "exp_w_gate": nrm(ks[27], (DEPTH, N_EXPERTS, D_MODEL, D_FF_EXPERT), D_MODEL ** -0.5),
        "exp_w_up": nrm(ks[28], (DEPTH, N_EXPERTS, D_MODEL, D_FF_EXPERT), D_MODEL ** -0.5),
        "exp_w_down": nrm(ks[29], (DEPTH, N_EXPERTS, D_FF_EXPERT, D_MODEL), D_FF_EXPERT ** -0.5),
        "final_norm_g": 1.0 + nrm(ks[30], (D_MODEL,), 0.02),
    }


def reference(x, c, ctx, c_ctx, w_mod, b_mod, norm1_g, norm2_g, w_in, sgu_norm_g, sgu_w, sgu_b,
              conv_w, s5_lam_re, s5_lam_im, s5_log_dt, s5_b_re, s5_b_im, s5_c_re, s5_c_im, s5_d,
              glu_w, glu_b, w_branch, w_out, router_w, router_b, exp_w_gate, exp_w_up, exp_w_down,
              final_norm_g):
    h, hc = x, ctx
    for l in range(DEPTH):
        last = l == DEPTH - 1
        p = {
            "w_in": w_in[l], "sgu_norm_g": sgu_norm_g[l], "sgu_w": sgu_w[l], "sgu_b": sgu_b[l],
            "conv_w": conv_w[l], "s5_lam_re": s5_lam_re[l], "s5_lam_im": s5_lam_im[l],
            "s5_log_dt": s5_log_dt[l], "s5_b_re": s5_b_re[l], "s5_b_im": s5_b_im[l],
            "s5_c_re": s5_c_re[l], "s5_c_im": s5_c_im[l], "s5_d": s5_d[l],
            "glu_w": glu_w[l], "glu_b": glu_b[l], "w_branch": w_branch[l], "w_out": w_out[l],
        }
        sh1, sc1, g1, sh2, sc2, g2 = _adaln(c, w_mod[l], b_mod[l])
        csh1, csc1, cg1, csh2, csc2, cg2 = _adaln(c_ctx, w_mod[l], b_mod[l])
        cn = _modulate(_rmsnorm(hc, norm1_g[l]), csh1, csc1)
        if last:
            ctx_states = _s5_states(cn @ w_in[l][:, S5_COL0:S5_COL0 + W_MIX], p, None)
        else:
            ctx_mix, ctx_states = _token_mix(cn, p, False, None)
        h0 = _final_states(ctx_states)
        xn = _modulate(_rmsnorm(h, norm1_g[l]), sh1, sc1)
        lat_mix, _ = _token_mix(xn, p, True, h0)
        h = h + g1 * lat_mix
        h = h + g2 * _moe(_modulate(_rmsnorm(h, norm2_g[l]), sh2, sc2),
                          router_w, router_b, exp_w_gate[l], exp_w_up[l], exp_w_down[l])
        if not last:
            hc = hc + cg1 * ctx_mix
            hc = hc + cg2 * _moe(_modulate(_rmsnorm(hc, norm2_g[l]), csh2, csc2),
                                 router_w, router_b, exp_w_gate[l], exp_w_up[l], exp_w_down[l])
    return _rmsnorm(h, final_norm_g)
```

```python
import math
import numpy as np
from contextlib import ExitStack, contextmanager
import concourse.bass as bass
import concourse.mybir as mybir
from concourse.bass_utils import run_bass_kernel_spmd

F32 = mybir.dt.float32
BF16 = mybir.dt.bfloat16
I32 = mybir.dt.int32
AF = mybir.ActivationFunctionType
ALU = mybir.AluOpType
AX = mybir.AxisListType

D = 1024
LS = 2048
LC = 256
DEPTH = 2
DIN = 6144
WM = 512
NE = 16
DFF = 512
NCORES = 8
EPS = 1e-6
BIG = 1.0e4
TWO_PI = 2.0 * math.pi


class Eng:
    def __init__(self, nc, e, name):
        self.e = e
        self.sem = nc.alloc_semaphore(name)
        self.n = 0
        self.seen = {}

    def wait(self, evs):
        for sem, val in evs.items():
            if self.seen.get(sem, 0) >= val:
                continue
            self.e.wait_ge(sem, val)
            self.seen[sem] = val

    def sig(self, ins):
        self.n += 1
        ins.then_inc(self.sem, 1)
        return (self.sem, self.n)


class Buf:
    def __init__(self, ap=None):
        self.ap = ap
        self.w = {}
        self.r = {}
        self.dsem = None
        self.slot = None
        self.dv = 0


def _merge(d, ev):
    s, v = ev
    if d.get(s, 0) < v:
        d[s] = v


class Ring:
    def __init__(self, bufs):
        self.bufs = bufs
        self.i = 0

    def next(self):
        b = self.bufs[self.i]
        self.i = (self.i + 1) % len(self.bufs)
        return b


class Ker:
    def __init__(self, nc):
        self.nc = nc
        self.pe = Eng(nc, nc.tensor, "s_pe")
        self.act = Eng(nc, nc.scalar, "s_act")
        self.dve = Eng(nc, nc.vector, "s_dve")
        self.pool = Eng(nc, nc.gpsimd, "s_pool")
        self.sp = Eng(nc, nc.sync, "s_sp")
        self.engs = [self.pe, self.act, self.dve, self.pool, self.sp]
        self.dbufs = []
        self.uid = 0
        self.free_slots = []
        self.scope_bufs = [[]]

    def _pre(self, eng, reads, writes, after):
        evs = {}
        for b in reads:
            for s, v in b.w.items():
                _merge(evs, (s, v))
        for b in writes:
            for s, v in b.w.items():
                _merge(evs, (s, v))
            for s, v in b.r.items():
                _merge(evs, (s, v))
        for ev in after:
            _merge(evs, ev)
        eng.wait(evs)

    def _post(self, ev, reads, writes, pwrites):
        for b in reads:
            _merge(b.r, ev)
        for b in writes:
            b.w = {ev[0]: ev[1]}
            b.r = {}
        for b in pwrites:
            _merge(b.w, ev)

    def do(self, eng, emit, reads=(), writes=(), pwrites=(), after=()):
        self._pre(eng, reads, writes, after)
        ins = emit()
        ev = eng.sig(ins)
        self._post(ev, reads, writes, pwrites)
        return ev

    def dma(self, q, emits, owner, reads=(), writes=(), pwrites=(), after=()):
        self._pre(q, reads, writes, after)
        if owner.dsem is None:
            if self.free_slots:
                owner.slot = self.free_slots.pop()
            else:
                self.uid += 1
                owner.slot = [self.nc.alloc_semaphore(f"dq{self.uid}"), 0]
            owner.dsem = owner.slot[0]
            owner.dv = owner.slot[1]
            self.dbufs.append(owner)
            self.scope_bufs[-1].append(owner)
        for em in emits:
            ins = em()
            ins.then_inc(owner.dsem, 16)
            owner.dv += 16
        owner.slot[1] = owner.dv
        ev = (owner.dsem, owner.dv)
        self._post(ev, reads, writes, pwrites)
        return ev

    def barrier(self):
        evs = {}
        for e in self.engs:
            if e.n > 0:
                evs[e.sem] = e.n
        for b in self.dbufs:
            if b.dv > 0:
                evs[b.dsem] = b.dv
        for e in self.engs:
            e.wait(evs)

    def open_scope(self):
        self.scope_bufs.append([])

    def close_scope(self):
        for b in self.scope_bufs.pop():
            self.free_slots.append(b.slot)
            self.dbufs.remove(b)
            b.dsem = None
            b.slot = None


def build_program(debug=(), seqs=(0, 1, 2, 3), stop_after=None, nlayers=DEPTH):
    debug = set(debug)
    nc = bass.Bass("TRN2", target_bir_lowering=False)
    K = Ker(nc)
    es = ExitStack()

    def dram_in(name, shape):
        return nc.dram_tensor(name, list(shape), F32, kind="ExternalInput").ap()

    def dram_scr(name, shape, dt):
        if name in debug:
            return nc.dram_tensor(name, list(shape), dt, kind="ExternalOutput").ap()
        return nc.dram_tensor(name, list(shape), dt).ap()

    x_in = dram_in("x", [2, LS, D])
    ctx_in = dram_in("ctx", [2, LC, D])
    cvec = dram_in("cvec", [3, D])
    w_mod = dram_in("w_mod", [DEPTH, D, 6 * D])
    b_mod = dram_in("b_mod", [DEPTH, 6 * D])
    norm1_g = dram_in("norm1_g", [DEPTH, D])
    norm2_g = dram_in("norm2_g", [DEPTH, D])
    w_in = dram_in("w_in", [DEPTH, D, DIN])
    sgu_norm_g = dram_in("sgu_norm_g", [DEPTH, WM])
    sgu_w = dram_in("sgu_w", [DEPTH, 4, 128, 128])
    sgu_b = dram_in("sgu_b", [DEPTH, 4, 128])
    conv_w = dram_in("conv_w", [DEPTH, 3, WM])
    s5_lam_re = dram_in("s5_lam_re", [DEPTH, 2, 32, 64])
    s5_lam_im = dram_in("s5_lam_im", [DEPTH, 2, 32, 64])
    s5_log_dt = dram_in("s5_log_dt", [DEPTH, 2, 32])
    s5_b_re = dram_in("s5_b_re", [DEPTH, 2, 32, 64, 16])
    s5_b_im = dram_in("s5_b_im", [DEPTH, 2, 32, 64, 16])
    s5_c_re = dram_in("s5_c_re", [DEPTH, 2, 32, 16, 64])
    s5_c_im = dram_in("s5_c_im", [DEPTH, 2, 32, 16, 64])
    s5_d = dram_in("s5_d", [DEPTH, WM])
    glu_w = dram_in("glu_w", [DEPTH, WM, WM])
    glu_b = dram_in("glu_b", [DEPTH, WM])
    w_branch = dram_in("w_branch", [DEPTH, 3, WM, D])
    w_out = dram_in("w_out", [DEPTH, D, D])
    router_w = dram_in("router_w", [D, NE])
    router_b = dram_in("router_b", [NE])
    exp_w_gate = dram_in("exp_w_gate", [DEPTH, NE, D, DFF])
    exp_w_up = dram_in("exp_w_up", [DEPTH, NE, D, DFF])
    exp_w_down = dram_in("exp_w_down", [DEPTH, NE, DFF, D])
    final_norm_g = dram_in("final_norm_g", [D])
    out_d = nc.dram_tensor("out", [2, LS, D], F32, kind="ExternalOutput").ap()

    MOD = dram_scr("MOD", [DEPTH, 3, 6 * D], F32)
    SEQ = []
    for s in range(4):
        isctx = s < 2
        b = s % 2
        Ls = LC if isctx else LS
        SEQ.append(dict(
            s=s, b=b, ctx=isctx, L=Ls, modrow=(2 if isctx else b),
            src=(ctx_in[b] if isctx else x_in[b]),
            CU=dram_scr(f"CU{s}", [WM, Ls], BF16),
            YC=dram_scr(f"YC{s}", [WM, Ls], BF16),
            MAB=dram_scr(f"MAB{s}", [D, Ls], BF16),
            GC=dram_scr(f"GC{s}", [D, Ls], BF16),
            XN2T=dram_scr(f"XN2T{s}", [D, Ls], BF16),
            HA=dram_scr(f"HA{s}", [Ls, D], F32),
            HB=dram_scr(f"HB{s}", [Ls, D], F32),
        ))
    WG = dram_scr("WG", [DEPTH, NE, D, DFF], BF16)
    WU = dram_scr("WU", [DEPTH, NE, D, DFF], BF16)
    WD = dram_scr("WD", [DEPTH, NE * DFF, D], BF16)
    W1D = dram_scr("W1D", [DEPTH, 2, 128, 32, 128], BF16)
    W2D = dram_scr("W2D", [DEPTH, 2, 64, 32, 2, 128], BF16)
    KMD = dram_scr("KMD", [DEPTH, 128, 32, 128], BF16)
    SELD = dram_scr("SELD", [128, 8, 8, 128], BF16)
    SELTD = dram_scr("SELTD", [128, 8, 8, 128], BF16)

    def sb(name, shape, dt, stack=None):
        K.uid += 1
        return (stack or es).enter_context(nc.sbuf_tensor(f"{name}_{K.uid}", list(shape), dt))

    @contextmanager
    def scope():
        st = ExitStack()
        K.open_scope()
        try:
            yield st
        finally:
            K.barrier()
            K.close_scope()
            st.close()

    def mkring(name, shape, dt, n, stack):
        return Ring([Buf(sb(f"{name}{i}", shape, dt, stack)) for i in range(n)])

    ident_bf = sb("ident_bf", [128, 128], BF16)
    ident_f = sb("ident_f", [128, 128], F32)
    neg_half = sb("neg_half", [128, 1], F32)
    onesel = sb("onesel", [16, NE, 128], BF16)
    a8_ar = sb("a8_ar", [64, DEPTH * 2, 2, 32], F32)
    a8_ai = sb("a8_ai", [64, DEPTH * 2, 2, 32], F32)
    h0st = sb("h0st", [64, 4, 2, 32], F32)
    B_const = Buf()
    B_a8 = Buf()
    B_h0 = Buf()

    ps_t = [es.enter_context(nc.psum_tensor(f"ps{i}", [128, 512], F32)) for i in range(8)]
    PS = [Buf(ps_t[i]) for i in range(8)]
    ps8 = Ring(PS)
    psA = Ring(PS[0:4])
    psB = Ring(PS[4:8])

    pe, act, dve, pool, sp = K.pe, K.act, K.dve, K.pool, K.sp
    V, A, T, G, S = nc.vector, nc.scalar, nc.tensor, nc.gpsimd, nc.sync

    def mm_group(out_ap, pairs, reads, pbuf, pw=False):
        def emit():
            n = len(pairs)
            ins = None
            for i, (lh, rh) in enumerate(pairs):
                ins = T.matmul(out_ap, lh, rh, start=(i == 0), stop=(i == n - 1))
            return ins
        if pw:
            return K.do(pe, emit, reads=reads, pwrites=[pbuf])
        return K.do(pe, emit, reads=reads, writes=[pbuf])

    def load_T(dst, dst_ap, srcs, F, P, partial=False):
        with scope() as tmp:
            stg = Buf(sb("ldT", [F, P], F32, tmp))
            K.dma(sp, [lambda r0=r0, n=n, a=a: S.dma_start(out=stg.ap[r0:r0 + n, :], in_=a) for (r0, n, a) in srcs],
                  stg, writes=[stg])
            pb = ps8.next()
            K.do(pe, lambda: T.transpose(pb.ap[0:P, 0:F], stg.ap[:], ident_f[0:F, 0:F]), reads=[stg, B_const], writes=[pb])
            kw = dict(pwrites=[dst]) if partial else dict(writes=[dst])
            K.do(act, lambda: A.copy(dst_ap, pb.ap[0:P, 0:F]), reads=[pb], **kw)

    def init_consts():
        def emit():
            G.memset(ident_bf[:], 0.0)
            G.affine_select(out=ident_bf[:], in_=ident_bf[:], compare_op=ALU.not_equal, fill=1.0,
                            base=0, pattern=[[-1, 128]], channel_multiplier=1)
            G.memset(ident_f[:], 0.0)
            G.affine_select(out=ident_f[:], in_=ident_f[:], compare_op=ALU.not_equal, fill=1.0,
                            base=0, pattern=[[-1, 128]], channel_multiplier=1)
            G.memset(neg_half[:], -0.5)
            G.memset(onesel[:], 0.0)
            G.memset(h0st[:], 0.0)
            return G.affine_select(out=onesel[:], in_=onesel[:], compare_op=ALU.not_equal, fill=1.0,
                                   base=0, pattern=[[-1, NE], [0, 128]], channel_multiplier=1)
        K.do(pool, emit, writes=[B_const, B_h0])
        with scope() as ph:
            selt = sb("selt", [128, 8, 8, 128], BF16, ph)
            Bs = Buf()

            def emit_sel(transposed):
                def f():
                    ins = G.memset(selt[:], 0.0)
                    idv = ident_bf[:].rearrange("p (a b) -> p a b", b=16)
                    for j in range(8):
                        if not transposed:
                            ins = G.tensor_copy(selt[:, :, j, 16 * j:16 * j + 16], idv)
                        else:
                            ins = G.tensor_copy(selt[:, j, :, 16 * j:16 * j + 16], idv)
                    return ins
                return f
            K.do(pool, emit_sel(False), reads=[B_const], writes=[Bs])
            K.dma(sp, [lambda: S.dma_start(out=SELD, in_=selt[:])], Bs, reads=[Bs])
            K.do(pool, emit_sel(True), reads=[B_const], writes=[Bs])
            K.dma(sp, [lambda: S.dma_start(out=SELTD, in_=selt[:])], Bs, reads=[Bs])

    B_expw = [Buf() for _ in range(DEPTH)]

    def convert_experts(l):
        ems = []
        for e in range(NE):
            ems.append(lambda e=e: G.dma_start(out=WG[l, e].rearrange("(r a) f -> r (a f)", r=64), in_=exp_w_gate[l, e].rearrange("(r a) f -> r (a f)", r=64)))
            ems.append(lambda e=e: G.dma_start(out=WU[l, e].rearrange("(r a) f -> r (a f)", r=64), in_=exp_w_up[l, e].rearrange("(r a) f -> r (a f)", r=64)))
            ems.append(lambda e=e: G.dma_start(out=WD[l, e * DFF:(e + 1) * DFF, :].rearrange("(r a) f -> r (a f)", r=64), in_=exp_w_down[l, e].rearrange("(r a) f -> r (a f)", r=64)))
        K.dma(pool, ems, B_expw[l], pwrites=[B_expw[l]])

    def phase_adaln(l):
        with scope() as ph:
            csb = Buf(sb("csb", [3, D], F32, ph))
            scT = Buf(sb("scT", [128, 8, 3], F32, ph))
            bm = Buf(sb("bm", [3, 6 * D], F32, ph))
            modsb = Buf(sb("modsb", [3, 6 * D], F32, ph))
            wmr = mkring("wm", [128, 8, 512], F32, 2, ph)
            K.dma(sp, [lambda: S.dma_start(out=csb.ap[:], in_=cvec)], csb, writes=[csb])
            K.dma(sp, [lambda: S.dma_start(out=bm.ap[:], in_=b_mod[l].partition_broadcast(3))], bm, writes=[bm])
            pb = ps8.next()
            K.do(pe, lambda: [T.transpose(pb.ap[:, kt * 3:kt * 3 + 3], csb.ap[:, kt * 128:(kt + 1) * 128],
                                          ident_f[0:3, 0:3]) for kt in range(8)][-1],
                 reads=[csb, B_const], writes=[pb])
            K.do(act, lambda: A.activation(out=scT.ap[:], in_=pb.ap[:, 0:24].rearrange("p (k j) -> p k j", k=8),
                                           func=AF.Silu), reads=[pb], writes=[scT])
            for cc in range(12):
                wm = wmr.next()
                K.dma(sp, [lambda: S.dma_start(out=wm.ap[:], in_=w_mod[l][:, cc * 512:(cc + 1) * 512]
                                               .rearrange("(kt p) n -> p kt n", p=128))], wm, writes=[wm])
                pb = ps8.next()
                mm_group(pb.ap[0:3, :], [(scT.ap[:, kt, :], wm.ap[:, kt, :]) for kt in range(8)],
                         reads=[scT, wm], pbuf=pb)
                K.do(dve, lambda: V.tensor_tensor(out=modsb.ap[:, cc * 512:(cc + 1) * 512], in0=pb.ap[0:3, :],
                                                  in1=bm.ap[:, cc * 512:(cc + 1) * 512], op=ALU.add),
                     reads=[pb, bm], pwrites=[modsb])
            K.dma(sp, [lambda: S.dma_start(out=MOD[l], in_=modsb.ap[:])], modsb, reads=[modsb])

    def make_mod_tiles(l, modrow, g_row, sc_off, sh_off, ph, name):
        gt = Buf(sb(name + "_g", [128, D], F32, ph))
        ht = Buf(sb(name + "_h", [128, D], F32, ph))
        with scope() as tmp:
            st = Buf(sb(name + "_s", [128, D], F32, tmp))
            K.dma(sp, [lambda: S.dma_start(out=gt.ap[:], in_=g_row.partition_broadcast(128))], gt, writes=[gt])
            K.dma(sp, [lambda: S.dma_start(out=st.ap[:], in_=MOD[l, modrow, sc_off:sc_off + D].partition_broadcast(128))],
                  st, writes=[st])
            K.dma(sp, [lambda: S.dma_start(out=ht.ap[:], in_=MOD[l, modrow, sh_off:sh_off + D].partition_broadcast(128))],
                  ht, writes=[ht])
            K.do(dve, lambda: V.scalar_tensor_tensor(out=gt.ap[:], in0=st.ap[:], scalar=1.0, in1=gt.ap[:],
                                                     op0=ALU.add, op1=ALU.mult), reads=[st], writes=[gt])
        return gt, ht

    def rstd_from_sumsq(ss):
        K.do(dve, lambda: V.tensor_scalar(ss.ap[:, 0:1], ss.ap[:, 0:1], 1.0 / D, EPS, ALU.mult, ALU.add), writes=[ss])
        K.do(pool, lambda: G.tensor_tensor(ss.ap[:, 1:2], ss.ap[:, 0:1], neg_half[:], ALU.pow),
             reads=[B_const], writes=[ss])

    def rms_mod(xt, Gb, SHb, xn, tmp, ss):
        K.do(dve, lambda: V.tensor_tensor(out=tmp.ap[:], in0=xt.ap[:], in1=xt.ap[:], op=ALU.mult), reads=[xt], writes=[tmp])
        K.do(dve, lambda: V.tensor_reduce(out=ss.ap[:, 0:1], in_=tmp.ap[:], axis=AX.X, op=ALU.add), reads=[tmp], writes=[ss])
        rstd_from_sumsq(ss)
        K.do(dve, lambda: V.scalar_tensor_tensor(out=tmp.ap[:], in0=xt.ap[:], scalar=ss.ap[:, 1:2], in1=Gb.ap[:],
                                                 op0=ALU.mult, op1=ALU.mult), reads=[xt, ss, Gb], writes=[tmp])
        if SHb is None:
            return
        K.do(dve, lambda: V.tensor_tensor(out=xn.ap[:], in0=tmp.ap[:], in1=SHb.ap[:], op=ALU.add),
             reads=[tmp, SHb], writes=[xn])

    def transpose_tile(xn, dest_ap3, dest_buf, pw=False):
        pb = ps8.next()
        pv = pb.ap[:].bitcast(BF16)
        K.do(pe, lambda: [T.transpose(pv[:, kt * 128:(kt + 1) * 128], xn.ap[:, kt * 128:(kt + 1) * 128], ident_bf[:])
                          for kt in range(8)][-1], reads=[xn, B_const], writes=[pb])
        kw = dict(pwrites=[dest_buf]) if pw else dict(writes=[dest_buf])
        K.do(act, lambda: A.copy(dest_ap3, pv.rearrange("p (k t) -> p k t", k=8)), reads=[pb], **kw)

    def seq_src(l, sq):
        return sq["src"] if l == 0 else sq["HB"]

    def phase_front(l, sq, only_cu=False):
        Ls = sq["L"]
        N = min(512, Ls)
        ntt = Ls // N
        nj = Ls // 128
        jpt = N // 128
        grid = not sq["ctx"]
        with scope() as ph:
            xnT = sb("xnT", [128, 8, Ls], BF16, ph)
            B_xn = [Buf() for _ in range(nj)]
            with scope() as p1:
                Gb, SHb = make_mod_tiles(l, sq["modrow"], norm1_g[l], D, 0, p1, "m1")
                xring = mkring("xin", [128, D], F32, 2, p1)
                tmpb = Buf(sb("tmp1", [128, D], F32, p1))
                xnr = mkring("xnr", [128, D], BF16, 2, p1)
                ssr = mkring("ss", [128, 2], F32, 2, p1)
                src = seq_src(l, sq)
                for j in range(nj):
                    xt = xring.next()
                    K.dma(sp, [lambda: S.dma_start(out=xt.ap[:], in_=src[j * 128:(j + 1) * 128, :])], xt, writes=[xt])
                    xn = xnr.next()
                    ss = ssr.next()
                    rms_mod(xt, Gb, SHb, xn, tmpb, ss)
                    transpose_tile(xn, xnT[:, :, j * 128:(j + 1) * 128], B_xn[j])
            if "XNT" in debug and sq["s"] == debug_seq:
                K.dma(sp, [lambda: S.dma_start(out=DBG["XNT"].rearrange("(kt p) t -> p kt t", p=128), in_=xnT[:])],
                      Buf(), reads=B_xn)

            wring = mkring("wblk", [128, 8, 512], BF16, 4, ph)
            tf = mkring("tf", [128, N], F32, 6, ph)
            tb = mkring("tb", [128, N], BF16, 8, ph)

            def load_wblk(cb):
                wb = wring.next()
                K.dma(pool, [lambda: G.dma_start(out=wb.ap[:], in_=w_in[l][:, cb * 512:(cb + 1) * 512]
                                                 .rearrange("(kt p) n -> p kt n", p=128))], wb, writes=[wb])
                return wb

            def proj_fm(pb, wb, mt, tt):
                mm_group(pb.ap[:, 0:N], [(wb.ap[:, kt, mt * 128:(mt + 1) * 128], xnT[:, kt, tt * N:(tt + 1) * N])
                                         for kt in range(8)],
                         reads=[wb] + B_xn[tt * jpt:(tt + 1) * jpt], pbuf=pb)

            def store(dst, t):
                K.dma(pool, [lambda: G.dma_start(out=dst, in_=t.ap[:])], t, reads=[t])

            wb = load_wblk(5)
            for mt in range(4):
                for tt in range(ntt):
                    pb = ps8.next()
                    proj_fm(pb, wb, mt, tt)
                    cs = tb.next()
                    K.do(act, lambda: A.copy(cs.ap[:], pb.ap[:, 0:N]), reads=[pb], writes=[cs])
                    store(sq["CU"][mt * 128:(mt + 1) * 128, tt * N:(tt + 1) * N], cs)
            if only_cu:
                return

            uT = sb("uT", [128, 4, Ls], BF16, ph)
            B_u = [[Buf() for _ in range(ntt)] for _ in range(4)]
            wb = load_wblk(0)
            for mt in range(4):
                for tt in range(ntt):
                    pb = ps8.next()
                    proj_fm(pb, wb, mt, tt)
                    K.do(act, lambda: A.activation(out=uT[:, mt, tt * N:(tt + 1) * N], in_=pb.ap[:, 0:N],
                                                   func=AF.Gelu_apprx_tanh), reads=[pb], writes=[B_u[mt][tt]])

            wsq = Buf(sb("wsq", [128, 4, 128], BF16, ph))
            wsT = Buf(sb("wsT", [128, 4, 128], BF16, ph))
            sguc = Buf(sb("sguc", [128, 4], F32, ph))
            bsrow = Buf(sb("bsrow", [128, 4, 128], F32, ph))
            K.dma(pool, [lambda: G.dma_start(out=wsq.ap[:], in_=sgu_w[l].rearrange("h q k -> q h k"))], wsq, writes=[wsq])
            load_T(sguc, sguc.ap[:], [(0, 4, sgu_norm_g[l].rearrange("(h p) -> h p", p=128))], 4, 128)
            K.dma(sp, [lambda: S.dma_start(out=bsrow.ap[:].rearrange("p h q -> p (h q)"),
                                           in_=sgu_b[l].rearrange("h q -> (h q)").partition_broadcast(128))],
                  bsrow, writes=[bsrow])
            pb = ps8.next()
            pv = pb.ap[:].bitcast(BF16)
            K.do(pe, lambda: [T.transpose(pv[:, h * 128:(h + 1) * 128], wsq.ap[:, h, :], ident_bf[:])
                              for h in range(4)][-1], reads=[wsq, B_const], writes=[pb])
            K.do(act, lambda: A.copy(wsT.ap[:], pv[:, 0:512].rearrange("p (h q) -> p h q", h=4)),
                 reads=[pb], writes=[wsT])
            yaT = sb("yaT", [128, 4, Ls], BF16, ph)
            B_ya = [Buf() for _ in range(ntt)]
            gvr = mkring("gv", [128, 512], F32, 2, ph)
            vnr = mkring("vn", [128, 512], BF16, 2, ph)
            bnr = mkring("bnst", [128, 8], F32, 2, ph)
            wb = load_wblk(1)
            mixps = None
            for j in range(nj):
                tt, jj = divmod(j, jpt)
                if jj == 0:
                    mixps = [psB.next() for _ in range(4)]
                pb = psA.next()
                mm_group(pb.ap[:, :], [(xnT[:, kt, j * 128:(j + 1) * 128], wb.ap[:, kt, :]) for kt in range(8)],
                         reads=[wb, B_xn[j]], pbuf=pb)
                gv = gvr.next()
                K.do(act, lambda: A.activation(out=gv.ap[:], in_=pb.ap[:], func=AF.Gelu_apprx_tanh),
                     reads=[pb], writes=[gv])
                st = bnr.next()
                K.do(dve, lambda: V.bn_stats(out=st.ap[:, 0:6], in_=gv.ap[:]), reads=[gv], writes=[st])
                K.do(dve, lambda: V.bn_aggr(out=st.ap[:, 6:8], in_=st.ap[:, 0:6]), writes=[st])
                K.do(dve, lambda: V.tensor_scalar(st.ap[:, 7:8], st.ap[:, 7:8], EPS, None, ALU.add), writes=[st])
                K.do(pool, lambda: G.tensor_tensor(st.ap[:, 7:8], st.ap[:, 7:8], neg_half[:], ALU.pow),
                     reads=[B_const], writes=[st])
                vn = vnr.next()
                K.do(dve, lambda: V.tensor_scalar(vn.ap[:], gv.ap[:], st.ap[:, 6:7], st.ap[:, 7:8],
                                                  ALU.subtract, ALU.mult), reads=[gv, st], writes=[vn])
                for h in range(4):
                    mm_group(mixps[h].ap[:, jj * 128:(jj + 1) * 128], [(vn.ap[:, h * 128:(h + 1) * 128], wsT.ap[:, h, :])],
                             reads=[vn, wsT], pbuf=mixps[h], pw=(jj > 0))
                if jj == jpt - 1:
                    for h in range(4):
                        tm = tf.next()
                        bsap = bass.AP(bsrow.ap, h * 128, [[512, 128], [0, jpt], [1, 128]])
                        K.do(dve, lambda: V.scalar_tensor_tensor(
                            out=tm.ap[:].rearrange("p (a q) -> p a q", a=jpt),
                            in0=mixps[h].ap[:, 0:N].rearrange("p (a q) -> p a q", a=jpt),
                            scalar=sguc.ap[:, h:h + 1], in1=bsap, op0=ALU.mult, op1=ALU.add),
                            reads=[mixps[h], sguc, bsrow], writes=[tm])
                        K.do(dve, lambda: V.tensor_tensor(out=yaT[:, h, tt * N:(tt + 1) * N], in0=tm.ap[:],
                                                          in1=uT[:, h, tt * N:(tt + 1) * N], op=ALU.mult),
                             reads=[tm, B_u[h][tt]], pwrites=[B_ya[tt]])

            cw = Buf(sb("cw", [128, 3, 4], F32, ph))
            load_T(cw, cw.ap[:].rearrange("p j m -> p (j m)"), [(0, 12, conv_w[l].rearrange("j (m p) -> (j m) p", p=128))], 12, 128)
            ybT = sb("ybT", [128, 4, Ls], BF16, ph)
            B_yb = [Buf() for _ in range(ntt)]
            wbb, wbc, wbh = load_wblk(2), load_wblk(3), load_wblk(4)
            for mt in range(4):
                for tt in range(ntt):
                    pc_, ph_, pb_ = ps8.next(), ps8.next(), ps8.next()
                    proj_fm(pc_, wbc, mt, tt)
                    proj_fm(ph_, wbh, mt, tt)
                    proj_fm(pb_, wbb, mt, tt)
                    bcs = tf.next()
                    K.do(act, lambda: A.copy(bcs.ap[:], pc_.ap[:, 0:N]), reads=[pc_], writes=[bcs])
                    Pt = tf.next()
                    K.do(dve, lambda: V.tensor_tensor(out=Pt.ap[:], in0=bcs.ap[:], in1=ph_.ap[:, 0:N], op=ALU.mult),
                         reads=[bcs, ph_], writes=[Pt])
                    Tt = tf.next()
                    K.do(dve, lambda: V.tensor_scalar(Tt.ap[:], Pt.ap[:], cw.ap[:, 1, mt:mt + 1], None, ALU.mult),
                         reads=[Pt, cw], writes=[Tt])
                    if grid:
                        Tv = Tt.ap[:].rearrange("p (r c) -> p r c", c=64)
                        Pv = Pt.ap[:].rearrange("p (r c) -> p r c", c=64)
                        o1, i1, o2, i2 = Tv[:, :, 1:64], Pv[:, :, 0:63], Tv[:, :, 0:63], Pv[:, :, 1:64]
                    else:
                        o1, i1, o2, i2 = Tt.ap[:, 1:N], Pt.ap[:, 0:N - 1], Tt.ap[:, 0:N - 1], Pt.ap[:, 1:N]
                    K.do(dve, lambda: V.scalar_tensor_tensor(out=o1, in0=i1, scalar=cw.ap[:, 0, mt:mt + 1], in1=o1,
                                                             op0=ALU.mult, op1=ALU.add), reads=[Pt], writes=[Tt])
                    K.do(dve, lambda: V.scalar_tensor_tensor(out=o2, in0=i2, scalar=cw.ap[:, 2, mt:mt + 1], in1=o2,
                                                             op0=ALU.mult, op1=ALU.add), reads=[Pt], writes=[Tt])
                    K.do(dve, lambda: V.tensor_tensor(out=ybT[:, mt, tt * N:(tt + 1) * N], in0=Tt.ap[:],
                                                      in1=pb_.ap[:, 0:N], op=ALU.mult),
                         reads=[Tt, pb_], pwrites=[B_yb[tt]])

            wbr = [Buf(sb(f"wbr{i}", [128, 4, D], BF16, ph)) for i in range(2)]
            for i in range(2):
                K.dma(pool, [lambda: G.dma_start(out=wbr[i].ap[:], in_=w_branch[l, i]
                                                 .rearrange("(kt p) n -> p kt n", p=128))], wbr[i], writes=[wbr[i]])
            for half in range(2):
                wga, wgb, wgc = load_wblk(6 + half), load_wblk(8 + half), load_wblk(10 + half)
                for dtl in range(4):
                    dt_ = half * 4 + dtl
                    for tt in range(ntt):
                        pga, pgb, pgc, pba, pbb = [ps8.next() for _ in range(5)]
                        proj_fm(pga, wga, dtl, tt)
                        proj_fm(pgb, wgb, dtl, tt)
                        proj_fm(pgc, wgc, dtl, tt)
                        mm_group(pba.ap[:, 0:N], [(wbr[0].ap[:, kt, dt_ * 128:(dt_ + 1) * 128],
                                                   yaT[:, kt, tt * N:(tt + 1) * N]) for kt in range(4)],
                                 reads=[wbr[0], B_ya[tt]], pbuf=pba)
                        mm_group(pbb.ap[:, 0:N], [(wbr[1].ap[:, kt, dt_ * 128:(dt_ + 1) * 128],
                                                   ybT[:, kt, tt * N:(tt + 1) * N]) for kt in range(4)],
                                 reads=[wbr[1], B_yb[tt]], pbuf=pbb)
                        ga, gb, gc = tb.next(), tb.next(), tb.next()
                        for gg, pp in ((ga, pga), (gb, pgb), (gc, pgc)):
                            K.do(act, lambda: A.activation(out=gg.ap[:], in_=pp.ap[:, 0:N], func=AF.Sigmoid),
                                 reads=[pp], writes=[gg])
                        store(sq["GC"][dt_ * 128:(dt_ + 1) * 128, tt * N:(tt + 1) * N], gc)
                        m1, m2 = tf.next(), tf.next()
                        K.do(dve, lambda: V.tensor_tensor(out=m1.ap[:], in0=ga.ap[:], in1=pba.ap[:, 0:N], op=ALU.mult),
                             reads=[ga, pba], writes=[m1])
                        K.do(dve, lambda: V.tensor_tensor(out=m2.ap[:], in0=gb.ap[:], in1=pbb.ap[:, 0:N], op=ALU.mult),
                             reads=[gb, pbb], writes=[m2])
                        mab = tb.next()
                        K.do(pool, lambda: G.tensor_tensor(out=mab.ap[:], in0=m1.ap[:], in1=m2.ap[:], op=ALU.add),
                             reads=[m1, m2], writes=[mab])
                        store(sq["MAB"][dt_ * 128:(dt_ + 1) * 128, tt * N:(tt + 1) * N], mab)

    def phase_s5_tables(l):
        with scope() as po:
            Kacc = Buf(sb("Kacc", [128, 32, 128], F32, po))
            maskf = sb("maskf", [128, 128], F32, po)
            maskb = sb("maskb", [128, 128], F32, po)
            dcol = Buf(sb("dcol", [128, 32], F32, po))
            Bm = Buf()

            def emit_masks():
                G.memset(maskf[:], 0.0)
                G.affine_select(out=maskf[:].rearrange("p (t h) -> p t h", h=16), in_=maskf[:].rearrange("p (t h) -> p t h", h=16),
                                compare_op=ALU.is_ge, fill=1.0, base=-16, pattern=[[-16, 8], [0, 16]], channel_multiplier=1)
                G.memset(maskb[:], 1.0)
                return G.affine_select(out=maskb[:].rearrange("p (t h) -> p t h", h=16), in_=maskb[:].rearrange("p (t h) -> p t h", h=16),
                                       compare_op=ALU.is_ge, fill=0.0, base=0, pattern=[[-16, 8], [0, 16]], channel_multiplier=1)
            K.do(pool, emit_masks, writes=[Bm])
            with scope() as tmpd:
                dstg = Buf(sb("dstg", [32, 8, 16], F32, tmpd))
                K.dma(sp, [lambda k=k: S.dma_start(out=dstg.ap[:, k, :], in_=s5_d[l].rearrange("(g h) -> g h", h=16))
                           for k in range(8)], dstg, writes=[dstg])
                pbd = ps8.next()
                K.do(pe, lambda: T.transpose(pbd.ap[:, 0:32], dstg.ap[:].rearrange("g k h -> g (k h)"), ident_f[0:32, 0:32]),
                     reads=[dstg, B_const], writes=[pbd])
                K.do(act, lambda: A.copy(dcol.ap[:], pbd.ap[:, 0:32]), reads=[pbd], writes=[dcol])
            for d in range(2):
                with scope() as ph:
                    def t(name, shape, dt=F32):
                        return Buf(sb(name, shape, dt, ph))
                    lre, lim, dtb = t("lre", [64, 32]), t("lim", [64, 32]), t("dtb", [64, 32])
                    load_T(lre, lre.ap[:], [(0, 32, s5_lam_re[l, d])], 32, 64)
                    load_T(lim, lim.ap[:], [(0, 32, s5_lam_im[l, d])], 32, 64)
                    K.dma(sp, [lambda: S.dma_start(out=dtb.ap[:], in_=s5_log_dt[l, d].partition_broadcast(64))],
                          dtb, writes=[dtb])
                    K.do(act, lambda: A.activation(out=dtb.ap[:], in_=dtb.ap[:], func=AF.Exp), writes=[dtb])
                    X, ANG = t("X", [64, 32]), t("ANG", [64, 32])
                    K.do(dve, lambda: V.tensor_tensor(out=X.ap[:], in0=lre.ap[:], in1=dtb.ap[:], op=ALU.mult),
                         reads=[lre, dtb], writes=[X])
                    K.do(dve, lambda: V.tensor_tensor(out=ANG.ap[:], in0=lim.ap[:], in1=dtb.ap[:], op=ALU.mult),
                         reads=[lim, dtb], writes=[ANG])
                    EV = t("EV", [64, 16])
                    K.do(pool, lambda: [G.memset(EV.ap[:, i:i + 1], float(i - 7)) for i in range(16)][-1], writes=[EV])

                    def outer(dst, src):
                        in0 = bass.AP(src.ap, 0, [[32, 64], [1, 32], [0, 16]])
                        in1 = bass.AP(EV.ap, 0, [[16, 64], [0, 32], [1, 16]])
                        K.do(dve, lambda: V.tensor_tensor(out=dst.ap[:], in0=in0, in1=in1, op=ALU.mult),
                             reads=[src, EV], writes=[dst])
                    XE, AE = t("XE", [64, 32, 16]), t("AE", [64, 32, 16])
                    outer(XE, X)
                    outer(AE, ANG)
                    MAG = t("MAG", [64, 32, 16])
                    K.do(act, lambda: A.activation(out=MAG.ap[:], in_=XE.ap[:], func=AF.Exp), reads=[XE], writes=[MAG])
                    PRE, PIM = t("PRE", [64, 32, 16]), t("PIM", [64, 32, 16])
                    YI = t("YI", [64, 32, 16], I32)
                    YF, FR, MK = t("YF", [64, 32, 16]), t("FR", [64, 32, 16]), t("MK", [64, 32, 16])
                    for (dst, off) in ((PIM, 64.0), (PRE, 64.25)):
                        K.do(dve, lambda: V.tensor_scalar(FR.ap[:], AE.ap[:], 1.0 / TWO_PI, off, ALU.mult, ALU.add),
                             reads=[AE], writes=[FR])
                        K.do(dve, lambda: V.tensor_copy(YI.ap[:], FR.ap[:]), reads=[FR], writes=[YI])
                        K.do(dve, lambda: V.tensor_copy(YF.ap[:], YI.ap[:]), reads=[YI], writes=[YF])
                        K.do(dve, lambda: V.tensor_tensor(out=FR.ap[:], in0=FR.ap[:], in1=YF.ap[:], op=ALU.subtract),
                             reads=[YF], writes=[FR])
                        K.do(dve, lambda: V.tensor_scalar(MK.ap[:], FR.ap[:], 0.5, None, ALU.is_gt), reads=[FR], writes=[MK])
                        K.do(dve, lambda: V.tensor_tensor(out=FR.ap[:], in0=FR.ap[:], in1=MK.ap[:], op=ALU.subtract),
                             reads=[MK], writes=[FR])
                        K.do(dve, lambda: V.tensor_scalar(FR.ap[:], FR.ap[:], -0.5, 0.5, ALU.max, ALU.min), writes=[FR])
                        K.do(act, lambda: A.activation(out=dst.ap[:], in_=FR.ap[:], func=AF.Sin, scale=TWO_PI),
                             reads=[FR], writes=[dst])
                        K.do(dve, lambda: V.tensor_tensor(out=dst.ap[:], in0=dst.ap[:], in1=MAG.ap[:], op=ALU.mult),
                             reads=[MAG], writes=[dst])
                    ld = l * 2 + d
                    K.do(dve, lambda: [V.tensor_copy(a8_ar[:, ld, 0, :], PRE.ap[:, :, 15]),
                                       V.tensor_copy(a8_ar[:, ld, 1, :], PRE.ap[:, :, 15]),
                                       V.tensor_scalar(a8_ai[:, ld, 0, :], PIM.ap[:, :, 15], -1.0, None, ALU.mult),
                                       V.tensor_copy(a8_ai[:, ld, 1, :], PIM.ap[:, :, 15])][-1],
                         reads=[PRE, PIM], pwrites=[B_a8])
                    den, nr, fre, fim, t1, t2 = (t(n, [64, 32]) for n in ("den", "nr", "fre", "fim", "t1s", "t2s"))

                    def tt_(out, a, b, op, rd=(), wr=()):
                        K.do(dve, lambda: V.tensor_tensor(out=out, in0=a, in1=b, op=op), reads=list(rd), writes=list(wr))
                    tt_(den.ap[:], lre.ap[:], lre.ap[:], ALU.mult, [lre], [den])
                    tt_(t1.ap[:], lim.ap[:], lim.ap[:], ALU.mult, [lim], [t1])
                    tt_(den.ap[:], den.ap[:], t1.ap[:], ALU.add, [t1], [den])
                    K.do(dve, lambda: V.reciprocal(den.ap[:], den.ap[:]), writes=[den])
                    K.do(dve, lambda: V.tensor_scalar(nr.ap[:], PRE.ap[:, :, 8], -1.0, None, ALU.add), reads=[PRE], writes=[nr])
                    tt_(fre.ap[:], nr.ap[:], lre.ap[:], ALU.mult, [nr, lre], [fre])
                    tt_(t1.ap[:], PIM.ap[:, :, 8], lim.ap[:], ALU.mult, [PIM, lim], [t1])
                    tt_(fre.ap[:], fre.ap[:], t1.ap[:], ALU.add, [t1], [fre])
                    tt_(fre.ap[:], fre.ap[:], den.ap[:], ALU.mult, [den], [fre])
                    tt_(fim.ap[:], PIM.ap[:, :, 8], lre.ap[:], ALU.mult, [PIM, lre], [fim])
                    tt_(t2.ap[:], nr.ap[:], lim.ap[:], ALU.mult, [nr, lim], [t2])
                    tt_(fim.ap[:], fim.ap[:], t2.ap[:], ALU.subtract, [t2], [fim])
                    tt_(fim.ap[:], fim.ap[:], den.ap[:], ALU.mult, [den], [fim])
                    Bre, Bim = t("Bre", [64, 32, 16]), t("Bim", [64, 32, 16])
                    K.dma(sp, [lambda: S.dma_start(out=Bre.ap[:], in_=s5_b_re[l, d].rearrange("g p h -> p g h"))], Bre, writes=[Bre])
                    K.dma(sp, [lambda: S.dma_start(out=Bim.ap[:], in_=s5_b_im[l, d].rearrange("g p h -> p g h"))], Bim, writes=[Bim])
                    BBre, BBim, tq = t("BBre", [64, 32, 16]), t("BBim", [64, 32, 16]), t("tq", [64, 32, 16])

                    def fb(fsrc):
                        return bass.AP(fsrc.ap, 0, [[32, 64], [1, 32], [0, 16]])

                    def cmul(ore, oim, are, aim, bre, bim, tmp, rd):
                        tt_(ore[1], are, bre, ALU.mult, rd, [ore[0]])
                        tt_(tmp[1], aim, bim, ALU.mult, rd, [tmp[0]])
                        tt_(ore[1], ore[1], tmp[1], ALU.subtract, [tmp[0]], [ore[0]])
                        tt_(oim[1], are, bim, ALU.mult, rd, [oim[0]])
                        tt_(tmp[1], aim, bre, ALU.mult, rd, [tmp[0]])
                        tt_(oim[1], oim[1], tmp[1], ALU.add, [tmp[0]], [oim[0]])
                    cmul((BBre, BBre.ap[:]), (BBim, BBim.ap[:]), fb(fre), fb(fim), Bre.ap[:], Bim.ap[:], (tq, tq.ap[:]),
                         [fre, fim, Bre, Bim])
                    CTre, CTim = t("CTre", [64, 512]), t("CTim", [64, 512])
                    cin = t("cin", [128, 4, 64])
                    for (csrc, cdst) in ((s5_c_re, CTre), (s5_c_im, CTim)):
                        K.dma(sp, [lambda: S.dma_start(out=cin.ap[:], in_=csrc[l, d].rearrange("g h p -> (g h) p")
                                                       .rearrange("(a r) p -> r a p", r=128))], cin, writes=[cin])
                        pb = ps8.next()
                        K.do(pe, lambda: [T.transpose(pb.ap[0:64, a * 128:(a + 1) * 128], cin.ap[:, a, :], ident_f[:])
                                          for a in range(4)][-1], reads=[cin, B_const], writes=[pb])
                        K.do(act, lambda: A.copy(cdst.ap[:], pb.ap[0:64, :]), reads=[pb], writes=[cdst])
                    W1Tre, W1Tim, tw = t("W1Tre", [64, 32, 8, 16]), t("W1Tim", [64, 32, 8, 16]), t("tw", [64, 32, 8, 16])

                    def pview(Psrc, start, step):
                        return bass.AP(Psrc.ap, start, [[512, 64], [16, 32], [step, 8], [0, 16]])

                    def bview(Bsrc):
                        return bass.AP(Bsrc.ap, 0, [[512, 64], [16, 32], [0, 8], [1, 16]])
                    s1, st1 = (14, -1) if d == 0 else (7, 1)
                    cmul((W1Tre, W1Tre.ap[:]), (W1Tim, W1Tim.ap[:]), pview(PRE, s1, st1), pview(PIM, s1, st1),
                         bview(BBre), bview(BBim), (tw, tw.ap[:]), [PRE, PIM, BBre, BBim])
                    W2f = t("W2f", [64, 32, 2, 8, 16])
                    W2m = t("W2m", [64, 32, 2, 8, 16])

                    def w2build(dst, s2, st2):
                        ore = bass.AP(dst.ap, 0, [[8192, 64], [256, 32], [16, 8], [1, 16]])
                        oim = bass.AP(dst.ap, 128, [[8192, 64], [256, 32], [16, 8], [1, 16]])
                        cmul((dst, ore), (dst, oim), pview(PRE, s2, st2), pview(PIM, s2, st2),
                             bview(CTre), bview(CTim), (tw, tw.ap[:]), [PRE, PIM, CTre, CTim])
                        K.do(dve, lambda: V.tensor_scalar(oim, oim, -1.0, None, ALU.mult), writes=[dst])
                    s2, st2 = (8, 1) if d == 0 else (15, -1)
                    w2build(W2f, s2, st2)
                    s3, st3 = (0, 1) if d == 0 else (7, -1)
                    w2build(W2m, s3, st3)
                    W2b = t("W2b", [64, 32, 2, 128], BF16)
                    K.do(act, lambda: A.copy(W2b.ap[:].rearrange("p g r x -> p (g r x)"),
                                             W2f.ap[:].rearrange("p g r t h -> p (g r t h)")), reads=[W2f], writes=[W2b])
                    K.dma(sp, [lambda: S.dma_start(out=W2D[l, d], in_=W2b.ap[:])], W2b, reads=[W2b])
                    mask = maskf if d == 0 else maskb
                    tkr = mkring("tk", [128, 4, 128], F32, 2, ph)
                    for g4 in range(8):
                        pb = ps8.next()
                        for gg in range(4):
                            g = g4 * 4 + gg
                            mm_group(pb.ap[:, gg * 128:(gg + 1) * 128],
                                     [(W1Tre.ap[:, g].rearrange("p k h -> p (k h)"), W2m.ap[:, g, 0].rearrange("p t h -> p (t h)")),
                                      (W1Tim.ap[:, g].rearrange("p k h -> p (k h)"), W2m.ap[:, g, 1].rearrange("p t h -> p (t h)"))],
                                     reads=[W1Tre, W1Tim, W2m], pbuf=pb, pw=(gg > 0))
                        mk = bass.AP(mask, 0, [[128, 128], [0, 4], [1, 128]])
                        kv = Kacc.ap[:, g4 * 4:(g4 + 1) * 4, :]
                        pv3 = pb.ap[:].rearrange("p (a x) -> p a x", a=4)
                        if d == 0:
                            K.do(dve, lambda: V.tensor_tensor(out=kv, in0=pv3, in1=mk, op=ALU.mult),
                                 reads=[pb, Bm], pwrites=[Kacc])
                        else:
                            tk = tkr.next()
                            K.do(dve, lambda: V.tensor_tensor(out=tk.ap[:], in0=pv3, in1=mk, op=ALU.mult),
                                 reads=[pb, Bm], writes=[tk])
                            K.do(dve, lambda: V.tensor_tensor(out=kv, in0=kv, in1=tk.ap[:], op=ALU.add),
                                 reads=[tk, Kacc], pwrites=[Kacc])
                    W1b = t("W1b", [128, 32, 128], BF16)
                    for g4 in range(8):
                        pb = ps8.next()

                        def emit_tr():
                            ins = None
                            for gg in range(4):
                                g = g4 * 4 + gg
                                for ri, src in enumerate((W1Tre, W1Tim)):
                                    ins = T.transpose(pb.ap[:, gg * 128 + ri * 64: gg * 128 + ri * 64 + 64],
                                                      src.ap[:, g].rearrange("p k h -> p (k h)"), ident_f[0:64, 0:64])
                            return ins
                        K.do(pe, emit_tr, reads=[W1Tre, W1Tim, B_const], writes=[pb])
                        K.do(act, lambda: A.copy(W1b.ap[:, g4 * 4:(g4 + 1) * 4, :], pb.ap[:].rearrange("p (a x) -> p a x", a=4)),
                             reads=[pb], pwrites=[W1b])
                    K.dma(sp, [lambda: S.dma_start(out=W1D[l, d], in_=W1b.ap[:])], W1b, reads=[W1b])
            Kb = Buf(sb("Kb", [128, 32, 128], BF16, po))
            for g in range(32):
                K.do(dve, lambda: V.scalar_tensor_tensor(out=Kacc.ap[:, g, :], in0=ident_f[:], scalar=dcol.ap[:, g:g + 1],
                                                         in1=Kacc.ap[:, g, :], op0=ALU.mult, op1=ALU.add),
                     reads=[B_const, dcol, Kacc], pwrites=[Kacc])
            K.do(act, lambda: A.copy(Kb.ap[:], Kacc.ap[:]), reads=[Kacc], writes=[Kb])
            K.dma(sp, [lambda: S.dma_start(out=KMD[l], in_=Kb.ap[:])], Kb, reads=[Kb])

    def phase_s5(l, sq):
        Ls = sq["L"]
        C = Ls // 8
        b = sq["b"]
        with scope() as ph:
            U = sb("U", [128, 32, C], BF16, ph)
            B_U = Buf()
            EB = [sb(f"EB{d}", [64, C, 2, 32], BF16, ph) for d in range(2)]
            B_E = [Buf(), Buf()]
            B_H = [Buf(), Buf()]
            with scope() as pa:
                cuT = Buf(sb("cuT", [128, 4, Ls], BF16, pa))
                W1 = Buf(sb("W1", [128, 2, 32, 128], BF16, pa))
                SEL = Buf(sb("SEL", [128, 8, 8, 128], BF16, pa))
                K.dma(sp, [lambda: S.dma_start(out=cuT.ap[:], in_=sq["CU"].rearrange("(m p) t -> p m t", p=128))], cuT, writes=[cuT])
                K.dma(sp, [lambda d=d: S.dma_start(out=W1.ap[:, d], in_=W1D[l, d]) for d in range(2)], W1, writes=[W1])
                K.dma(sp, [lambda: S.dma_start(out=SEL.ap[:], in_=SELD)], SEL, writes=[SEL])
                for g in range(32):
                    mt, gl = divmod(g, 8)
                    pb = ps8.next()
                    mm_group(pb.ap[:, 0:C], [(SEL.ap[:, gl, k, :], cuT.ap[:, mt, k::8]) for k in range(8)],
                             reads=[SEL, cuT], pbuf=pb)
                    if g % 2 == 0:
                        K.do(act, lambda: A.copy(U[:, g, :], pb.ap[:, 0:C]), reads=[pb], pwrites=[B_U])
                    else:
                        K.do(dve, lambda: V.tensor_copy(U[:, g, :], pb.ap[:, 0:C]), reads=[pb], pwrites=[B_U])
                for d in range(2):
                    for g in range(32):
                        pb = ps8.next()
                        mm_group(pb.ap[:, 0:C], [(W1.ap[:, d, g, :], U[:, g, :])], reads=[W1, B_U], pbuf=pb)
                        K.do(act, lambda: A.copy(EB[d][:, :, 0, g], pb.ap[0:64, 0:C]), reads=[pb], pwrites=[B_E[d]])
                        K.do(dve, lambda: V.tensor_copy(EB[d][:, :, 1, g], pb.ap[64:128, 0:C]), reads=[pb], pwrites=[B_E[d]])
            with scope() as pscan:
                st_ = []
                for d in range(2):
                    eng, EO = (dve, V) if d == 0 else (pool, G)
                    ld = l * 2 + d
                    sring = mkring(f"sst{d}", [64, 64], F32, 4, pscan)
                    t1 = Buf(sb(f"sc_t1{d}", [64, 64], F32, pscan))
                    t2 = Buf(sb(f"sc_t2{d}", [64, 64], F32, pscan))
                    ar = a8_ar[:, ld].rearrange("p a g -> p (a g)")
                    ai3 = a8_ai[:, ld]
                    sprev = sring.next()
                    if sq["ctx"]:
                        K.do(eng, lambda: EO.memset(sprev.ap[:], 0.0), writes=[sprev])
                    else:
                        K.do(eng, lambda: EO.tensor_copy(sprev.ap[:], h0st[:, b * 2 + d].rearrange("p a g -> p (a g)")),
                             reads=[B_h0], writes=[sprev])
                    st_.append(dict(eng=eng, EO=EO, sring=sring, t1=t1, t2=t2, ar=ar, ai3=ai3, sprev=sprev))
                for i in range(C):
                    for d in range(2):
                        q = st_[d]
                        eng, EO, t1, t2, ar, ai3, sprev = q["eng"], q["EO"], q["t1"], q["t2"], q["ar"], q["ai3"], q["sprev"]
                        c = i if d == 0 else C - 1 - i
                        snew = q["sring"].next()
                        K.do(eng, lambda: EO.tensor_tensor(out=t1.ap[:], in0=sprev.ap[:], in1=ar, op=ALU.mult),
                             reads=[sprev, B_a8], writes=[t1])
                        swp = bass.AP(sprev.ap, 32, [[64, 64], [-32, 2], [1, 32]])
                        K.do(eng, lambda: EO.tensor_tensor(out=t2.ap[:].rearrange("p (a g) -> p a g", a=2), in0=swp, in1=ai3,
                                                           op=ALU.mult), reads=[sprev, B_a8], writes=[t2])
                        K.do(eng, lambda: EO.tensor_tensor(out=t1.ap[:], in0=t1.ap[:], in1=t2.ap[:], op=ALU.add),
                             reads=[t2], writes=[t1])
                        ev4 = K.do(eng, lambda: EO.tensor_tensor(out=snew.ap[:], in0=t1.ap[:],
                                                                 in1=EB[d][:, c].rearrange("p a g -> p (a g)"), op=ALU.add),
                                   reads=[t1, B_E[d]], writes=[snew])
                        K.do(act, lambda: A.copy(EB[d][:, c].rearrange("p a g -> p (a g)"), sprev.ap[:]),
                             reads=[sprev], pwrites=[B_H[d]], after=[ev4])
                        q["sprev"] = snew
                if sq["ctx"]:
                    for d in range(2):
                        q = st_[d]
                        K.do(q["eng"], lambda: q["EO"].tensor_copy(h0st[:, b * 2 + d].rearrange("p a g -> p (a g)"), q["sprev"].ap[:]),
                             reads=[q["sprev"]], pwrites=[B_h0])
            with scope() as rd:
                W2 = Buf(sb("W2", [64, 2, 32, 2, 128], BF16, rd))
                KM = Buf(sb("KM", [128, 32, 128], BF16, rd))
                SELT = Buf(sb("SELT", [128, 8, 8, 128], BF16, rd))
                Y = sb("Y", [128, 32, C], BF16, rd)
                B_Y = Buf()
                zT = Buf(sb("zT", [128, 4, Ls], BF16, rd))
                K.dma(sp, [lambda d=d: S.dma_start(out=W2.ap[:, d], in_=W2D[l, d]) for d in range(2)], W2, writes=[W2])
                K.dma(sp, [lambda: S.dma_start(out=KM.ap[:], in_=KMD[l])], KM, writes=[KM])
                K.dma(sp, [lambda: S.dma_start(out=SELT.ap[:], in_=SELTD)], SELT, writes=[SELT])
                for g in range(32):
                    pb = ps8.next()
                    pairs = [(KM.ap[:, g, :], U[:, g, :])]
                    for d in range(2):
                        for ri in range(2):
                            pairs.append((W2.ap[:, d, g, ri, :], EB[d][:, :, ri, g]))
                    mm_group(pb.ap[:, 0:C], pairs, reads=[KM, W2, B_U, B_H[0], B_H[1]], pbuf=pb)
                    if g % 2 == 0:
                        K.do(act, lambda: A.copy(Y[:, g, :], pb.ap[:, 0:C]), reads=[pb], pwrites=[B_Y])
                    else:
                        K.do(dve, lambda: V.tensor_copy(Y[:, g, :], pb.ap[:, 0:C]), reads=[pb], pwrites=[B_Y])
                for mt in range(4):
                    for tau in range(8):
                        pb = ps8.next()
                        mm_group(pb.ap[:, 0:C], [(SELT.ap[:, gl, tau, :], Y[:, mt * 8 + gl, :]) for gl in range(8)],
                                 reads=[SELT, B_Y], pbuf=pb)
                        K.do(act, lambda: A.activation(out=zT.ap[:, mt, tau::8], in_=pb.ap[:, 0:C], func=AF.Gelu_apprx_tanh),
                             reads=[pb], pwrites=[zT])
                K.dma(sp, [lambda: S.dma_start(out=sq["YC"].rearrange("(m p) t -> p m t", p=128), in_=zT.ap[:])],
                      zT, reads=[zT])

    def phase_back(l, sq):
        Ls = sq["L"]
        N = min(512, Ls)
        ntt = Ls // N
        jpt = N // 128
        src = seq_src(l, sq)
        with scope() as ph:
            G1b = Buf(sb("g1b", [128, D], F32, ph))
            K.dma(sp, [lambda: S.dma_start(out=G1b.ap[:], in_=MOD[l, sq["modrow"], 2 * D:3 * D].partition_broadcast(128))],
                  G1b, writes=[G1b])
            G2b, SH2b = make_mod_tiles(l, sq["modrow"], norm2_g[l], 4 * D, 3 * D, ph, "m2")
            gluw = Buf(sb("gluw", [128, 4, WM], BF16, ph))
            wbr2 = Buf(sb("wbr2", [128, 4, D], BF16, ph))
            wout = Buf(sb("wout", [128, 8, D], BF16, ph))
            glub = Buf(sb("glub", [128, 4], F32, ph))
            K.dma(pool, [lambda: G.dma_start(out=gluw.ap[:], in_=glu_w[l].rearrange("(kt p) n -> p kt n", p=128))], gluw, writes=[gluw])
            K.dma(pool, [lambda: G.dma_start(out=wbr2.ap[:], in_=w_branch[l, 2].rearrange("(kt p) n -> p kt n", p=128))], wbr2, writes=[wbr2])
            K.dma(pool, [lambda: G.dma_start(out=wout.ap[:], in_=w_out[l].rearrange("(kt p) n -> p kt n", p=128))], wout, writes=[wout])
            load_T(glub, glub.ap[:], [(0, 4, glu_b[l].rearrange("(m p) -> m p", p=128))], 4, 128)
            zr = mkring("zin", [128, 4, N], BF16, 2, ph)
            mabr = mkring("mabin", [128, 8, N], BF16, 2, ph)
            gcr = mkring("gcin", [128, 8, N], BF16, 2, ph)
            ycr = mkring("ycT", [128, 4, N], BF16, 2, ph)
            mgr = mkring("mgT", [128, 8, N], BF16, 2, ph)
            tf = mkring("tfb", [128, N], F32, 3, ph)
            tb = mkring("tbb", [128, N], BF16, 3, ph)
            hr = mkring("hin", [128, D], F32, 2, ph)
            hnr = mkring("hn", [128, D], F32, 2, ph)
            tmpb = Buf(sb("tmp2", [128, D], F32, ph))
            xnr = mkring("xn2", [128, D], BF16, 2, ph)
            ssr = mkring("ss2", [128, 2], F32, 2, ph)
            xstr = mkring("xst", [128, 8, N], BF16, 2, ph)
            for tt in range(ntt):
                ts = slice(tt * N, (tt + 1) * N)
                zt, mabt, gct = zr.next(), mabr.next(), gcr.next()
                K.dma(sp, [lambda: S.dma_start(out=zt.ap[:], in_=sq["YC"][:, ts].rearrange("(m p) t -> p m t", p=128))], zt, writes=[zt])
                K.dma(sp, [lambda: S.dma_start(out=mabt.ap[:], in_=sq["MAB"][:, ts].rearrange("(m p) t -> p m t", p=128))], mabt, writes=[mabt])
                K.dma(sp, [lambda: S.dma_start(out=gct.ap[:], in_=sq["GC"][:, ts].rearrange("(m p) t -> p m t", p=128))], gct, writes=[gct])
                yc = ycr.next()
                for mt in range(4):
                    pb = ps8.next()
                    mm_group(pb.ap[:, 0:N], [(gluw.ap[:, kt, mt * 128:(mt + 1) * 128], zt.ap[:, kt, :]) for kt in range(4)],
                             reads=[gluw, zt], pbuf=pb)
                    sg = tb.next()
                    K.do(act, lambda: A.activation(out=sg.ap[:], in_=pb.ap[:, 0:N], func=AF.Sigmoid, bias=glub.ap[:, mt:mt + 1]),
                         reads=[pb, glub], writes=[sg])
                    K.do(dve, lambda: V.tensor_tensor(out=yc.ap[:, mt, :], in0=zt.ap[:, mt, :], in1=sg.ap[:], op=ALU.mult),
                         reads=[zt, sg], **(dict(writes=[yc]) if mt == 0 else dict(pwrites=[yc])))
                mg = mgr.next()
                for dt_ in range(8):
                    pb = ps8.next()
                    mm_group(pb.ap[:, 0:N], [(wbr2.ap[:, kt, dt_ * 128:(dt_ + 1) * 128], yc.ap[:, kt, :]) for kt in range(4)],
                             reads=[wbr2, yc], pbuf=pb)
                    m = tf.next()
                    K.do(dve, lambda: V.tensor_tensor(out=m.ap[:], in0=gct.ap[:, dt_, :], in1=pb.ap[:, 0:N], op=ALU.mult),
                         reads=[gct, pb], writes=[m])
                    K.do(pool, lambda: G.tensor_tensor(out=mg.ap[:, dt_, :], in0=m.ap[:], in1=mabt.ap[:, dt_, :], op=ALU.add),
                         reads=[m, mabt], **(dict(writes=[mg]) if dt_ == 0 else dict(pwrites=[mg])))
                xst = xstr.next()
                for jj in range(jpt):
                    r0 = tt * N + jj * 128
                    ht = hr.next()
                    K.dma(sp, [lambda: S.dma_start(out=ht.ap[:], in_=src[r0:r0 + 128, :])], ht, writes=[ht])
                    hn = hnr.next()
                    for half in range(2):
                        pb = ps8.next()
                        mm_group(pb.ap[:, :], [(mg.ap[:, kt, jj * 128:(jj + 1) * 128], wout.ap[:, kt, half * 512:(half + 1) * 512])
                                               for kt in range(8)], reads=[mg, wout], pbuf=pb)
                        hs = slice(half * 512, (half + 1) * 512)
                        K.do(dve, lambda: V.tensor_tensor(out=hn.ap[:, hs], in0=pb.ap[:, :], in1=G1b.ap[:, hs], op=ALU.mult),
                             reads=[pb, G1b], **(dict(writes=[hn]) if half == 0 else dict(pwrites=[hn])))
                    K.do(dve, lambda: V.tensor_tensor(out=hn.ap[:], in0=hn.ap[:], in1=ht.ap[:], op=ALU.add),
                         reads=[ht], writes=[hn])
                    K.dma(pool, [lambda: G.dma_start(out=sq["HA"][r0:r0 + 128, :], in_=hn.ap[:])], hn, reads=[hn])
                    xn = xnr.next()
                    ss = ssr.next()
                    rms_mod(hn, G2b, SH2b, xn, tmpb, ss)
                    transpose_tile(xn, xst.ap[:, :, jj * 128:(jj + 1) * 128], xst, pw=(jj > 0))
                K.dma(pool, [lambda: G.dma_start(out=sq["XN2T"][:, ts].rearrange("(k p) t -> p k t", p=128), in_=xst.ap[:])],
                      xst, reads=[xst])

    def phase_moe(l, seq_list):
        last = (l == DEPTH - 1)
        blocks = []
        ctxs = [sq for sq in seq_list if sq["ctx"]]
        if ctxs and not last:
            blocks.append([(sq, 0, LC) for sq in ctxs])
        for sq in seq_list:
            if not sq["ctx"]:
                for tt in range(LS // 512):
                    blocks.append([(sq, tt * 512, 512)])
        with scope() as ph:
            rw = Buf(sb("rw", [128, 8, NE], BF16, ph))
            rbb = Buf(sb("rbb", [128, 4, NE], F32, ph))
            K.dma(pool, [lambda: G.dma_start(out=rw.ap[:], in_=router_w.rearrange("(kt p) e -> p kt e", p=128))], rw, writes=[rw])
            K.dma(sp, [lambda j=j: S.dma_start(out=rbb.ap[:, j, :], in_=router_b.partition_broadcast(128)) for j in range(4)],
                  rbb, writes=[rbb])
            g2c = Buf(sb("g2c", [128, 3, 8], F32, ph))
            for r in range(3):
                load_T(g2c, g2c.ap[:, r, :], [(0, 8, MOD[l, r, 5 * D:6 * D].rearrange("(k p) -> k p", p=128))], 8, 128,
                       partial=(r > 0))
            G3b = None
            if last:
                G3b = Buf(sb("g3b", [128, D], F32, ph))
                K.dma(sp, [lambda: S.dma_start(out=G3b.ap[:], in_=final_norm_g.partition_broadcast(128))], G3b, writes=[G3b])
            xTr = mkring("xT", [128, 8, 512], BF16, 2, ph)
            heT = sb("heT", [128, 64, 512], BF16, ph)
            B_he = Buf()
            WR = mkring("wstream", [128, 16384], BF16, 2, ph)
            moT = sb("moT", [128, 8, 512], F32, ph)
            B_mo = Buf()
            gbr = mkring("gb", [128, 512], BF16, 2, ph)
            sr = mkring("sl", [128, 512], BF16, 2, ph)
            t1r = mkring("t1", [128, 512], BF16, 2, ph)
            hr = mkring("hin2", [128, D], F32, 2, ph)
            hnr = mkring("hn2", [128, D], F32, 2, ph)
            tmpb = Buf(sb("tmp3", [128, D], F32, ph))
            ssr = mkring("ss3", [128, 2], F32, 2, ph)
            sc = Buf(sb("r_sc", [128, 4, NE], F32, ph))
            bb = Buf(sb("r_b", [128, 4, NE], F32, ph))
            b2 = Buf(sb("r_b2", [128, 4, NE], F32, ph))
            q1 = Buf(sb("r_q1", [128, 4, NE], F32, ph))
            m16 = Buf(sb("r_m16", [128, 16], F32, ph))
            m16b = Buf(sb("r_m16b", [128, 16], F32, ph))
            m4 = Buf(sb("r_m4", [128, 4], F32, ph))
            gts = Buf(sb("r_gts", [128, 4, NE], BF16, ph))
            gT = Buf(sb("r_gT", [16, 512], BF16, ph))

            def bl(ap_t, off, pstride, dims):
                return bass.AP(ap_t, off, [[pstride, 128]] + dims)

            for blk in blocks:
                xT = xTr.next()
                off = 0
                ems = []
                for (sq, t0, n) in blk:
                    ems.append(lambda sq=sq, t0=t0, n=n, off=off: S.dma_start(
                        out=xT.ap[:, :, off:off + n], in_=sq["XN2T"][:, t0:t0 + n].rearrange("(k p) t -> p k t", p=128)))
                    off += n
                K.dma(sp, ems, xT, writes=[xT])
                pr = ps8.next()
                for j in range(4):
                    mm_group(pr.ap[:, j * NE:(j + 1) * NE], [(xT.ap[:, kt, j * 128:(j + 1) * 128], rw.ap[:, kt, :]) for kt in range(8)],
                             reads=[xT, rw], pbuf=pr, pw=(j > 0))
                K.do(act, lambda: A.activation(out=sc.ap[:].rearrange("p j e -> p (j e)"), in_=pr.ap[:, 0:4 * NE], func=AF.Sigmoid),
                     reads=[pr], writes=[sc])

                def vv(fn, rd, wr):
                    K.do(dve, fn, reads=rd, writes=wr)
                flat = lambda t_: t_.ap[:].rearrange("p j e -> p (j e)")
                g44 = lambda t_: t_.ap[:].rearrange("p j (g e) -> p (j g) e", e=4)
                vv(lambda: V.tensor_tensor(out=flat(bb), in0=flat(sc), in1=flat(rbb), op=ALU.add), [sc, rbb], [bb])
                vv(lambda: V.tensor_reduce(out=m16.ap[:], in_=g44(bb), axis=AX.X, op=ALU.max), [bb], [m16])
                m16bc = bl(m16.ap, 0, 16, [[1, 16], [0, 4]])
                vv(lambda: V.tensor_tensor(out=g44(q1), in0=g44(bb), in1=m16bc, op=ALU.is_equal), [bb, m16], [q1])
                vv(lambda: V.scalar_tensor_tensor(out=flat(b2), in0=flat(q1), scalar=-BIG, in1=flat(bb), op0=ALU.mult, op1=ALU.add),
                   [q1, bb], [b2])
                vv(lambda: V.tensor_reduce(out=m16b.ap[:], in_=g44(b2), axis=AX.X, op=ALU.max), [b2], [m16b])
                vv(lambda: V.tensor_tensor(out=m16.ap[:], in0=m16.ap[:], in1=m16b.ap[:], op=ALU.add), [m16b], [m16])
                vv(lambda: V.tensor_reduce(out=m4.ap[:], in_=m16.ap[:].rearrange("p (j g) -> p j g", g=4), axis=AX.X, op=ALU.max),
                   [m16], [m4])
                m4bc = bl(m4.ap, 0, 4, [[1, 4], [0, 4]])
                vv(lambda: V.tensor_tensor(out=m16b.ap[:].rearrange("p (j g) -> p j g", g=4),
                                           in0=m16.ap[:].rearrange("p (j g) -> p j g", g=4), in1=m4bc, op=ALU.is_equal),
                   [m16, m4], [m16b])
                vv(lambda: V.tensor_scalar(m16b.ap[:], m16b.ap[:], -1.0, BIG, ALU.add, ALU.mult), [], [m16b])
                penbc = bl(m16b.ap, 0, 16, [[1, 16], [0, 4]])
                vv(lambda: V.tensor_tensor(out=g44(b2), in0=g44(bb), in1=penbc, op=ALU.add), [bb, m16b], [b2])
                vv(lambda: V.tensor_reduce(out=m4.ap[:], in_=b2.ap[:], axis=AX.X, op=ALU.max), [b2], [m4])
                m4e = bl(m4.ap, 0, 4, [[1, 4], [0, NE]])
                vv(lambda: V.tensor_tensor(out=q1.ap[:], in0=b2.ap[:], in1=m4e, op=ALU.is_equal), [b2, m4], [q1])
                vv(lambda: V.scalar_tensor_tensor(out=flat(b2), in0=flat(q1), scalar=-BIG, in1=flat(b2), op0=ALU.mult, op1=ALU.add),
                   [q1], [b2])
                vv(lambda: V.tensor_reduce(out=m4.ap[:], in_=b2.ap[:], axis=AX.X, op=ALU.max), [b2], [m4])
                vv(lambda: V.tensor_tensor(out=bb.ap[:], in0=b2.ap[:], in1=m4e, op=ALU.is_equal), [b2, m4], [bb])
                vv(lambda: V.tensor_tensor(out=flat(q1), in0=flat(q1), in1=flat(bb), op=ALU.add), [bb], [q1])
                vv(lambda: V.tensor_tensor(out=flat(q1), in0=flat(q1), in1=flat(sc), op=ALU.mult), [sc], [q1])
                vv(lambda: V.tensor_reduce(out=m4.ap[:], in_=q1.ap[:], axis=AX.X, op=ALU.add), [q1], [m4])
                vv(lambda: V.reciprocal(m4.ap[:], m4.ap[:]), [], [m4])
                vv(lambda: V.tensor_tensor(out=gts.ap[:], in0=q1.ap[:], in1=m4e, op=ALU.mult), [q1, m4], [gts])
                if "GATES" in debug:
                    pass
                pg = ps8.next()
                pgv = pg.ap[:].bitcast(BF16)
                K.do(pe, lambda: [T.transpose(pgv[0:NE, j * 128:(j + 1) * 128], gts.ap[:, j, :], ident_bf[:])
                                  for j in range(4)][-1], reads=[gts, B_const], writes=[pg])
                K.do(act, lambda: A.copy(gT.ap[:], pgv[0:NE, 0:512]), reads=[pg], writes=[gT])
                for e in range(NE):
                    ws = WR.next()
                    after = []
                    K.dma(sp, [lambda: S.dma_start(out=ws.ap[:, 0:4096].rearrange("p (k f) -> p k f", k=8),
                                                   in_=WG[l, e].rearrange("(k p) f -> p k f", p=128)),
                               lambda: S.dma_start(out=ws.ap[:, 4096:8192].rearrange("p (k f) -> p k f", k=8),
                                                   in_=WU[l, e].rearrange("(k p) f -> p k f", p=128))],
                          ws, reads=[B_expw[l]], writes=[ws])
                    wgv = ws.ap[:, 0:4096].rearrange("p (k f) -> p k f", k=8)
                    wuv = ws.ap[:, 4096:8192].rearrange("p (k f) -> p k f", k=8)
                    pbc = ps8.next()
                    mm_group(pbc.ap[:, :], [(onesel[:, e, :], gT.ap[:, :])], reads=[gT, B_const], pbuf=pbc)
                    gb = gbr.next()
                    K.do(act, lambda: A.copy(gb.ap[:], pbc.ap[:, :]), reads=[pbc], writes=[gb])
                    for ft in range(4):
                        pgt, put = ps8.next(), ps8.next()
                        mm_group(pgt.ap[:, :], [(wgv[:, kt, ft * 128:(ft + 1) * 128], xT.ap[:, kt, :]) for kt in range(8)],
                                 reads=[ws, xT], pbuf=pgt)
                        mm_group(put.ap[:, :], [(wuv[:, kt, ft * 128:(ft + 1) * 128], xT.ap[:, kt, :]) for kt in range(8)],
                                 reads=[ws, xT], pbuf=put)
                        sl = sr.next()
                        K.do(act, lambda: A.activation(out=sl.ap[:], in_=pgt.ap[:, :], func=AF.Silu), reads=[pgt], writes=[sl])
                        t1 = t1r.next()
                        K.do(dve, lambda: V.tensor_tensor(out=t1.ap[:], in0=sl.ap[:], in1=put.ap[:, :], op=ALU.mult),
                             reads=[sl, put], writes=[t1])
                        first = (e == 0 and ft == 0)
                        K.do(pool, lambda: G.tensor_tensor(out=heT[:, e * 4 + ft, :], in0=t1.ap[:], in1=gb.ap[:], op=ALU.mult),
                             reads=[t1, gb], **(dict(writes=[B_he]) if first else dict(pwrites=[B_he])))
                for dt2 in range(4):
                    ws = WR.next()
                    wdv = ws.ap[:].rearrange("p (i c) -> p i c", c=256)
                    K.dma(sp, [lambda: S.dma_start(out=wdv, in_=WD[l][:, dt2 * 256:(dt2 + 1) * 256].rearrange("(i p) c -> p i c", p=128))],
                          ws, reads=[B_expw[l]], writes=[ws])
                    for dtl in range(2):
                        dt_ = dt2 * 2 + dtl
                        po = ps8.next()
                        mm_group(po.ap[:, :], [(wdv[:, i, dtl * 128:(dtl + 1) * 128], heT[:, i, :]) for i in range(64)],
                                 reads=[ws, B_he], pbuf=po)
                        mr = blk[0][0]["modrow"]
                        if len(blk) == 1:
                            K.do(act, lambda: A.activation(out=moT[:, dt_, :], in_=po.ap[:, :], func=AF.Copy,
                                                           scale=g2c.ap[:, mr, dt_:dt_ + 1]),
                                 reads=[po, g2c], **(dict(writes=[B_mo]) if dt_ == 0 else dict(pwrites=[B_mo])))
                        else:
                            o2 = 0
                            for pi, (sq, t0, n) in enumerate(blk):
                                K.do(act, lambda: A.activation(out=moT[:, dt_, o2:o2 + n], in_=po.ap[:, o2:o2 + n], func=AF.Copy,
                                                               scale=g2c.ap[:, sq["modrow"], dt_:dt_ + 1]),
                                     reads=[po, g2c], **(dict(writes=[B_mo]) if (dt_ == 0 and pi == 0) else dict(pwrites=[B_mo])))
                                o2 += n
                off = 0
                for (sq, t0, n) in blk:
                    for jj in range(n // 128):
                        c0 = off + jj * 128
                        r0 = t0 + jj * 128
                        ht = hr.next()
                        K.dma(sp, [lambda: S.dma_start(out=ht.ap[:], in_=sq["HA"][r0:r0 + 128, :])], ht, writes=[ht])
                        hn = hnr.next()
                        for half in range(2):
                            pt = ps8.next()
                            K.do(pe, lambda: [T.transpose(pt.ap[:, k * 128:(k + 1) * 128], moT[:, half * 4 + k, c0:c0 + 128], ident_f[:])
                                              for k in range(4)][-1], reads=[B_mo, B_const], writes=[pt])
                            hs = slice(half * 512, (half + 1) * 512)
                            K.do(dve, lambda: V.tensor_tensor(out=hn.ap[:, hs], in0=pt.ap[:, :], in1=ht.ap[:, hs], op=ALU.add),
                                 reads=[pt, ht], **(dict(writes=[hn]) if half == 0 else dict(pwrites=[hn])))
                        if not last:
                            K.dma(pool, [lambda: G.dma_start(out=sq["HB"][r0:r0 + 128, :], in_=hn.ap[:])], hn, reads=[hn])
                        else:
                            ss = ssr.next()
                            rms_mod(hn, G3b, None, None, tmpb, ss)
                            K.dma(pool, [lambda: G.dma_start(out=out_d[sq["b"], r0:r0 + 128, :], in_=tmpb.ap[:])], tmpb, reads=[tmpb])
                    off += n

    DBG = {}
    debug_seq = 2
    if "XNT" in debug:
        DBG["XNT"] = nc.dram_tensor("XNT", [D, LS], BF16, kind="ExternalOutput").ap()

    def stop(name):
        return stop_after == name

    def finish():
        K.barrier()
        es.close()
        return nc

    init_consts()
    seq_list = [SEQ[s] for s in seqs]
    for l in range(nlayers):
        convert_experts(l)
    for l in range(nlayers):
        phase_adaln(l)
    if stop("adaln"):
        return finish()
    for l in range(nlayers):
        phase_s5_tables(l)
    if stop("tables"):
        return finish()
    for l in range(nlayers):
        last = (l == DEPTH - 1)
        for sq in seq_list:
            phase_front(l, sq, only_cu=(last and sq["ctx"]))
        if stop(f"front{l}"):
            return finish()
        for sq in seq_list:
            phase_s5(l, sq)
        if stop(f"s5{l}"):
            return finish()
        for sq in seq_list:
            if not (last and sq["ctx"]):
                phase_back(l, sq)
        if stop(f"back{l}"):
            return finish()
        phase_moe(l, seq_list)
        if stop(f"moe{l}"):
            return finish()
    return finish()


_PROG = {}


def _inputs_for_core(inputs, c):
    m = {}
    b0 = 2 * c
    f = lambda a: np.ascontiguousarray(np.asarray(a, dtype=np.float32))
    m["x"] = f(inputs["x"][b0:b0 + 2])
    m["ctx"] = f(inputs["ctx"][b0:b0 + 2])
    m["cvec"] = f(np.stack([inputs["c"][b0], inputs["c"][b0 + 1], inputs["c_ctx"]], axis=0))
    for k in ("w_mod", "b_mod", "norm1_g", "norm2_g", "w_in", "sgu_norm_g", "sgu_w", "sgu_b", "conv_w",
              "s5_lam_re", "s5_lam_im", "s5_log_dt", "s5_b_re", "s5_b_im", "s5_c_re", "s5_c_im", "s5_d",
              "glu_w", "glu_b", "w_branch", "w_out", "router_w", "router_b", "exp_w_gate", "exp_w_up",
              "exp_w_down", "final_norm_g"):
        m[k] = f(inputs[k])
    return m


def kernel(**inputs):
    if "nc" not in _PROG:
        _PROG["nc"] = build_program()
    nc = _PROG["nc"]
    shared = _inputs_for_core(inputs, 0)
    in_maps = []
    for c in range(NCORES):
        m = dict(shared)
        b0 = 2 * c
        m["x"] = np.ascontiguousarray(np.asarray(inputs["x"][b0:b0 + 2], dtype=np.float32))
        m["ctx"] = np.ascontiguousarray(np.asarray(inputs["ctx"][b0:b0 + 2], dtype=np.float32))
        m["cvec"] = np.ascontiguousarray(np.stack([inputs["c"][b0], inputs["c"][b0 + 1], inputs["c_ctx"]], axis=0).astype(np.float32))
        in_maps.append(m)
    res = run_bass_kernel_spmd(nc, in_maps, core_ids=list(range(NCORES)))
    out = np.concatenate([np.asarray(r["out"]) for r in res.results], axis=0)
    return out.astype(np.float32)
```

```python
import math
import numpy as np
from contextlib import ExitStack, contextmanager
import concourse.bass as bass
import concourse.mybir as mybir
from concourse.bass_utils import run_bass_kernel_spmd

F32 = mybir.dt.float32
BF16 = mybir.dt.bfloat16
I32 = mybir.dt.int32
AF = mybir.ActivationFunctionType
ALU = mybir.AluOpType
AX = mybir.AxisListType

D = 1024
LS = 2048
LC = 256
DEPTH = 2
DIN = 6144
WM = 512
NE = 16
DFF = 512
NCORES = 8
EPS = 1e-6
BIG = 1.0e4
TWO_PI = 2.0 * math.pi


class Eng:
    def __init__(self, nc, e, name):
        self.e = e
        self.sem = nc.alloc_semaphore(name)
        self.n = 0
        self.seen = {}

    def wait(self, evs):
        for sem, val in evs.items():
            if self.seen.get(sem, 0) >= val:
                continue
            self.e.wait_ge(sem, val)
            self.seen[sem] = val

    def sig(self, ins):
        self.n += 1
        ins.then_inc(self.sem, 1)
        return (self.sem, self.n)


class Buf:
    def __init__(self, ap=None):
        self.ap = ap
        self.w = {}
        self.r = {}
        self.dsem = None
        self.slot = None
        self.dv = 0


def _merge(d, ev):
    s, v = ev
    if d.get(s, 0) < v:
        d[s] = v


class Ring:
    def __init__(self, bufs):
        self.bufs = bufs
        self.i = 0

    def next(self):
        b = self.bufs[self.i]
        self.i = (self.i + 1) % len(self.bufs)
        return b


class Ker:
    def __init__(self, nc):
        self.nc = nc
        self.pe = Eng(nc, nc.tensor, "s_pe")
        self.act = Eng(nc, nc.scalar, "s_act")
        self.dve = Eng(nc, nc.vector, "s_dve")
        self.pool = Eng(nc, nc.gpsimd, "s_pool")
        self.sp = Eng(nc, nc.sync, "s_sp")
        self.engs = [self.pe, self.act, self.dve, self.pool, self.sp]
        self.dbufs = []
        self.uid = 0
        self.free_slots = []
        self.scope_bufs = [[]]

    def _pre(self, eng, reads, writes, after):
        evs = {}
        for b in reads:
            for s, v in b.w.items():
                _merge(evs, (s, v))
        for b in writes:
            for s, v in b.w.items():
                _merge(evs, (s, v))
            for s, v in b.r.items():
                _merge(evs, (s, v))
        for ev in after:
            _merge(evs, ev)
        eng.wait(evs)

    def _post(self, ev, reads, writes, pwrites):
        for b in reads:
            _merge(b.r, ev)
        for b in writes:
            b.w = {ev[0]: ev[1]}
            b.r = {}
        for b in pwrites:
            _merge(b.w, ev)

    def do(self, eng, emit, reads=(), writes=(), pwrites=(), after=()):
        self._pre(eng, reads, writes, after)
        ins = emit()
        ev = eng.sig(ins)
        self._post(ev, reads, writes, pwrites)
        return ev

    def dma(self, q, emits, owner, reads=(), writes=(), pwrites=(), after=()):
        self._pre(q, reads, writes, after)
        if owner.dsem is None:
            if self.free_slots:
                owner.slot = self.free_slots.pop()
            else:
                self.uid += 1
                owner.slot = [self.nc.alloc_semaphore(f"dq{self.uid}"), 0]
            owner.dsem = owner.slot[0]
            owner.dv = owner.slot[1]
            self.dbufs.append(owner)
            self.scope_bufs[-1].append(owner)
        for em in emits:
            ins = em()
            ins.then_inc(owner.dsem, 16)
            owner.dv += 16
        owner.slot[1] = owner.dv
        ev = (owner.dsem, owner.dv)
        self._post(ev, reads, writes, pwrites)
        return ev

    def barrier(self):
        evs = {}
        for e in self.engs:
            if e.n > 0:
                evs[e.sem] = e.n
        for b in self.dbufs:
            if b.dv > 0:
                evs[b.dsem] = b.dv
        for e in self.engs:
            e.wait(evs)

    def open_scope(self):
        self.scope_bufs.append([])

    def close_scope(self):
        for b in self.scope_bufs.pop():
            self.free_slots.append(b.slot)
            self.dbufs.remove(b)
            b.dsem = None
            b.slot = None


def build_program(debug=(), seqs=(0, 1, 2, 3), stop_after=None, nlayers=DEPTH):
    debug = set(debug)
    nc = bass.Bass("TRN2", target_bir_lowering=False)
    K = Ker(nc)
    es = ExitStack()

    def dram_in(name, shape):
        return nc.dram_tensor(name, list(shape), F32, kind="ExternalInput").ap()

    def dram_scr(name, shape, dt):
        if name in debug:
            return nc.dram_tensor(name, list(shape), dt, kind="ExternalOutput").ap()
        return nc.dram_tensor(name, list(shape), dt).ap()

    x_in = dram_in("x", [2, LS, D])
    ctx_in = dram_in("ctx", [2, LC, D])
    cvec = dram_in("cvec", [3, D])
    w_mod = dram_in("w_mod", [DEPTH, D, 6 * D])
    b_mod = dram_in("b_mod", [DEPTH, 6 * D])
    norm1_g = dram_in("norm1_g", [DEPTH, D])
    norm2_g = dram_in("norm2_g", [DEPTH, D])
    w_in = dram_in("w_in", [DEPTH, D, DIN])
    sgu_norm_g = dram_in("sgu_norm_g", [DEPTH, WM])
    sgu_w = dram_in("sgu_w", [DEPTH, 4, 128, 128])
    sgu_b = dram_in("sgu_b", [DEPTH, 4, 128])
    conv_w = dram_in("conv_w", [DEPTH, 3, WM])
    s5_lam_re = dram_in("s5_lam_re", [DEPTH, 2, 32, 64])
    s5_lam_im = dram_in("s5_lam_im", [DEPTH, 2, 32, 64])
    s5_log_dt = dram_in("s5_log_dt", [DEPTH, 2, 32])
    s5_b_re = dram_in("s5_b_re", [DEPTH, 2, 32, 64, 16])
    s5_b_im = dram_in("s5_b_im", [DEPTH, 2, 32, 64, 16])
    s5_c_re = dram_in("s5_c_re", [DEPTH, 2, 32, 16, 64])
    s5_c_im = dram_in("s5_c_im", [DEPTH, 2, 32, 16, 64])
    s5_d = dram_in("s5_d", [DEPTH, WM])
    glu_w = dram_in("glu_w", [DEPTH, WM, WM])
    glu_b = dram_in("glu_b", [DEPTH, WM])
    w_branch = dram_in("w_branch", [DEPTH, 3, WM, D])
    w_out = dram_in("w_out", [DEPTH, D, D])
    router_w = dram_in("router_w", [D, NE])
    router_b = dram_in("router_b", [NE])
    exp_w_gate = dram_in("exp_w_gate", [DEPTH, NE, D, DFF])
    exp_w_up = dram_in("exp_w_up", [DEPTH, NE, D, DFF])
    exp_w_down = dram_in("exp_w_down", [DEPTH, NE, DFF, D])
    final_norm_g = dram_in("final_norm_g", [D])
    out_d = nc.dram_tensor("out", [2, LS, D], F32, kind="ExternalOutput").ap()

    MOD = dram_scr("MOD", [DEPTH, 3, 6 * D], F32)
    SEQ = []
    for s in range(4):
        isctx = s < 2
        b = s % 2
        Ls = LC if isctx else LS
        SEQ.append(dict(
            s=s, b=b, ctx=isctx, L=Ls, modrow=(2 if isctx else b),
            src=(ctx_in[b] if isctx else x_in[b]),
            CU=dram_scr(f"CU{s}", [WM, Ls], BF16),
            YC=dram_scr(f"YC{s}", [WM, Ls], BF16),
            MAB=dram_scr(f"MAB{s}", [D, Ls], BF16),
            GC=dram_scr(f"GC{s}", [D, Ls], BF16),
            XN2T=dram_scr(f"XN2T{s}", [D, Ls], BF16),
            HA=dram_scr(f"HA{s}", [Ls, D], F32),
            HB=dram_scr(f"HB{s}", [Ls, D], F32),
        ))
    WG = dram_scr("WG", [DEPTH, NE, D, DFF], BF16)
    WU = dram_scr("WU", [DEPTH, NE, D, DFF], BF16)
    WD = dram_scr("WD", [DEPTH, NE * DFF, D], BF16)
    W1D = dram_scr("W1D", [DEPTH, 2, 128, 32, 128], BF16)
    W2D = dram_scr("W2D", [DEPTH, 2, 64, 32, 2, 128], BF16)
    KMD = dram_scr("KMD", [DEPTH, 128, 32, 128], BF16)
    SELD = dram_scr("SELD", [128, 8, 8, 128], BF16)
    SELTD = dram_scr("SELTD", [128, 8, 8, 128], BF16)

    def sb(name, shape, dt, stack=None):
        K.uid += 1
        return (stack or es).enter_context(nc.sbuf_tensor(f"{name}_{K.uid}", list(shape), dt))

    @contextmanager
    def scope():
        st = ExitStack()
        K.open_scope()
        try:
            yield st
        finally:
            K.barrier()
            K.close_scope()
            st.close()

    def mkring(name, shape, dt, n, stack):
        return Ring([Buf(sb(f"{name}{i}", shape, dt, stack)) for i in range(n)])

    ident_bf = sb("ident_bf", [128, 128], BF16)
    ident_f = sb("ident_f", [128, 128], F32)
    neg_half = sb("neg_half", [128, 1], F32)
    onesel = sb("onesel", [16, NE, 128], BF16)
    a8_ar = sb("a8_ar", [64, DEPTH * 2, 2, 32], F32)
    a8_ai = sb("a8_ai", [64, DEPTH * 2, 2, 32], F32)
    h0st = sb("h0st", [64, 4, 2, 32], F32)
    B_const = Buf()
    B_a8 = Buf()
    B_h0 = Buf()

    ps_t = [es.enter_context(nc.psum_tensor(f"ps{i}", [128, 512], F32)) for i in range(8)]
    PS = [Buf(ps_t[i]) for i in range(8)]
    ps8 = Ring(PS)
    psA = Ring(PS[0:4])
    psB = Ring(PS[4:8])

    pe, act, dve, pool, sp = K.pe, K.act, K.dve, K.pool, K.sp
    V, A, T, G, S = nc.vector, nc.scalar, nc.tensor, nc.gpsimd, nc.sync

    def mm_group(out_ap, pairs, reads, pbuf, pw=False):
        def emit():
            n = len(pairs)
            ins = None
            for i, (lh, rh) in enumerate(pairs):
                ins = T.matmul(out_ap, lh, rh, start=(i == 0), stop=(i == n - 1))
            return ins
        if pw:
            return K.do(pe, emit, reads=reads, pwrites=[pbuf])
        return K.do(pe, emit, reads=reads, writes=[pbuf])

    def load_T(dst, dst_ap, srcs, F, P, partial=False):
        with scope() as tmp:
            stg = Buf(sb("ldT", [F, P], F32, tmp))
            K.dma(sp, [lambda r0=r0, n=n, a=a: S.dma_start(out=stg.ap[r0:r0 + n, :], in_=a) for (r0, n, a) in srcs],
                  stg, writes=[stg])
            pb = ps8.next()
            K.do(pe, lambda: T.transpose(pb.ap[0:P, 0:F], stg.ap[:], ident_f[0:F, 0:F]), reads=[stg, B_const], writes=[pb])
            kw = dict(pwrites=[dst]) if partial else dict(writes=[dst])
            K.do(act, lambda: A.copy(dst_ap, pb.ap[0:P, 0:F]), reads=[pb], **kw)

    def init_consts():
        def emit():
            G.memset(ident_bf[:], 0.0)
            G.affine_select(out=ident_bf[:], in_=ident_bf[:], compare_op=ALU.not_equal, fill=1.0,
                            base=0, pattern=[[-1, 128]], channel_multiplier=1)
            G.memset(ident_f[:], 0.0)
            G.affine_select(out=ident_f[:], in_=ident_f[:], compare_op=ALU.not_equal, fill=1.0,
                            base=0, pattern=[[-1, 128]], channel_multiplier=1)
            G.memset(neg_half[:], -0.5)
            G.memset(onesel[:], 0.0)
            G.memset(h0st[:], 0.0)
            return G.affine_select(out=onesel[:], in_=onesel[:], compare_op=ALU.not_equal, fill=1.0,
                                   base=0, pattern=[[-1, NE], [0, 128]], channel_multiplier=1)
        K.do(pool, emit, writes=[B_const, B_h0])
        with scope() as ph:
            selt = sb("selt", [128, 8, 8, 128], BF16, ph)
            Bs = Buf()

            def emit_sel(transposed):
                def f():
                    ins = G.memset(selt[:], 0.0)
                    idv = ident_bf[:].rearrange("p (a b) -> p a b", b=16)
                    for j in range(8):
                        if not transposed:
                            ins = G.tensor_copy(selt[:, :, j, 16 * j:16 * j + 16], idv)
                        else:
                            ins = G.tensor_copy(selt[:, j, :, 16 * j:16 * j + 16], idv)
                    return ins
                return f
            K.do(pool, emit_sel(False), reads=[B_const], writes=[Bs])
            K.dma(sp, [lambda: S.dma_start(out=SELD, in_=selt[:])], Bs, reads=[Bs])
            K.do(pool, emit_sel(True), reads=[B_const], writes=[Bs])
            K.dma(sp, [lambda: S.dma_start(out=SELTD, in_=selt[:])], Bs, reads=[Bs])

    B_expw = [Buf() for _ in range(DEPTH)]

    def convert_experts(l):
        ems = []
        for e in range(NE):
            ems.append(lambda e=e: G.dma_start(out=WG[l, e].rearrange("(r a) f -> r (a f)", r=64), in_=exp_w_gate[l, e].rearrange("(r a) f -> r (a f)", r=64)))
            ems.append(lambda e=e: G.dma_start(out=WU[l, e].rearrange("(r a) f -> r (a f)", r=64), in_=exp_w_up[l, e].rearrange("(r a) f -> r (a f)", r=64)))
            ems.append(lambda e=e: G.dma_start(out=WD[l, e * DFF:(e + 1) * DFF, :].rearrange("(r a) f -> r (a f)", r=64), in_=exp_w_down[l, e].rearrange("(r a) f -> r (a f)", r=64)))
        K.dma(pool, ems, B_expw[l], pwrites=[B_expw[l]])

    def phase_adaln(l):
        with scope() as ph:
            csb = Buf(sb("csb", [3, D], F32, ph))
            scT = Buf(sb("scT", [128, 8, 3], F32, ph))
            bm = Buf(sb("bm", [3, 6 * D], F32, ph))
            modsb = Buf(sb("modsb", [3, 6 * D], F32, ph))
            wmr = mkring("wm", [128, 8, 512], F32, 2, ph)
            K.dma(sp, [lambda: S.dma_start(out=csb.ap[:], in_=cvec)], csb, writes=[csb])
            K.dma(sp, [lambda: S.dma_start(out=bm.ap[:], in_=b_mod[l].partition_broadcast(3))], bm, writes=[bm])
            pb = ps8.next()
            K.do(pe, lambda: [T.transpose(pb.ap[:, kt * 3:kt * 3 + 3], csb.ap[:, kt * 128:(kt + 1) * 128],
                                          ident_f[0:3, 0:3]) for kt in range(8)][-1],
                 reads=[csb, B_const], writes=[pb])
            K.do(act, lambda: A.activation(out=scT.ap[:], in_=pb.ap[:, 0:24].rearrange("p (k j) -> p k j", k=8),
                                           func=AF.Silu), reads=[pb], writes=[scT])
            for cc in range(12):
                wm = wmr.next()
                K.dma(sp, [lambda: S.dma_start(out=wm.ap[:], in_=w_mod[l][:, cc * 512:(cc + 1) * 512]
                                               .rearrange("(kt p) n -> p kt n", p=128))], wm, writes=[wm])
                pb = ps8.next()
                mm_group(pb.ap[0:3, :], [(scT.ap[:, kt, :], wm.ap[:, kt, :]) for kt in range(8)],
                         reads=[scT, wm], pbuf=pb)
                K.do(dve, lambda: V.tensor_tensor(out=modsb.ap[:, cc * 512:(cc + 1) * 512], in0=pb.ap[0:3, :],
                                                  in1=bm.ap[:, cc * 512:(cc + 1) * 512], op=ALU.add),
                     reads=[pb, bm], pwrites=[modsb])
            K.dma(sp, [lambda: S.dma_start(out=MOD[l], in_=modsb.ap[:])], modsb, reads=[modsb])

    def make_mod_tiles(l, modrow, g_row, sc_off, sh_off, ph, name):
        gt = Buf(sb(name + "_g", [128, D], F32, ph))
        ht = Buf(sb(name + "_h", [128, D], F32, ph))
        with scope() as tmp:
            st = Buf(sb(name + "_s", [128, D], F32, tmp))
            K.dma(sp, [lambda: S.dma_start(out=gt.ap[:], in_=g_row.partition_broadcast(128))], gt, writes=[gt])
            K.dma(sp, [lambda: S.dma_start(out=st.ap[:], in_=MOD[l, modrow, sc_off:sc_off + D].partition_broadcast(128))],
                  st, writes=[st])
            K.dma(sp, [lambda: S.dma_start(out=ht.ap[:], in_=MOD[l, modrow, sh_off:sh_off + D].partition_broadcast(128))],
                  ht, writes=[ht])
            K.do(dve, lambda: V.scalar_tensor_tensor(out=gt.ap[:], in0=st.ap[:], scalar=1.0, in1=gt.ap[:],
                                                     op0=ALU.add, op1=ALU.mult), reads=[st], writes=[gt])
        return gt, ht

    def rstd_from_sumsq(ss):
        K.do(dve, lambda: V.tensor_scalar(ss.ap[:, 0:1], ss.ap[:, 0:1], 1.0 / D, EPS, ALU.mult, ALU.add), writes=[ss])
        K.do(pool, lambda: G.tensor_tensor(ss.ap[:, 1:2], ss.ap[:, 0:1], neg_half[:], ALU.pow),
             reads=[B_const], writes=[ss])

    def rms_mod(xt, Gb, SHb, xn, tmp, ss):
        K.do(dve, lambda: V.tensor_tensor(out=tmp.ap[:], in0=xt.ap[:], in1=xt.ap[:], op=ALU.mult), reads=[xt], writes=[tmp])
        K.do(dve, lambda: V.tensor_reduce(out=ss.ap[:, 0:1], in_=tmp.ap[:], axis=AX.X, op=ALU.add), reads=[tmp], writes=[ss])
        rstd_from_sumsq(ss)
        K.do(dve, lambda: V.scalar_tensor_tensor(out=tmp.ap[:], in0=xt.ap[:], scalar=ss.ap[:, 1:2], in1=Gb.ap[:],
                                                 op0=ALU.mult, op1=ALU.mult), reads=[xt, ss, Gb], writes=[tmp])
        if SHb is None:
            return
        K.do(dve, lambda: V.tensor_tensor(out=xn.ap[:], in0=tmp.ap[:], in1=SHb.ap[:], op=ALU.add),
             reads=[tmp, SHb], writes=[xn])

    def transpose_tile(xn, dest_ap3, dest_buf, pw=False):
        pb = ps8.next()
        pv = pb.ap[:].bitcast(BF16)
        K.do(pe, lambda: [T.transpose(pv[:, kt * 128:(kt + 1) * 128], xn.ap[:, kt * 128:(kt + 1) * 128], ident_bf[:])
                          for kt in range(8)][-1], reads=[xn, B_const], writes=[pb])
        kw = dict(pwrites=[dest_buf]) if pw else dict(writes=[dest_buf])
        K.do(act, lambda: A.copy(dest_ap3, pv.rearrange("p (k t) -> p k t", k=8)), reads=[pb], **kw)

    def seq_src(l, sq):
        return sq["src"] if l == 0 else sq["HB"]

    def phase_front(l, sq, only_cu=False):
        Ls = sq["L"]
        N = min(512, Ls)
        ntt = Ls // N
        nj = Ls // 128
        jpt = N // 128
        grid = not sq["ctx"]
        with scope() as ph:
            xnT = sb("xnT", [128, 8, Ls], BF16, ph)
            B_xn = [Buf() for _ in range(nj)]
            with scope() as p1:
                Gb, SHb = make_mod_tiles(l, sq["modrow"], norm1_g[l], D, 0, p1, "m1")
                xring = mkring("xin", [128, D], F32, 2, p1)
                tmpb = Buf(sb("tmp1", [128, D], F32, p1))
                xnr = mkring("xnr", [128, D], BF16, 2, p1)
                ssr = mkring("ss", [128, 2], F32, 2, p1)
                src = seq_src(l, sq)
                for j in range(nj):
                    xt = xring.next()
                    K.dma(sp, [lambda: S.dma_start(out=xt.ap[:], in_=src[j * 128:(j + 1) * 128, :])], xt, writes=[xt])
                    xn = xnr.next()
                    ss = ssr.next()
                    rms_mod(xt, Gb, SHb, xn, tmpb, ss)
                    transpose_tile(xn, xnT[:, :, j * 128:(j + 1) * 128], B_xn[j])
            if "XNT" in debug and sq["s"] == debug_seq:
                K.dma(sp, [lambda: S.dma_start(out=DBG["XNT"].rearrange("(kt p) t -> p kt t", p=128), in_=xnT[:])],
                      Buf(), reads=B_xn)

            wring = mkring("wblk", [128, 8, 512], BF16, 4, ph)
            tf = mkring("tf", [128, N], F32, 6, ph)
            tb = mkring("tb", [128, N], BF16, 8, ph)

            def load_wblk(cb):
                wb = wring.next()
                K.dma(pool, [lambda: G.dma_start(out=wb.ap[:], in_=w_in[l][:, cb * 512:(cb + 1) * 512]
                                                 .rearrange("(kt p) n -> p kt n", p=128))], wb, writes=[wb])
                return wb

            def proj_fm(pb, wb, mt, tt):
                mm_group(pb.ap[:, 0:N], [(wb.ap[:, kt, mt * 128:(mt + 1) * 128], xnT[:, kt, tt * N:(tt + 1) * N])
                                         for kt in range(8)],
                         reads=[wb] + B_xn[tt * jpt:(tt + 1) * jpt], pbuf=pb)

            def store(dst, t):
                K.dma(pool, [lambda: G.dma_start(out=dst, in_=t.ap[:])], t, reads=[t])

            wb = load_wblk(5)
            for mt in range(4):
                for tt in range(ntt):
                    pb = ps8.next()
                    proj_fm(pb, wb, mt, tt)
                    cs = tb.next()
                    K.do(act, lambda: A.copy(cs.ap[:], pb.ap[:, 0:N]), reads=[pb], writes=[cs])
                    store(sq["CU"][mt * 128:(mt + 1) * 128, tt * N:(tt + 1) * N], cs)
            if only_cu:
                return

            uT = sb("uT", [128, 4, Ls], BF16, ph)
            B_u = [[Buf() for _ in range(ntt)] for _ in range(4)]
            wb = load_wblk(0)
            for mt in range(4):
                for tt in range(ntt):
                    pb = ps8.next()
                    proj_fm(pb, wb, mt, tt)
                    K.do(act, lambda: A.activation(out=uT[:, mt, tt * N:(tt + 1) * N], in_=pb.ap[:, 0:N],
                                                   func=AF.Gelu_apprx_tanh), reads=[pb], writes=[B_u[mt][tt]])

            wsq = Buf(sb("wsq", [128, 4, 128], BF16, ph))
            wsT = Buf(sb("wsT", [128, 4, 128], BF16, ph))
            sguc = Buf(sb("sguc", [128, 4], F32, ph))
            bsrow = Buf(sb("bsrow", [128, 4, 128], F32, ph))
            K.dma(pool, [lambda: G.dma_start(out=wsq.ap[:], in_=sgu_w[l].rearrange("h q k -> q h k"))], wsq, writes=[wsq])
            load_T(sguc, sguc.ap[:], [(0, 4, sgu_norm_g[l].rearrange("(h p) -> h p", p=128))], 4, 128)
            K.dma(sp, [lambda: S.dma_start(out=bsrow.ap[:].rearrange("p h q -> p (h q)"),
                                           in_=sgu_b[l].rearrange("h q -> (h q)").partition_broadcast(128))],
                  bsrow, writes=[bsrow])
            pb = ps8.next()
            pv = pb.ap[:].bitcast(BF16)
            K.do(pe, lambda: [T.transpose(pv[:, h * 128:(h + 1) * 128], wsq.ap[:, h, :], ident_bf[:])
                              for h in range(4)][-1], reads=[wsq, B_const], writes=[pb])
            K.do(act, lambda: A.copy(wsT.ap[:], pv[:, 0:512].rearrange("p (h q) -> p h q", h=4)),
                 reads=[pb], writes=[wsT])
            yaT = sb("yaT", [128, 4, Ls], BF16, ph)
            B_ya = [Buf() for _ in range(ntt)]
            gvr = mkring("gv", [128, 512], F32, 2, ph)
            vnr = mkring("vn", [128, 512], BF16, 2, ph)
            bnr = mkring("bnst", [128, 8], F32, 2, ph)
            wb = load_wblk(1)
            mixps = None
            for j in range(nj):
                tt, jj = divmod(j, jpt)
                if jj == 0:
                    mixps = [psB.next() for _ in range(4)]
                pb = psA.next()
                mm_group(pb.ap[:, :], [(xnT[:, kt, j * 128:(j + 1) * 128], wb.ap[:, kt, :]) for kt in range(8)],
                         reads=[wb, B_xn[j]], pbuf=pb)
                gv = gvr.next()
                K.do(act, lambda: A.activation(out=gv.ap[:], in_=pb.ap[:], func=AF.Gelu_apprx_tanh),
                     reads=[pb], writes=[gv])
                st = bnr.next()
                K.do(dve, lambda: V.bn_stats(out=st.ap[:, 0:6], in_=gv.ap[:]), reads=[gv], writes=[st])
                K.do(dve, lambda: V.bn_aggr(out=st.ap[:, 6:8], in_=st.ap[:, 0:6]), writes=[st])
                K.do(dve, lambda: V.tensor_scalar(st.ap[:, 7:8], st.ap[:, 7:8], EPS, None, ALU.add), writes=[st])
                K.do(pool, lambda: G.tensor_tensor(st.ap[:, 7:8], st.ap[:, 7:8], neg_half[:], ALU.pow),
                     reads=[B_const], writes=[st])
                vn = vnr.next()
                K.do(dve, lambda: V.tensor_scalar(vn.ap[:], gv.ap[:], st.ap[:, 6:7], st.ap[:, 7:8],
                                                  ALU.subtract, ALU.mult), reads=[gv, st], writes=[vn])
                for h in range(4):
                    mm_group(mixps[h].ap[:, jj * 128:(jj + 1) * 128], [(vn.ap[:, h * 128:(h + 1) * 128], wsT.ap[:, h, :])],
                             reads=[vn, wsT], pbuf=mixps[h], pw=(jj > 0))
                if jj == jpt - 1:
                    for h in range(4):
                        tm = tf.next()
                        bsap = bass.AP(bsrow.ap, h * 128, [[512, 128], [0, jpt], [1, 128]])
                        K.do(dve, lambda: V.scalar_tensor_tensor(
                            out=tm.ap[:].rearrange("p (a q) -> p a q", a=jpt),
                            in0=mixps[h].ap[:, 0:N].rearrange("p (a q) -> p a q", a=jpt),
                            scalar=sguc.ap[:, h:h + 1], in1=bsap, op0=ALU.mult, op1=ALU.add),
                            reads=[mixps[h], sguc, bsrow], writes=[tm])
                        K.do(dve, lambda: V.tensor_tensor(out=yaT[:, h, tt * N:(tt + 1) * N], in0=tm.ap[:],
                                                          in1=uT[:, h, tt * N:(tt + 1) * N], op=ALU.mult),
                             reads=[tm, B_u[h][tt]], pwrites=[B_ya[tt]])

            cw = Buf(sb("cw", [128, 3, 4], F32, ph))
            load_T(cw, cw.ap[:].rearrange("p j m -> p (j m)"), [(0, 12, conv_w[l].rearrange("j (m p) -> (j m) p", p=128))], 12, 128)
            ybT = sb("ybT", [128, 4, Ls], BF16, ph)
            B_yb = [Buf() for _ in range(ntt)]
            wbb, wbc, wbh = load_wblk(2), load_wblk(3), load_wblk(4)
            for mt in range(4):
                for tt in range(ntt):
                    pc_, ph_, pb_ = ps8.next(), ps8.next(), ps8.next()
                    proj_fm(pc_, wbc, mt, tt)
                    proj_fm(ph_, wbh, mt, tt)
                    proj_fm(pb_, wbb, mt, tt)
                    bcs = tf.next()
                    K.do(act, lambda: A.copy(bcs.ap[:], pc_.ap[:, 0:N]), reads=[pc_], writes=[bcs])
                    Pt = tf.next()
                    K.do(dve, lambda: V.tensor_tensor(out=Pt.ap[:], in0=bcs.ap[:], in1=ph_.ap[:, 0:N], op=ALU.mult),
                         reads=[bcs, ph_], writes=[Pt])
                    Tt = tf.next()
                    K.do(dve, lambda: V.tensor_scalar(Tt.ap[:], Pt.ap[:], cw.ap[:, 1, mt:mt + 1], None, ALU.mult),
                         reads=[Pt, cw], writes=[Tt])
                    if grid:
                        Tv = Tt.ap[:].rearrange("p (r c) -> p r c", c=64)
                        Pv = Pt.ap[:].rearrange("p (r c) -> p r c", c=64)
                        o1, i1, o2, i2 = Tv[:, :, 1:64], Pv[:, :, 0:63], Tv[:, :, 0:63], Pv[:, :, 1:64]
                    else:
                        o1, i1, o2, i2 = Tt.ap[:, 1:N], Pt.ap[:, 0:N - 1], Tt.ap[:, 0:N - 1], Pt.ap[:, 1:N]
                    K.do(dve, lambda: V.scalar_tensor_tensor(out=o1, in0=i1, scalar=cw.ap[:, 0, mt:mt + 1], in1=o1,
                                                             op0=ALU.mult, op1=ALU.add), reads=[Pt], writes=[Tt])
                    K.do(dve, lambda: V.scalar_tensor_tensor(out=o2, in0=i2, scalar=cw.ap[:, 2, mt:mt + 1], in1=o2,
                                                             op0=ALU.mult, op1=ALU.add), reads=[Pt], writes=[Tt])
                    K.do(dve, lambda: V.tensor_tensor(out=ybT[:, mt, tt * N:(tt + 1) * N], in0=Tt.ap[:],
                                                      in1=pb_.ap[:, 0:N], op=ALU.mult),
                         reads=[Tt, pb_], pwrites=[B_yb[tt]])

            wbr = [Buf(sb(f"wbr{i}", [128, 4, D], BF16, ph)) for i in range(2)]
            for i in range(2):
                K.dma(pool, [lambda: G.dma_start(out=wbr[i].ap[:], in_=w_branch[l, i]
                                                 .rearrange("(kt p) n -> p kt n", p=128))], wbr[i], writes=[wbr[i]])
            for half in range(2):
                wga, wgb, wgc = load_wblk(6 + half), load_wblk(8 + half), load_wblk(10 + half)
                for dtl in range(4):
                    dt_ = half * 4 + dtl
                    for tt in range(ntt):
                        pga, pgb, pgc, pba, pbb = [ps8.next() for _ in range(5)]
                        proj_fm(pga, wga, dtl, tt)
                        proj_fm(pgb, wgb, dtl, tt)
                        proj_fm(pgc, wgc, dtl, tt)
                        mm_group(pba.ap[:, 0:N], [(wbr[0].ap[:, kt, dt_ * 128:(dt_ + 1) * 128],
                                                   yaT[:, kt, tt * N:(tt + 1) * N]) for kt in range(4)],
                                 reads=[wbr[0], B_ya[tt]], pbuf=pba)
                        mm_group(pbb.ap[:, 0:N], [(wbr[1].ap[:, kt, dt_ * 128:(dt_ + 1) * 128],
                                                   ybT[:, kt, tt * N:(tt + 1) * N]) for kt in range(4)],
                                 reads=[wbr[1], B_yb[tt]], pbuf=pbb)
                        ga, gb, gc = tb.next(), tb.next(), tb.next()
                        for gg, pp in ((ga, pga), (gb, pgb), (gc, pgc)):
                            K.do(act, lambda: A.activation(out=gg.ap[:], in_=pp.ap[:, 0:N], func=AF.Sigmoid),
                                 reads=[pp], writes=[gg])
                        store(sq["GC"][dt_ * 128:(dt_ + 1) * 128, tt * N:(tt + 1) * N], gc)
                        m1, m2 = tf.next(), tf.next()
                        K.do(dve, lambda: V.tensor_tensor(out=m1.ap[:], in0=ga.ap[:], in1=pba.ap[:, 0:N], op=ALU.mult),
                             reads=[ga, pba], writes=[m1])
                        K.do(dve, lambda: V.tensor_tensor(out=m2.ap[:], in0=gb.ap[:], in1=pbb.ap[:, 0:N], op=ALU.mult),
                             reads=[gb, pbb], writes=[m2])
                        mab = tb.next()
                        K.do(pool, lambda: G.tensor_tensor(out=mab.ap[:], in0=m1.ap[:], in1=m2.ap[:], op=ALU.add),
                             reads=[m1, m2], writes=[mab])
                        store(sq["MAB"][dt_ * 128:(dt_ + 1) * 128, tt * N:(tt + 1) * N], mab)

    def phase_s5_tables(l):
        with scope() as po:
            Kacc = Buf(sb("Kacc", [128, 32, 128], F32, po))
            maskf = sb("maskf", [128, 128], F32, po)
            maskb = sb("maskb", [128, 128], F32, po)
            dcol = Buf(sb("dcol", [128, 32], F32, po))
            Bm = Buf()

            def emit_masks():
                G.memset(maskf[:], 0.0)
                G.affine_select(out=maskf[:].rearrange("p (t h) -> p t h", h=16), in_=maskf[:].rearrange("p (t h) -> p t h", h=16),
                                compare_op=ALU.is_ge, fill=1.0, base=-16, pattern=[[-16, 8], [0, 16]], channel_multiplier=1)
                G.memset(maskb[:], 1.0)
                return G.affine_select(out=maskb[:].rearrange("p (t h) -> p t h", h=16), in_=maskb[:].rearrange("p (t h) -> p t h", h=16),
                                       compare_op=ALU.is_ge, fill=0.0, base=0, pattern=[[-16, 8], [0, 16]], channel_multiplier=1)
            K.do(pool, emit_masks, writes=[Bm])
            with scope() as tmpd:
                dstg = Buf(sb("dstg", [32, 8, 16], F32, tmpd))
                K.dma(sp, [lambda k=k: S.dma_start(out=dstg.ap[:, k, :], in_=s5_d[l].rearrange("(g h) -> g h", h=16))
                           for k in range(8)], dstg, writes=[dstg])
                pbd = ps8.next()
                K.do(pe, lambda: T.transpose(pbd.ap[:, 0:32], dstg.ap[:].rearrange("g k h -> g (k h)"), ident_f[0:32, 0:32]),
                     reads=[dstg, B_const], writes=[pbd])
                K.do(act, lambda: A.copy(dcol.ap[:], pbd.ap[:, 0:32]), reads=[pbd], writes=[dcol])
            for d in range(2):
                with scope() as ph:
                    def t(name, shape, dt=F32):
                        return Buf(sb(name, shape, dt, ph))
                    lre, lim, dtb = t("lre", [64, 32]), t("lim", [64, 32]), t("dtb", [64, 32])
                    load_T(lre, lre.ap[:], [(0, 32, s5_lam_re[l, d])], 32, 64)
                    load_T(lim, lim.ap[:], [(0, 32, s5_lam_im[l, d])], 32, 64)
                    K.dma(sp, [lambda: S.dma_start(out=dtb.ap[:], in_=s5_log_dt[l, d].partition_broadcast(64))],
                          dtb, writes=[dtb])
                    K.do(act, lambda: A.activation(out=dtb.ap[:], in_=dtb.ap[:], func=AF.Exp), writes=[dtb])
                    X, ANG = t("X", [64, 32]), t("ANG", [64, 32])
                    K.do(dve, lambda: V.tensor_tensor(out=X.ap[:], in0=lre.ap[:], in1=dtb.ap[:], op=ALU.mult),
                         reads=[lre, dtb], writes=[X])
                    K.do(dve, lambda: V.tensor_tensor(out=ANG.ap[:], in0=lim.ap[:], in1=dtb.ap[:], op=ALU.mult),
                         reads=[lim, dtb], writes=[ANG])
                    EV = t("EV", [64, 16])
                    K.do(pool, lambda: [G.memset(EV.ap[:, i:i + 1], float(i - 7)) for i in range(16)][-1], writes=[EV])

                    def outer(dst, src):
                        in0 = bass.AP(src.ap, 0, [[32, 64], [1, 32], [0, 16]])
                        in1 = bass.AP(EV.ap, 0, [[16, 64], [0, 32], [1, 16]])
                        K.do(dve, lambda: V.tensor_tensor(out=dst.ap[:], in0=in0, in1=in1, op=ALU.mult),
                             reads=[src, EV], writes=[dst])
                    XE, AE = t("XE", [64, 32, 16]), t("AE", [64, 32, 16])
                    outer(XE, X)
                    outer(AE, ANG)
                    MAG = t("MAG", [64, 32, 16])
                    K.do(act, lambda: A.activation(out=MAG.ap[:], in_=XE.ap[:], func=AF.Exp), reads=[XE], writes=[MAG])
                    PRE, PIM = t("PRE", [64, 32, 16]), t("PIM", [64, 32, 16])
                    YI = t("YI", [64, 32, 16], I32)
                    YF, FR, MK = t("YF", [64, 32, 16]), t("FR", [64, 32, 16]), t("MK", [64, 32, 16])
                    for (dst, off) in ((PIM, 64.0), (PRE, 64.25)):
                        K.do(dve, lambda: V.tensor_scalar(FR.ap[:], AE.ap[:], 1.0 / TWO_PI, off, ALU.mult, ALU.add),
                             reads=[AE], writes=[FR])
                        K.do(dve, lambda: V.tensor_copy(YI.ap[:], FR.ap[:]), reads=[FR], writes=[YI])
                        K.do(dve, lambda: V.tensor_copy(YF.ap[:], YI.ap[:]), reads=[YI], writes=[YF])
                        K.do(dve, lambda: V.tensor_tensor(out=FR.ap[:], in0=FR.ap[:], in1=YF.ap[:], op=ALU.subtract),
                             reads=[YF], writes=[FR])
                        K.do(dve, lambda: V.tensor_scalar(MK.ap[:], FR.ap[:], 0.5, None, ALU.is_gt), reads=[FR], writes=[MK])
                        K.do(dve, lambda: V.tensor_tensor(out=FR.ap[:], in0=FR.ap[:], in1=MK.ap[:], op=ALU.subtract),
                             reads=[MK], writes=[FR])
                        K.do(dve, lambda: V.tensor_scalar(FR.ap[:], FR.ap[:], -0.5, 0.5, ALU.max, ALU.min), writes=[FR])
                        K.do(act, lambda: A.activation(out=dst.ap[:], in_=FR.ap[:], func=AF.Sin, scale=TWO_PI),
                             reads=[FR], writes=[dst])
                        K.do(dve, lambda: V.tensor_tensor(out=dst.ap[:], in0=dst.ap[:], in1=MAG.ap[:], op=ALU.mult),
                             reads=[MAG], writes=[dst])
                    ld = l * 2 + d
                    K.do(dve, lambda: [V.tensor_copy(a8_ar[:, ld, 0, :], PRE.ap[:, :, 15]),
                                       V.tensor_copy(a8_ar[:, ld, 1, :], PRE.ap[:, :, 15]),
                                       V.tensor_scalar(a8_ai[:, ld, 0, :], PIM.ap[:, :, 15], -1.0, None, ALU.mult),
                                       V.tensor_copy(a8_ai[:, ld, 1, :], PIM.ap[:, :, 15])][-1],
                         reads=[PRE, PIM], pwrites=[B_a8])
                    den, nr, fre, fim, t1, t2 = (t(n, [64, 32]) for n in ("den", "nr", "fre", "fim", "t1s", "t2s"))

                    def tt_(out, a, b, op, rd=(), wr=()):
                        K.do(dve, lambda: V.tensor_tensor(out=out, in0=a, in1=b, op=op), reads=list(rd), writes=list(wr))
                    tt_(den.ap[:], lre.ap[:], lre.ap[:], ALU.mult, [lre], [den])
                    tt_(t1.ap[:], lim.ap[:], lim.ap[:], ALU.mult, [lim], [t1])
                    tt_(den.ap[:], den.ap[:], t1.ap[:], ALU.add, [t1], [den])
                    K.do(dve, lambda: V.reciprocal(den.ap[:], den.ap[:]), writes=[den])
                    K.do(dve, lambda: V.tensor_scalar(nr.ap[:], PRE.ap[:, :, 8], -1.0, None, ALU.add), reads=[PRE], writes=[nr])
                    tt_(fre.ap[:], nr.ap[:], lre.ap[:], ALU.mult, [nr, lre], [fre])
                    tt_(t1.ap[:], PIM.ap[:, :, 8], lim.ap[:], ALU.mult, [PIM, lim], [t1])
                    tt_(fre.ap[:], fre.ap[:], t1.ap[:], ALU.add, [t1], [fre])
                    tt_(fre.ap[:], fre.ap[:], den.ap[:], ALU.mult, [den], [fre])
                    tt_(fim.ap[:], PIM.ap[:, :, 8], lre.ap[:], ALU.mult, [PIM, lre], [fim])
                    tt_(t2.ap[:], nr.ap[:], lim.ap[:], ALU.mult, [nr, lim], [t2])
                    tt_(fim.ap[:], fim.ap[:], t2.ap[:], ALU.subtract, [t2], [fim])
                    tt_(fim.ap[:], fim.ap[:], den.ap[:], ALU.mult, [den], [fim])
                    Bre, Bim = t("Bre", [64, 32, 16]), t("Bim", [64, 32, 16])
                    K.dma(sp, [lambda: S.dma_start(out=Bre.ap[:], in_=s5_b_re[l, d].rearrange("g p h -> p g h"))], Bre, writes=[Bre])
                    K.dma(sp, [lambda: S.dma_start(out=Bim.ap[:], in_=s5_b_im[l, d].rearrange("g p h -> p g h"))], Bim, writes=[Bim])
                    BBre, BBim, tq = t("BBre", [64, 32, 16]), t("BBim", [64, 32, 16]), t("tq", [64, 32, 16])

                    def fb(fsrc):
                        return bass.AP(fsrc.ap, 0, [[32, 64], [1, 32], [0, 16]])

                    def cmul(ore, oim, are, aim, bre, bim, tmp, rd):
                        tt_(ore[1], are, bre, ALU.mult, rd, [ore[0]])
                        tt_(tmp[1], aim, bim, ALU.mult, rd, [tmp[0]])
                        tt_(ore[1], ore[1], tmp[1], ALU.subtract, [tmp[0]], [ore[0]])
                        tt_(oim[1], are, bim, ALU.mult, rd, [oim[0]])
                        tt_(tmp[1], aim, bre, ALU.mult, rd, [tmp[0]])
                        tt_(oim[1], oim[1], tmp[1], ALU.add, [tmp[0]], [oim[0]])
                    cmul((BBre, BBre.ap[:]), (BBim, BBim.ap[:]), fb(fre), fb(fim), Bre.ap[:], Bim.ap[:], (tq, tq.ap[:]),
                         [fre, fim, Bre, Bim])
                    CTre, CTim = t("CTre", [64, 512]), t("CTim", [64, 512])
                    cin = t("cin", [128, 4, 64])
                    for (csrc, cdst) in ((s5_c_re, CTre), (s5_c_im, CTim)):
                        K.dma(sp, [lambda: S.dma_start(out=cin.ap[:], in_=csrc[l, d].rearrange("g h p -> (g h) p")
                                                       .rearrange("(a r) p -> r a p", r=128))], cin, writes=[cin])
                        pb = ps8.next()
                        K.do(pe, lambda: [T.transpose(pb.ap[0:64, a * 128:(a + 1) * 128], cin.ap[:, a, :], ident_f[:])
                                          for a in range(4)][-1], reads=[cin, B_const], writes=[pb])
                        K.do(act, lambda: A.copy(cdst.ap[:], pb.ap[0:64, :]), reads=[pb], writes=[cdst])
                    W1Tre, W1Tim, tw = t("W1Tre", [64, 32, 8, 16]), t("W1Tim", [64, 32, 8, 16]), t("tw", [64, 32, 8, 16])

                    def pview(Psrc, start, step):
                        return bass.AP(Psrc.ap, start, [[512, 64], [16, 32], [step, 8], [0, 16]])

                    def bview(Bsrc):
                        return bass.AP(Bsrc.ap, 0, [[512, 64], [16, 32], [0, 8], [1, 16]])
                    s1, st1 = (14, -1) if d == 0 else (7, 1)
                    cmul((W1Tre, W1Tre.ap[:]), (W1Tim, W1Tim.ap[:]), pview(PRE, s1, st1), pview(PIM, s1, st1),
                         bview(BBre), bview(BBim), (tw, tw.ap[:]), [PRE, PIM, BBre, BBim])
                    W2f = t("W2f", [64, 32, 2, 8, 16])
                    W2m = t("W2m", [64, 32, 2, 8, 16])

                    def w2build(dst, s2, st2):
                        ore = bass.AP(dst.ap, 0, [[8192, 64], [256, 32], [16, 8], [1, 16]])
                        oim = bass.AP(dst.ap, 128, [[8192, 64], [256, 32], [16, 8], [1, 16]])
                        cmul((dst, ore), (dst, oim), pview(PRE, s2, st2), pview(PIM, s2, st2),
                             bview(CTre), bview(CTim), (tw, tw.ap[:]), [PRE, PIM, CTre, CTim])
                        K.do(dve, lambda: V.tensor_scalar(oim, oim, -1.0, None, ALU.mult), writes=[dst])
                    s2, st2 = (8, 1) if d == 0 else (15, -1)
                    w2build(W2f, s2, st2)
                    s3, st3 = (0, 1) if d == 0 else (7, -1)
                    w2build(W2m, s3, st3)
                    W2b = t("W2b", [64, 32, 2, 128], BF16)
                    K.do(act, lambda: A.copy(W2b.ap[:].rearrange("p g r x -> p (g r x)"),
                                             W2f.ap[:].rearrange("p g r t h -> p (g r t h)")), reads=[W2f], writes=[W2b])
                    K.dma(sp, [lambda: S.dma_start(out=W2D[l, d], in_=W2b.ap[:])], W2b, reads=[W2b])
                    mask = maskf if d == 0 else maskb
                    tkr = mkring("tk", [128, 4, 128], F32, 2, ph)
                    for g4 in range(8):
                        pb = ps8.next()
                        for gg in range(4):
                            g = g4 * 4 + gg
                            mm_group(pb.ap[:, gg * 128:(gg + 1) * 128],
                                     [(W1Tre.ap[:, g].rearrange("p k h -> p (k h)"), W2m.ap[:, g, 0].rearrange("p t h -> p (t h)")),
                                      (W1Tim.ap[:, g].rearrange("p k h -> p (k h)"), W2m.ap[:, g, 1].rearrange("p t h -> p (t h)"))],
                                     reads=[W1Tre, W1Tim, W2m], pbuf=pb, pw=(gg > 0))
                        mk = bass.AP(mask, 0, [[128, 128], [0, 4], [1, 128]])
                        kv = Kacc.ap[:, g4 * 4:(g4 + 1) * 4, :]
                        pv3 = pb.ap[:].rearrange("p (a x) -> p a x", a=4)
                        if d == 0:
                            K.do(dve, lambda: V.tensor_tensor(out=kv, in0=pv3, in1=mk, op=ALU.mult),
                                 reads=[pb, Bm], pwrites=[Kacc])
                        else:
                            tk = tkr.next()
                            K.do(dve, lambda: V.tensor_tensor(out=tk.ap[:], in0=pv3, in1=mk, op=ALU.mult),
                                 reads=[pb, Bm], writes=[tk])
                            K.do(dve, lambda: V.tensor_tensor(out=kv, in0=kv, in1=tk.ap[:], op=ALU.add),
                                 reads=[tk, Kacc], pwrites=[Kacc])
                    W1b = t("W1b", [128, 32, 128], BF16)
                    for g4 in range(8):
                        pb = ps8.next()

                        def emit_tr():
                            ins = None
                            for gg in range(4):
                                g = g4 * 4 + gg
                                for ri, src in enumerate((W1Tre, W1Tim)):
                                    ins = T.transpose(pb.ap[:, gg * 128 + ri * 64: gg * 128 + ri * 64 + 64],
                                                      src.ap[:, g].rearrange("p k h -> p (k h)"), ident_f[0:64, 0:64])
                            return ins
                        K.do(pe, emit_tr, reads=[W1Tre, W1Tim, B_const], writes=[pb])
                        K.do(act, lambda: A.copy(W1b.ap[:, g4 * 4:(g4 + 1) * 4, :], pb.ap[:].rearrange("p (a x) -> p a x", a=4)),
                             reads=[pb], pwrites=[W1b])
                    K.dma(sp, [lambda: S.dma_start(out=W1D[l, d], in_=W1b.ap[:])], W1b, reads=[W1b])
            Kb = Buf(sb("Kb", [128, 32, 128], BF16, po))
            for g in range(32):
                K.do(dve, lambda: V.scalar_tensor_tensor(out=Kacc.ap[:, g, :], in0=ident_f[:], scalar=dcol.ap[:, g:g + 1],
                                                         in1=Kacc.ap[:, g, :], op0=ALU.mult, op1=ALU.add),
                     reads=[B_const, dcol, Kacc], pwrites=[Kacc])
            K.do(act, lambda: A.copy(Kb.ap[:], Kacc.ap[:]), reads=[Kacc], writes=[Kb])
            K.dma(sp, [lambda: S.dma_start(out=KMD[l], in_=Kb.ap[:])], Kb, reads=[Kb])

    def phase_s5(l, sq):
        Ls = sq["L"]
        C = Ls // 8
        b = sq["b"]
        with scope() as ph:
            U = sb("U", [128, 32, C], BF16, ph)
            B_U = Buf()
            EBm = sb("EBm", [64, 2, C, 2, 32], BF16, ph)
            EB = [EBm[:, d] for d in range(2)]
            B_E = [Buf(), Buf()]
            B_H = [Buf(), Buf()]
            with scope() as pa:
                cuT = Buf(sb("cuT", [128, 4, Ls], BF16, pa))
                W1 = Buf(sb("W1", [128, 2, 32, 128], BF16, pa))
                SEL = Buf(sb("SEL", [128, 8, 8, 128], BF16, pa))
                K.dma(sp, [lambda: S.dma_start(out=cuT.ap[:], in_=sq["CU"].rearrange("(m p) t -> p m t", p=128))], cuT, writes=[cuT])
                K.dma(sp, [lambda d=d: S.dma_start(out=W1.ap[:, d], in_=W1D[l, d]) for d in range(2)], W1, writes=[W1])
                K.dma(sp, [lambda: S.dma_start(out=SEL.ap[:], in_=SELD)], SEL, writes=[SEL])
                for g in range(32):
                    mt, gl = divmod(g, 8)
                    pb = ps8.next()
                    mm_group(pb.ap[:, 0:C], [(SEL.ap[:, gl, k, :], cuT.ap[:, mt, k::8]) for k in range(8)],
                             reads=[SEL, cuT], pbuf=pb)
                    if g % 2 == 0:
                        K.do(act, lambda: A.copy(U[:, g, :], pb.ap[:, 0:C]), reads=[pb], pwrites=[B_U])
                    else:
                        K.do(dve, lambda: V.tensor_copy(U[:, g, :], pb.ap[:, 0:C]), reads=[pb], pwrites=[B_U])
                for d in range(2):
                    for g in range(32):
                        pb = ps8.next()
                        mm_group(pb.ap[:, 0:C], [(W1.ap[:, d, g, :], U[:, g, :])], reads=[W1, B_U], pbuf=pb)
                        K.do(act, lambda: A.copy(EB[d][:, :, 0, g], pb.ap[0:64, 0:C]), reads=[pb], pwrites=[B_E[d]])
                        K.do(dve, lambda: V.tensor_copy(EB[d][:, :, 1, g], pb.ap[64:128, 0:C]), reads=[pb], pwrites=[B_E[d]])
            with scope() as pscan:
                sring = mkring("sst", [64, 128], F32, 4, pscan)
                t1 = Buf(sb("sc_t1", [64, 128], F32, pscan))
                t2 = Buf(sb("sc_t2", [64, 128], F32, pscan))
                ar = a8_ar[:, 2 * l:2 * l + 2].rearrange("p d a g -> p (d a g)")
                ai4 = a8_ai[:, 2 * l:2 * l + 2]
                h0v = h0st[:, 2 * b:2 * b + 2].rearrange("p d a g -> p (d a g)")
                sprev = sring.next()
                if sq["ctx"]:
                    K.do(dve, lambda: V.memset(sprev.ap[:], 0.0), writes=[sprev])
                else:
                    K.do(dve, lambda: V.tensor_copy(sprev.ap[:], h0v), reads=[B_h0], writes=[sprev])
                for i in range(C):
                    snew = sring.next()
                    ev_ap = bass.AP(EBm, i * 64, [[2 * C * 64, 64], [(2 * C - 1 - 2 * i) * 64, 2], [1, 64]])
                    swp = bass.AP(sprev.ap, 32, [[128, 64], [64, 2], [-32, 2], [1, 32]])

                    def step():
                        V.tensor_tensor(out=t1.ap[:], in0=sprev.ap[:], in1=ar, op=ALU.mult)
                        V.tensor_tensor(out=t2.ap[:].rearrange("p (d a g) -> p d a g", d=2, a=2), in0=swp, in1=ai4, op=ALU.mult)
                        V.tensor_tensor(out=t1.ap[:], in0=t1.ap[:], in1=t2.ap[:], op=ALU.add)
                        return V.tensor_tensor(out=snew.ap[:].rearrange("p (d x) -> p d x", d=2),
                                               in0=t1.ap[:].rearrange("p (d x) -> p d x", d=2), in1=ev_ap, op=ALU.add)
                    ev4 = K.do(dve, step, reads=[sprev, B_a8, B_E[0], B_E[1]], writes=[snew, t1, t2])
                    K.do(act, lambda: A.copy(ev_ap, sprev.ap[:].rearrange("p (d x) -> p d x", d=2)),
                         reads=[sprev], pwrites=[B_H[0], B_H[1]], after=[ev4])
                    sprev = snew
                if sq["ctx"]:
                    K.do(dve, lambda: V.tensor_copy(h0v, sprev.ap[:]), reads=[sprev], pwrites=[B_h0])
            with scope() as rd:
                W2 = Buf(sb("W2", [64, 2, 32, 2, 128], BF16, rd))
                KM = Buf(sb("KM", [128, 32, 128], BF16, rd))
                SELT = Buf(sb("SELT", [128, 8, 8, 128], BF16, rd))
                Y = sb("Y", [128, 32, C], BF16, rd)
                B_Y = Buf()
                zT = Buf(sb("zT", [128, 4, Ls], BF16, rd))
                K.dma(sp, [lambda d=d: S.dma_start(out=W2.ap[:, d], in_=W2D[l, d]) for d in range(2)], W2, writes=[W2])
                K.dma(sp, [lambda: S.dma_start(out=KM.ap[:], in_=KMD[l])], KM, writes=[KM])
                K.dma(sp, [lambda: S.dma_start(out=SELT.ap[:], in_=SELTD)], SELT, writes=[SELT])
                for g in range(32):
                    pb = ps8.next()
                    pairs = [(KM.ap[:, g, :], U[:, g, :])]
                    for d in range(2):
                        for ri in range(2):
                            pairs.append((W2.ap[:, d, g, ri, :], EB[d][:, :, ri, g]))
                    mm_group(pb.ap[:, 0:C], pairs, reads=[KM, W2, B_U, B_H[0], B_H[1]], pbuf=pb)
                    if g % 2 == 0:
                        K.do(act, lambda: A.copy(Y[:, g, :], pb.ap[:, 0:C]), reads=[pb], pwrites=[B_Y])
                    else:
                        K.do(dve, lambda: V.tensor_copy(Y[:, g, :], pb.ap[:, 0:C]), reads=[pb], pwrites=[B_Y])
                for mt in range(4):
                    for tau in range(8):
                        pb = ps8.next()
                        mm_group(pb.ap[:, 0:C], [(SELT.ap[:, gl, tau, :], Y[:, mt * 8 + gl, :]) for gl in range(8)],
                                 reads=[SELT, B_Y], pbuf=pb)
                        K.do(act, lambda: A.activation(out=zT.ap[:, mt, tau::8], in_=pb.ap[:, 0:C], func=AF.Gelu_apprx_tanh),
                             reads=[pb], pwrites=[zT])
                K.dma(sp, [lambda: S.dma_start(out=sq["YC"].rearrange("(m p) t -> p m t", p=128), in_=zT.ap[:])],
                      zT, reads=[zT])

    def phase_back(l, sq):
        Ls = sq["L"]
        N = min(512, Ls)
        ntt = Ls // N
        jpt = N // 128
        src = seq_src(l, sq)
        with scope() as ph:
            G1b = Buf(sb("g1b", [128, D], F32, ph))
            K.dma(sp, [lambda: S.dma_start(out=G1b.ap[:], in_=MOD[l, sq["modrow"], 2 * D:3 * D].partition_broadcast(128))],
                  G1b, writes=[G1b])
            G2b, SH2b = make_mod_tiles(l, sq["modrow"], norm2_g[l], 4 * D, 3 * D, ph, "m2")
            gluw = Buf(sb("gluw", [128, 4, WM], BF16, ph))
            wbr2 = Buf(sb("wbr2", [128, 4, D], BF16, ph))
            wout = Buf(sb("wout", [128, 8, D], BF16, ph))
            glub = Buf(sb("glub", [128, 4], F32, ph))
            K.dma(pool, [lambda: G.dma_start(out=gluw.ap[:], in_=glu_w[l].rearrange("(kt p) n -> p kt n", p=128))], gluw, writes=[gluw])
            K.dma(pool, [lambda: G.dma_start(out=wbr2.ap[:], in_=w_branch[l, 2].rearrange("(kt p) n -> p kt n", p=128))], wbr2, writes=[wbr2])
            K.dma(pool, [lambda: G.dma_start(out=wout.ap[:], in_=w_out[l].rearrange("(kt p) n -> p kt n", p=128))], wout, writes=[wout])
            load_T(glub, glub.ap[:], [(0, 4, glu_b[l].rearrange("(m p) -> m p", p=128))], 4, 128)
            zr = mkring("zin", [128, 4, N], BF16, 2, ph)
            mabr = mkring("mabin", [128, 8, N], BF16, 2, ph)
            gcr = mkring("gcin", [128, 8, N], BF16, 2, ph)
            ycr = mkring("ycT", [128, 4, N], BF16, 2, ph)
            mgr = mkring("mgT", [128, 8, N], BF16, 2, ph)
            tf = mkring("tfb", [128, N], F32, 3, ph)
            tb = mkring("tbb", [128, N], BF16, 3, ph)
            hr = mkring("hin", [128, D], F32, 2, ph)
            hnr = mkring("hn", [128, D], F32, 2, ph)
            tmpb = Buf(sb("tmp2", [128, D], F32, ph))
            xnr = mkring("xn2", [128, D], BF16, 2, ph)
            ssr = mkring("ss2", [128, 2], F32, 2, ph)
            xstr = mkring("xst", [128, 8, N], BF16, 2, ph)
            for tt in range(ntt):
                ts = slice(tt * N, (tt + 1) * N)
                zt, mabt, gct = zr.next(), mabr.next(), gcr.next()
                K.dma(sp, [lambda: S.dma_start(out=zt.ap[:], in_=sq["YC"][:, ts].rearrange("(m p) t -> p m t", p=128))], zt, writes=[zt])
                K.dma(sp, [lambda: S.dma_start(out=mabt.ap[:], in_=sq["MAB"][:, ts].rearrange("(m p) t -> p m t", p=128))], mabt, writes=[mabt])
                K.dma(sp, [lambda: S.dma_start(out=gct.ap[:], in_=sq["GC"][:, ts].rearrange("(m p) t -> p m t", p=128))], gct, writes=[gct])
                yc = ycr.next()
                for mt in range(4):
                    pb = ps8.next()
                    mm_group(pb.ap[:, 0:N], [(gluw.ap[:, kt, mt * 128:(mt + 1) * 128], zt.ap[:, kt, :]) for kt in range(4)],
                             reads=[gluw, zt], pbuf=pb)
                    sg = tb.next()
                    K.do(act, lambda: A.activation(out=sg.ap[:], in_=pb.ap[:, 0:N], func=AF.Sigmoid, bias=glub.ap[:, mt:mt + 1]),
                         reads=[pb, glub], writes=[sg])
                    K.do(dve, lambda: V.tensor_tensor(out=yc.ap[:, mt, :], in0=zt.ap[:, mt, :], in1=sg.ap[:], op=ALU.mult),
                         reads=[zt, sg], **(dict(writes=[yc]) if mt == 0 else dict(pwrites=[yc])))
                mg = mgr.next()
                for dt_ in range(8):
                    pb = ps8.next()
                    mm_group(pb.ap[:, 0:N], [(wbr2.ap[:, kt, dt_ * 128:(dt_ + 1) * 128], yc.ap[:, kt, :]) for kt in range(4)],
                             reads=[wbr2, yc], pbuf=pb)
                    m = tf.next()
                    K.do(dve, lambda: V.tensor_tensor(out=m.ap[:], in0=gct.ap[:, dt_, :], in1=pb.ap[:, 0:N], op=ALU.mult),
                         reads=[gct, pb], writes=[m])
                    K.do(pool, lambda: G.tensor_tensor(out=mg.ap[:, dt_, :], in0=m.ap[:], in1=mabt.ap[:, dt_, :], op=ALU.add),
                         reads=[m, mabt], **(dict(writes=[mg]) if dt_ == 0 else dict(pwrites=[mg])))
                xst = xstr.next()
                for jj in range(jpt):
                    r0 = tt * N + jj * 128
                    ht = hr.next()
                    K.dma(sp, [lambda: S.dma_start(out=ht.ap[:], in_=src[r0:r0 + 128, :])], ht, writes=[ht])
                    hn = hnr.next()
                    for half in range(2):
                        pb = ps8.next()
                        mm_group(pb.ap[:, :], [(mg.ap[:, kt, jj * 128:(jj + 1) * 128], wout.ap[:, kt, half * 512:(half + 1) * 512])
                                               for kt in range(8)], reads=[mg, wout], pbuf=pb)
                        hs = slice(half * 512, (half + 1) * 512)
                        K.do(dve, lambda: V.tensor_tensor(out=hn.ap[:, hs], in0=pb.ap[:, :], in1=G1b.ap[:, hs], op=ALU.mult),
                             reads=[pb, G1b], **(dict(writes=[hn]) if half == 0 else dict(pwrites=[hn])))
                    K.do(dve, lambda: V.tensor_tensor(out=hn.ap[:], in0=hn.ap[:], in1=ht.ap[:], op=ALU.add),
                         reads=[ht], writes=[hn])
                    K.dma(pool, [lambda: G.dma_start(out=sq["HA"][r0:r0 + 128, :], in_=hn.ap[:])], hn, reads=[hn])
                    xn = xnr.next()
                    ss = ssr.next()
                    rms_mod(hn, G2b, SH2b, xn, tmpb, ss)
                    transpose_tile(xn, xst.ap[:, :, jj * 128:(jj + 1) * 128], xst, pw=(jj > 0))
                K.dma(pool, [lambda: G.dma_start(out=sq["XN2T"][:, ts].rearrange("(k p) t -> p k t", p=128), in_=xst.ap[:])],
                      xst, reads=[xst])

    def phase_moe(l, seq_list):
        last = (l == DEPTH - 1)
        blocks = []
        ctxs = [sq for sq in seq_list if sq["ctx"]]
        if ctxs and not last:
            blocks.append([(sq, 0, LC) for sq in ctxs])
        for sq in seq_list:
            if not sq["ctx"]:
                for tt in range(LS // 512):
                    blocks.append([(sq, tt * 512, 512)])
        with scope() as ph:
            rw = Buf(sb("rw", [128, 8, NE], BF16, ph))
            rbb = Buf(sb("rbb", [128, 4, NE], F32, ph))
            K.dma(pool, [lambda: G.dma_start(out=rw.ap[:], in_=router_w.rearrange("(kt p) e -> p kt e", p=128))], rw, writes=[rw])
            K.dma(sp, [lambda j=j: S.dma_start(out=rbb.ap[:, j, :], in_=router_b.partition_broadcast(128)) for j in range(4)],
                  rbb, writes=[rbb])
            g2c = Buf(sb("g2c", [128, 3, 8], F32, ph))
            for r in range(3):
                load_T(g2c, g2c.ap[:, r, :], [(0, 8, MOD[l, r, 5 * D:6 * D].rearrange("(k p) -> k p", p=128))], 8, 128,
                       partial=(r > 0))
            G3b = None
            if last:
                G3b = Buf(sb("g3b", [128, D], F32, ph))
                K.dma(sp, [lambda: S.dma_start(out=G3b.ap[:], in_=final_norm_g.partition_broadcast(128))], G3b, writes=[G3b])
            xTr = mkring("xT", [128, 8, 512], BF16, 2, ph)
            heT = sb("heT", [128, 64, 512], BF16, ph)
            B_he = Buf()
            WR = mkring("wstream", [128, 16384], BF16, 2, ph)
            moT = sb("moT", [128, 8, 512], F32, ph)
            B_mo = Buf()
            gbr = mkring("gb", [128, 512], BF16, 2, ph)
            sr = mkring("sl", [128, 512], BF16, 2, ph)
            t1r = mkring("t1", [128, 512], BF16, 2, ph)
            hr = mkring("hin2", [128, D], F32, 2, ph)
            hnr = mkring("hn2", [128, D], F32, 2, ph)
            tmpb = Buf(sb("tmp3", [128, D], F32, ph))
            ssr = mkring("ss3", [128, 2], F32, 2, ph)
            sc = Buf(sb("r_sc", [128, 4, NE], F32, ph))
            bb = Buf(sb("r_b", [128, 4, NE], F32, ph))
            b2 = Buf(sb("r_b2", [128, 4, NE], F32, ph))
            q1 = Buf(sb("r_q1", [128, 4, NE], F32, ph))
            m16 = Buf(sb("r_m16", [128, 16], F32, ph))
            m16b = Buf(sb("r_m16b", [128, 16], F32, ph))
            m4 = Buf(sb("r_m4", [128, 4], F32, ph))
            gts = Buf(sb("r_gts", [128, 4, NE], BF16, ph))
            gT = Buf(sb("r_gT", [16, 512], BF16, ph))

            def bl(ap_t, off, pstride, dims):
                return bass.AP(ap_t, off, [[pstride, 128]] + dims)

            for blk in blocks:
                xT = xTr.next()
                off = 0
                ems = []
                for (sq, t0, n) in blk:
                    ems.append(lambda sq=sq, t0=t0, n=n, off=off: S.dma_start(
                        out=xT.ap[:, :, off:off + n], in_=sq["XN2T"][:, t0:t0 + n].rearrange("(k p) t -> p k t", p=128)))
                    off += n
                K.dma(sp, ems, xT, writes=[xT])
                pr = ps8.next()
                for j in range(4):
                    mm_group(pr.ap[:, j * NE:(j + 1) * NE], [(xT.ap[:, kt, j * 128:(j + 1) * 128], rw.ap[:, kt, :]) for kt in range(8)],
                             reads=[xT, rw], pbuf=pr, pw=(j > 0))
                K.do(act, lambda: A.activation(out=sc.ap[:].rearrange("p j e -> p (j e)"), in_=pr.ap[:, 0:4 * NE], func=AF.Sigmoid),
                     reads=[pr], writes=[sc])

                def vv(fn, rd, wr):
                    K.do(dve, fn, reads=rd, writes=wr)
                flat = lambda t_: t_.ap[:].rearrange("p j e -> p (j e)")
                g44 = lambda t_: t_.ap[:].rearrange("p j (g e) -> p (j g) e", e=4)
                vv(lambda: V.tensor_tensor(out=flat(bb), in0=flat(sc), in1=flat(rbb), op=ALU.add), [sc, rbb], [bb])
                vv(lambda: V.tensor_reduce(out=m16.ap[:], in_=g44(bb), axis=AX.X, op=ALU.max), [bb], [m16])
                m16bc = bl(m16.ap, 0, 16, [[1, 16], [0, 4]])
                vv(lambda: V.tensor_tensor(out=g44(q1), in0=g44(bb), in1=m16bc, op=ALU.is_equal), [bb, m16], [q1])
                vv(lambda: V.scalar_tensor_tensor(out=flat(b2), in0=flat(q1), scalar=-BIG, in1=flat(bb), op0=ALU.mult, op1=ALU.add),
                   [q1, bb], [b2])
                vv(lambda: V.tensor_reduce(out=m16b.ap[:], in_=g44(b2), axis=AX.X, op=ALU.max), [b2], [m16b])
                vv(lambda: V.tensor_tensor(out=m16.ap[:], in0=m16.ap[:], in1=m16b.ap[:], op=ALU.add), [m16b], [m16])
                vv(lambda: V.tensor_reduce(out=m4.ap[:], in_=m16.ap[:].rearrange("p (j g) -> p j g", g=4), axis=AX.X, op=ALU.max),
                   [m16], [m4])
                m4bc = bl(m4.ap, 0, 4, [[1, 4], [0, 4]])
                vv(lambda: V.tensor_tensor(out=m16b.ap[:].rearrange("p (j g) -> p j g", g=4),
                                           in0=m16.ap[:].rearrange("p (j g) -> p j g", g=4), in1=m4bc, op=ALU.is_equal),
                   [m16, m4], [m16b])
                vv(lambda: V.tensor_scalar(m16b.ap[:], m16b.ap[:], -1.0, BIG, ALU.add, ALU.mult), [], [m16b])
                penbc = bl(m16b.ap, 0, 16, [[1, 16], [0, 4]])
                vv(lambda: V.tensor_tensor(out=g44(b2), in0=g44(bb), in1=penbc, op=ALU.add), [bb, m16b], [b2])
                vv(lambda: V.tensor_reduce(out=m4.ap[:], in_=b2.ap[:], axis=AX.X, op=ALU.max), [b2], [m4])
                m4e = bl(m4.ap, 0, 4, [[1, 4], [0, NE]])
                vv(lambda: V.tensor_tensor(out=q1.ap[:], in0=b2.ap[:], in1=m4e, op=ALU.is_equal), [b2, m4], [q1])
                vv(lambda: V.scalar_tensor_tensor(out=flat(b2), in0=flat(q1), scalar=-BIG, in1=flat(b2), op0=ALU.mult, op1=ALU.add),
                   [q1], [b2])
                vv(lambda: V.tensor_reduce(out=m4.ap[:], in_=b2.ap[:], axis=AX.X, op=ALU.max), [b2], [m4])
                vv(lambda: V.tensor_tensor(out=bb.ap[:], in0=b2.ap[:], in1=m4e, op=ALU.is_equal), [b2, m4], [bb])
                vv(lambda: V.tensor_tensor(out=flat(q1), in0=flat(q1), in1=flat(bb), op=ALU.add), [bb], [q1])
                vv(lambda: V.tensor_tensor(out=flat(q1), in0=flat(q1), in1=flat(sc), op=ALU.mult), [sc], [q1])
                vv(lambda: V.tensor_reduce(out=m4.ap[:], in_=q1.ap[:], axis=AX.X, op=ALU.add), [q1], [m4])
                vv(lambda: V.reciprocal(m4.ap[:], m4.ap[:]), [], [m4])
                vv(lambda: V.tensor_tensor(out=gts.ap[:], in0=q1.ap[:], in1=m4e, op=ALU.mult), [q1, m4], [gts])
                if "GATES" in debug:
                    pass
                pg = ps8.next()
                pgv = pg.ap[:].bitcast(BF16)
                K.do(pe, lambda: [T.transpose(pgv[0:NE, j * 128:(j + 1) * 128], gts.ap[:, j, :], ident_bf[:])
                                  for j in range(4)][-1], reads=[gts, B_const], writes=[pg])
                K.do(act, lambda: A.copy(gT.ap[:], pgv[0:NE, 0:512]), reads=[pg], writes=[gT])
                for e in range(NE):
                    ws = WR.next()
                    after = []
                    K.dma(sp, [lambda: S.dma_start(out=ws.ap[:, 0:4096].rearrange("p (k f) -> p k f", k=8),
                                                   in_=WG[l, e].rearrange("(k p) f -> p k f", p=128)),
                               lambda: S.dma_start(out=ws.ap[:, 4096:8192].rearrange("p (k f) -> p k f", k=8),
                                                   in_=WU[l, e].rearrange("(k p) f -> p k f", p=128))],
                          ws, reads=[B_expw[l]], writes=[ws])
                    wgv = ws.ap[:, 0:4096].rearrange("p (k f) -> p k f", k=8)
                    wuv = ws.ap[:, 4096:8192].rearrange("p (k f) -> p k f", k=8)
                    pbc = ps8.next()
                    mm_group(pbc.ap[:, :], [(onesel[:, e, :], gT.ap[:, :])], reads=[gT, B_const], pbuf=pbc)
                    gb = gbr.next()
                    K.do(act, lambda: A.copy(gb.ap[:], pbc.ap[:, :]), reads=[pbc], writes=[gb])
                    for ft in range(4):
                        pgt, put = ps8.next(), ps8.next()
                        mm_group(pgt.ap[:, :], [(wgv[:, kt, ft * 128:(ft + 1) * 128], xT.ap[:, kt, :]) for kt in range(8)],
                                 reads=[ws, xT], pbuf=pgt)
                        mm_group(put.ap[:, :], [(wuv[:, kt, ft * 128:(ft + 1) * 128], xT.ap[:, kt, :]) for kt in range(8)],
                                 reads=[ws, xT], pbuf=put)
                        sl = sr.next()
                        K.do(act, lambda: A.activation(out=sl.ap[:], in_=pgt.ap[:, :], func=AF.Silu), reads=[pgt], writes=[sl])
                        t1 = t1r.next()
                        K.do(dve, lambda: V.tensor_tensor(out=t1.ap[:], in0=sl.ap[:], in1=put.ap[:, :], op=ALU.mult),
                             reads=[sl, put], writes=[t1])
                        first = (e == 0 and ft == 0)
                        K.do(pool, lambda: G.tensor_tensor(out=heT[:, e * 4 + ft, :], in0=t1.ap[:], in1=gb.ap[:], op=ALU.mult),
                             reads=[t1, gb], **(dict(writes=[B_he]) if first else dict(pwrites=[B_he])))
                for dt2 in range(4):
                    ws = WR.next()
                    wdv = ws.ap[:].rearrange("p (i c) -> p i c", c=256)
                    K.dma(sp, [lambda: S.dma_start(out=wdv, in_=WD[l][:, dt2 * 256:(dt2 + 1) * 256].rearrange("(i p) c -> p i c", p=128))],
                          ws, reads=[B_expw[l]], writes=[ws])
                    for dtl in range(2):
                        dt_ = dt2 * 2 + dtl
                        po = ps8.next()
                        mm_group(po.ap[:, :], [(wdv[:, i, dtl * 128:(dtl + 1) * 128], heT[:, i, :]) for i in range(64)],
                                 reads=[ws, B_he], pbuf=po)
                        mr = blk[0][0]["modrow"]
                        if len(blk) == 1:
                            K.do(act, lambda: A.activation(out=moT[:, dt_, :], in_=po.ap[:, :], func=AF.Copy,
                                                           scale=g2c.ap[:, mr, dt_:dt_ + 1]),
                                 reads=[po, g2c], **(dict(writes=[B_mo]) if dt_ == 0 else dict(pwrites=[B_mo])))
                        else:
                            o2 = 0
                            for pi, (sq, t0, n) in enumerate(blk):
                                K.do(act, lambda: A.activation(out=moT[:, dt_, o2:o2 + n], in_=po.ap[:, o2:o2 + n], func=AF.Copy,
                                                               scale=g2c.ap[:, sq["modrow"], dt_:dt_ + 1]),
                                     reads=[po, g2c], **(dict(writes=[B_mo]) if (dt_ == 0 and pi == 0) else dict(pwrites=[B_mo])))
                                o2 += n
                off = 0
                for (sq, t0, n) in blk:
                    for jj in range(n // 128):
                        c0 = off + jj * 128
                        r0 = t0 + jj * 128
                        ht = hr.next()
                        K.dma(sp, [lambda: S.dma_start(out=ht.ap[:], in_=sq["HA"][r0:r0 + 128, :])], ht, writes=[ht])
                        hn = hnr.next()
                        for half in range(2):
                            pt = ps8.next()
                            K.do(pe, lambda: [T.transpose(pt.ap[:, k * 128:(k + 1) * 128], moT[:, half * 4 + k, c0:c0 + 128], ident_f[:])
                                              for k in range(4)][-1], reads=[B_mo, B_const], writes=[pt])
                            hs = slice(half * 512, (half + 1) * 512)
                            K.do(dve, lambda: V.tensor_tensor(out=hn.ap[:, hs], in0=pt.ap[:, :], in1=ht.ap[:, hs], op=ALU.add),
                                 reads=[pt, ht], **(dict(writes=[hn]) if half == 0 else dict(pwrites=[hn])))
                        if not last:
                            K.dma(pool, [lambda: G.dma_start(out=sq["HB"][r0:r0 + 128, :], in_=hn.ap[:])], hn, reads=[hn])
                        else:
                            ss = ssr.next()
                            rms_mod(hn, G3b, None, None, tmpb, ss)
                            K.dma(pool, [lambda: G.dma_start(out=out_d[sq["b"], r0:r0 + 128, :], in_=tmpb.ap[:])], tmpb, reads=[tmpb])
                    off += n

    DBG = {}
    debug_seq = 2
    if "XNT" in debug:
        DBG["XNT"] = nc.dram_tensor("XNT", [D, LS], BF16, kind="ExternalOutput").ap()

    def stop(name):
        return stop_after == name

    def finish():
        K.barrier()
        es.close()
        return nc

    init_consts()
    seq_list = [SEQ[s] for s in seqs]
    for l in range(nlayers):
        convert_experts(l)
    for l in range(nlayers):
        phase_adaln(l)
    if stop("adaln"):
        return finish()
    for l in range(nlayers):
        phase_s5_tables(l)
    if stop("tables"):
        return finish()
    for l in range(nlayers):
        last = (l == DEPTH - 1)
        for sq in seq_list:
            phase_front(l, sq, only_cu=(last and sq["ctx"]))
        if stop(f"front{l}"):
            return finish()
        for sq in seq_list:
            phase_s5(l, sq)
        if stop(f"s5{l}"):
            return finish()
        for sq in seq_list:
            if not (last and sq["ctx"]):
                phase_back(l, sq)
        if stop(f"back{l}"):
            return finish()
        phase_moe(l, seq_list)
        if stop(f"moe{l}"):
            return finish()
    return finish()


_PROG = {}


def _inputs_for_core(inputs, c):
    m = {}
    b0 = 2 * c
    f = lambda a: np.ascontiguousarray(np.asarray(a, dtype=np.float32))
    m["x"] = f(inputs["x"][b0:b0 + 2])
    m["ctx"] = f(inputs["ctx"][b0:b0 + 2])
    m["cvec"] = f(np.stack([inputs["c"][b0], inputs["c"][b0 + 1], inputs["c_ctx"]], axis=0))
    for k in ("w_mod", "b_mod", "norm1_g", "norm2_g", "w_in", "sgu_norm_g", "sgu_w", "sgu_b", "conv_w",
              "s5_lam_re", "s5_lam_im", "s5_log_dt", "s5_b_re", "s5_b_im", "s5_c_re", "s5_c_im", "s5_d",
              "glu_w", "glu_b", "w_branch", "w_out", "router_w", "router_b", "exp_w_gate", "exp_w_up",
              "exp_w_down", "final_norm_g"):
        m[k] = f(inputs[k])
    return m


def kernel(**inputs):
    if "nc" not in _PROG:
        _PROG["nc"] = build_program()
    nc = _PROG["nc"]
    shared = _inputs_for_core(inputs, 0)
    in_maps = []
    for c in range(NCORES):
        m = dict(shared)
        b0 = 2 * c
        m["x"] = np.ascontiguousarray(np.asarray(inputs["x"][b0:b0 + 2], dtype=np.float32))
        m["ctx"] = np.ascontiguousarray(np.asarray(inputs["ctx"][b0:b0 + 2], dtype=np.float32))
        m["cvec"] = np.ascontiguousarray(np.stack([inputs["c"][b0], inputs["c"][b0 + 1], inputs["c_ctx"]], axis=0).astype(np.float32))
        in_maps.append(m)
    res = run_bass_kernel_spmd(nc, in_maps, core_ids=list(range(NCORES)))
    out = np.concatenate([np.asarray(r["out"]) for r in res.results], axis=0)
    return out.astype(np.float32)
```

```python
import math
import numpy as np
from contextlib import ExitStack, contextmanager
import concourse.bass as bass
import concourse.mybir as mybir
from concourse.bass_utils import run_bass_kernel_spmd

F32 = mybir.dt.float32
BF16 = mybir.dt.bfloat16
I32 = mybir.dt.int32
AF = mybir.ActivationFunctionType
ALU = mybir.AluOpType
AX = mybir.AxisListType

D = 1024
LS = 2048
LC = 256
DEPTH = 2
DIN = 6144
WM = 512
NE = 16
DFF = 512
NCORES = 8
EPS = 1e-6
BIG = 1.0e4
TWO_PI = 2.0 * math.pi


class Eng:
    def __init__(self, nc, e, name):
        self.e = e
        self.sem = nc.alloc_semaphore(name)
        self.n = 0
        self.seen = {}

    def wait(self, evs):
        for sem, val in evs.items():
            if self.seen.get(sem, 0) >= val:
                continue
            self.e.wait_ge(sem, val)
            self.seen[sem] = val

    def sig(self, ins):
        self.n += 1
        ins.then_inc(self.sem, 1)
        return (self.sem, self.n)


class Buf:
    def __init__(self, ap=None):
        self.ap = ap
        self.w = {}
        self.r = {}
        self.dsem = None
        self.slot = None
        self.dv = 0


def _merge(d, ev):
    s, v = ev
    if d.get(s, 0) < v:
        d[s] = v


class Ring:
    def __init__(self, bufs):
        self.bufs = bufs
        self.i = 0

    def next(self):
        b = self.bufs[self.i]
        self.i = (self.i + 1) % len(self.bufs)
        return b


class Ker:
    def __init__(self, nc):
        self.nc = nc
        self.pe = Eng(nc, nc.tensor, "s_pe")
        self.act = Eng(nc, nc.scalar, "s_act")
        self.dve = Eng(nc, nc.vector, "s_dve")
        self.pool = Eng(nc, nc.gpsimd, "s_pool")
        self.sp = Eng(nc, nc.sync, "s_sp")
        self.engs = [self.pe, self.act, self.dve, self.pool, self.sp]
        self.dbufs = []
        self.uid = 0
        self.free_slots = []
        self.scope_bufs = [[]]

    def _pre(self, eng, reads, writes, after):
        evs = {}
        for b in reads:
            for s, v in b.w.items():
                _merge(evs, (s, v))
        for b in writes:
            for s, v in b.w.items():
                _merge(evs, (s, v))
            for s, v in b.r.items():
                _merge(evs, (s, v))
        for ev in after:
            _merge(evs, ev)
        eng.wait(evs)

    def _post(self, ev, reads, writes, pwrites):
        for b in reads:
            _merge(b.r, ev)
        for b in writes:
            b.w = {ev[0]: ev[1]}
            b.r = {}
        for b in pwrites:
            _merge(b.w, ev)

    def do(self, eng, emit, reads=(), writes=(), pwrites=(), after=()):
        self._pre(eng, reads, writes, after)
        ins = emit()
        ev = eng.sig(ins)
        self._post(ev, reads, writes, pwrites)
        return ev

    def dma(self, q, emits, owner, reads=(), writes=(), pwrites=(), after=()):
        self._pre(q, reads, writes, after)
        if owner.dsem is None:
            if self.free_slots:
                owner.slot = self.free_slots.pop()
            else:
                self.uid += 1
                owner.slot = [self.nc.alloc_semaphore(f"dq{self.uid}"), 0]
            owner.dsem = owner.slot[0]
            owner.dv = owner.slot[1]
            self.dbufs.append(owner)
            self.scope_bufs[-1].append(owner)
        for em in emits:
            ins = em()
            ins.then_inc(owner.dsem, 16)
            owner.dv += 16
        owner.slot[1] = owner.dv
        ev = (owner.dsem, owner.dv)
        self._post(ev, reads, writes, pwrites)
        return ev

    def barrier(self):
        evs = {}
        for e in self.engs:
            if e.n > 0:
                evs[e.sem] = e.n
        for b in self.dbufs:
            if b.dv > 0:
                evs[b.dsem] = b.dv
        for e in self.engs:
            e.wait(evs)

    def open_scope(self):
        self.scope_bufs.append([])

    def close_scope(self):
        for b in self.scope_bufs.pop():
            self.free_slots.append(b.slot)
            self.dbufs.remove(b)
            b.dsem = None
            b.slot = None


def build_program(debug=(), seqs=(0, 1, 2, 3), stop_after=None, nlayers=DEPTH):
    debug = set(debug)
    nc = bass.Bass("TRN2", target_bir_lowering=False)
    K = Ker(nc)
    es = ExitStack()

    def dram_in(name, shape):
        return nc.dram_tensor(name, list(shape), F32, kind="ExternalInput").ap()

    def dram_scr(name, shape, dt):
        if name in debug:
            return nc.dram_tensor(name, list(shape), dt, kind="ExternalOutput").ap()
        return nc.dram_tensor(name, list(shape), dt).ap()

    x_in = dram_in("x", [2, LS, D])
    ctx_in = dram_in("ctx", [2, LC, D])
    cvec = dram_in("cvec", [3, D])
    w_mod = dram_in("w_mod", [DEPTH, D, 6 * D])
    b_mod = dram_in("b_mod", [DEPTH, 6 * D])
    norm1_g = dram_in("norm1_g", [DEPTH, D])
    norm2_g = dram_in("norm2_g", [DEPTH, D])
    w_in = dram_in("w_in", [DEPTH, D, DIN])
    sgu_norm_g = dram_in("sgu_norm_g", [DEPTH, WM])
    sgu_w = dram_in("sgu_w", [DEPTH, 4, 128, 128])
    sgu_b = dram_in("sgu_b", [DEPTH, 4, 128])
    conv_w = dram_in("conv_w", [DEPTH, 3, WM])
    s5_lam_re = dram_in("s5_lam_re", [DEPTH, 2, 32, 64])
    s5_lam_im = dram_in("s5_lam_im", [DEPTH, 2, 32, 64])
    s5_log_dt = dram_in("s5_log_dt", [DEPTH, 2, 32])
    s5_b_re = dram_in("s5_b_re", [DEPTH, 2, 32, 64, 16])
    s5_b_im = dram_in("s5_b_im", [DEPTH, 2, 32, 64, 16])
    s5_c_re = dram_in("s5_c_re", [DEPTH, 2, 32, 16, 64])
    s5_c_im = dram_in("s5_c_im", [DEPTH, 2, 32, 16, 64])
    s5_d = dram_in("s5_d", [DEPTH, WM])
    glu_w = dram_in("glu_w", [DEPTH, WM, WM])
    glu_b = dram_in("glu_b", [DEPTH, WM])
    w_branch = dram_in("w_branch", [DEPTH, 3, WM, D])
    w_out = dram_in("w_out", [DEPTH, D, D])
    router_w = dram_in("router_w", [D, NE])
    router_b = dram_in("router_b", [NE])
    exp_w_gate = dram_in("exp_w_gate", [DEPTH, NE, D, DFF])
    exp_w_up = dram_in("exp_w_up", [DEPTH, NE, D, DFF])
    exp_w_down = dram_in("exp_w_down", [DEPTH, NE, DFF, D])
    final_norm_g = dram_in("final_norm_g", [D])
    out_d = nc.dram_tensor("out", [2, LS, D], F32, kind="ExternalOutput").ap()

    MOD = dram_scr("MOD", [DEPTH, 3, 6 * D], F32)
    SEQ = []
    for s in range(4):
        isctx = s < 2
        b = s % 2
        Ls = LC if isctx else LS
        SEQ.append(dict(
            s=s, b=b, ctx=isctx, L=Ls, modrow=(2 if isctx else b),
            src=(ctx_in[b] if isctx else x_in[b]),
            CU=dram_scr(f"CU{s}", [WM, Ls], BF16),
            YC=dram_scr(f"YC{s}", [WM, Ls], BF16),
            MAB=dram_scr(f"MAB{s}", [D, Ls], BF16),
            GC=dram_scr(f"GC{s}", [D, Ls], BF16),
            XN2T=dram_scr(f"XN2T{s}", [D, Ls], BF16),
            XN2R=dram_scr(f"XN2R{s}", [Ls, D], BF16),
            HA=dram_scr(f"HA{s}", [Ls, D], F32),
            HB=dram_scr(f"HB{s}", [Ls, D], F32),
        ))
    WG = [dram_scr(f"WG{l}", [NE * 128, 8 * DFF], BF16) for l in range(DEPTH)]
    WU = [dram_scr(f"WU{l}", [NE * 128, 8 * DFF], BF16) for l in range(DEPTH)]
    WD = [dram_scr(f"WD{l}", [NE * 128, 4 * D], BF16) for l in range(DEPTH)]
    TMAX = (2 * (2 * LS + 2 * LC) + NE * 511) // 512
    XS = dram_scr("XS", [TMAX * 512, D], BF16)
    YS = dram_scr("YS", [TMAX * 512, D], BF16)
    W1D = dram_scr("W1D", [DEPTH, 2, 128, 32, 128], BF16)
    W2D = dram_scr("W2D", [DEPTH, 2, 64, 32, 2, 128], BF16)
    KMD = dram_scr("KMD", [DEPTH, 128, 32, 128], BF16)
    SELD = dram_scr("SELD", [128, 8, 8, 128], BF16)
    SELTD = dram_scr("SELTD", [128, 8, 8, 128], BF16)

    def sb(name, shape, dt, stack=None):
        K.uid += 1
        return (stack or es).enter_context(nc.sbuf_tensor(f"{name}_{K.uid}", list(shape), dt))

    @contextmanager
    def scope():
        st = ExitStack()
        K.open_scope()
        try:
            yield st
        finally:
            K.barrier()
            K.close_scope()
            st.close()

    def mkring(name, shape, dt, n, stack):
        return Ring([Buf(sb(f"{name}{i}", shape, dt, stack)) for i in range(n)])

    ident_bf = sb("ident_bf", [128, 128], BF16)
    ident_f = sb("ident_f", [128, 128], F32)
    neg_half = sb("neg_half", [128, 1], F32)
    onesel = sb("onesel", [16, NE, 128], BF16)
    a8_ar = sb("a8_ar", [64, DEPTH * 2, 2, 32], F32)
    a8_ai = sb("a8_ai", [64, DEPTH * 2, 2, 32], F32)
    h0st = sb("h0st", [64, 4, 2, 32], F32)
    ones_bf = sb("ones_bf", [128, 128], BF16)
    tri_bf = sb("tri_bf", [128, 128], BF16)
    iot = sb("iot", [128, TMAX], F32)
    basek = sb("basek", [128, 8], F32)
    B_XS = Buf()
    B_YS = Buf()
    B_const = Buf()
    B_a8 = Buf()
    B_h0 = Buf()

    ps_t = [es.enter_context(nc.psum_tensor(f"ps{i}", [128, 512], F32)) for i in range(8)]
    PS = [Buf(ps_t[i]) for i in range(8)]
    ps8 = Ring(PS)
    psA = Ring(PS[0:4])
    psB = Ring(PS[4:8])

    pe, act, dve, pool, sp = K.pe, K.act, K.dve, K.pool, K.sp
    V, A, T, G, S = nc.vector, nc.scalar, nc.tensor, nc.gpsimd, nc.sync

    def mm_group(out_ap, pairs, reads, pbuf, pw=False):
        def emit():
            n = len(pairs)
            ins = None
            for i, (lh, rh) in enumerate(pairs):
                ins = T.matmul(out_ap, lh, rh, start=(i == 0), stop=(i == n - 1))
            return ins
        if pw:
            return K.do(pe, emit, reads=reads, pwrites=[pbuf])
        return K.do(pe, emit, reads=reads, writes=[pbuf])

    def load_T(dst, dst_ap, srcs, F, P, partial=False):
        with scope() as tmp:
            stg = Buf(sb("ldT", [F, P], F32, tmp))
            K.dma(sp, [lambda r0=r0, n=n, a=a: S.dma_start(out=stg.ap[r0:r0 + n, :], in_=a) for (r0, n, a) in srcs],
                  stg, writes=[stg])
            pb = ps8.next()
            K.do(pe, lambda: T.transpose(pb.ap[0:P, 0:F], stg.ap[:], ident_f[0:F, 0:F]), reads=[stg, B_const], writes=[pb])
            kw = dict(pwrites=[dst]) if partial else dict(writes=[dst])
            K.do(act, lambda: A.copy(dst_ap, pb.ap[0:P, 0:F]), reads=[pb], **kw)

    def init_consts():
        def emit():
            G.memset(ident_bf[:], 0.0)
            G.affine_select(out=ident_bf[:], in_=ident_bf[:], compare_op=ALU.not_equal, fill=1.0,
                            base=0, pattern=[[-1, 128]], channel_multiplier=1)
            G.memset(ident_f[:], 0.0)
            G.affine_select(out=ident_f[:], in_=ident_f[:], compare_op=ALU.not_equal, fill=1.0,
                            base=0, pattern=[[-1, 128]], channel_multiplier=1)
            G.memset(neg_half[:], -0.5)
            G.memset(onesel[:], 0.0)
            G.memset(h0st[:], 0.0)
            return G.affine_select(out=onesel[:], in_=onesel[:], compare_op=ALU.not_equal, fill=1.0,
                                   base=0, pattern=[[-1, NE], [0, 128]], channel_multiplier=1)
        K.do(pool, emit, writes=[B_const, B_h0])

        def emit2():
            G.memset(ones_bf[:], 1.0)
            G.memset(tri_bf[:], 0.0)
            G.affine_select(out=tri_bf[:], in_=tri_bf[:], compare_op=ALU.is_ge, fill=1.0,
                            base=0, pattern=[[-1, 128]], channel_multiplier=1)
            ins = None
            for i in range(TMAX):
                ins = G.memset(iot[:, i:i + 1], float(i))
            return ins
        K.do(pool, emit2, pwrites=[B_const])
        pbp = ps8.next()
        K.do(pe, lambda: T.matmul(pbp.ap[:, 0:1], tri_bf[:], ones_bf[:, 0:1], start=True, stop=True), reads=[B_const], writes=[pbp])
        K.do(dve, lambda: [V.tensor_scalar(basek[:, kt:kt + 1], pbp.ap[:, 0:1], float(kt * 128), None, ALU.add)
                           for kt in range(8)][-1], reads=[pbp], pwrites=[B_const])
        with scope() as pz:
            zt = Buf(sb("zfill", [128, 8192], BF16, pz))
            K.do(dve, lambda: V.memset(zt.ap[:], 0.0), writes=[zt])
            nrows = TMAX * 512
            ems = []
            for r0 in range(0, nrows, 1024):
                nr = min(1024, nrows - r0)
                ems.append(lambda r0=r0, nr=nr: G.dma_start(out=XS[r0:r0 + nr, :].rearrange("(p a) d -> p (a d)", p=128),
                                                            in_=zt.ap[:, 0:(nr // 128) * D]))
            K.dma(pool, ems, zt, reads=[zt], pwrites=[B_XS])
        with scope() as ph:
            selt = sb("selt", [128, 8, 8, 128], BF16, ph)
            Bs = Buf()

            def emit_sel(transposed):
                def f():
                    ins = G.memset(selt[:], 0.0)
                    idv = ident_bf[:].rearrange("p (a b) -> p a b", b=16)
                    for j in range(8):
                        if not transposed:
                            ins = G.tensor_copy(selt[:, :, j, 16 * j:16 * j + 16], idv)
                        else:
                            ins = G.tensor_copy(selt[:, j, :, 16 * j:16 * j + 16], idv)
                    return ins
                return f
            K.do(pool, emit_sel(False), reads=[B_const], writes=[Bs])
            K.dma(sp, [lambda: S.dma_start(out=SELD, in_=selt[:])], Bs, reads=[Bs])
            K.do(pool, emit_sel(True), reads=[B_const], writes=[Bs])
            K.dma(sp, [lambda: S.dma_start(out=SELTD, in_=selt[:])], Bs, reads=[Bs])

    B_expw = [Buf() for _ in range(DEPTH)]

    def convert_experts(l):
        ems = []
        for e in range(NE):
            for (dst, src) in ((WG[l], exp_w_gate[l, e]), (WU[l], exp_w_up[l, e]), (WD[l], exp_w_down[l, e])):
                ems.append(lambda dst=dst, src=src, e=e: G.dma_start(
                    out=dst[e * 128:(e + 1) * 128, :].rearrange("(r a) c -> r (a c)", r=64),
                    in_=src.rearrange("(r a) f -> r (a f)", r=64)))
        K.dma(pool, ems, B_expw[l], pwrites=[B_expw[l]])

    def phase_adaln(l):
        with scope() as ph:
            csb = Buf(sb("csb", [3, D], F32, ph))
            scT = Buf(sb("scT", [128, 8, 3], F32, ph))
            bm = Buf(sb("bm", [3, 6 * D], F32, ph))
            modsb = Buf(sb("modsb", [3, 6 * D], F32, ph))
            wmr = mkring("wm", [128, 8, 512], F32, 4, ph)
            K.dma(sp, [lambda: S.dma_start(out=csb.ap[:], in_=cvec)], csb, writes=[csb])
            K.dma(sp, [lambda: S.dma_start(out=bm.ap[:], in_=b_mod[l].partition_broadcast(3))], bm, writes=[bm])
            pb = ps8.next()
            K.do(pe, lambda: [T.transpose(pb.ap[:, kt * 3:kt * 3 + 3], csb.ap[:, kt * 128:(kt + 1) * 128],
                                          ident_f[0:3, 0:3]) for kt in range(8)][-1],
                 reads=[csb, B_const], writes=[pb])
            K.do(act, lambda: A.activation(out=scT.ap[:], in_=pb.ap[:, 0:24].rearrange("p (k j) -> p k j", k=8),
                                           func=AF.Silu), reads=[pb], writes=[scT])
            for cc in range(12):
                wm = wmr.next()
                qe, QE = sp, S
                K.dma(qe, [lambda h=h: QE.dma_start(out=wm.ap[:, h * 4:(h + 1) * 4, :], in_=w_mod[l][h * 512:(h + 1) * 512, cc * 512:(cc + 1) * 512]
                                                    .rearrange("(kt p) n -> p kt n", p=128)) for h in range(2)], wm, writes=[wm])
                pb = ps8.next()
                mm_group(pb.ap[0:3, :], [(scT.ap[:, kt, :], wm.ap[:, kt, :]) for kt in range(8)],
                         reads=[scT, wm], pbuf=pb)
                K.do(dve, lambda: V.tensor_tensor(out=modsb.ap[:, cc * 512:(cc + 1) * 512], in0=pb.ap[0:3, :],
                                                  in1=bm.ap[:, cc * 512:(cc + 1) * 512], op=ALU.add),
                     reads=[pb, bm], pwrites=[modsb])
            K.dma(sp, [lambda: S.dma_start(out=MOD[l], in_=modsb.ap[:])], modsb, reads=[modsb])

    def make_mod_tiles(l, modrow, g_row, sc_off, sh_off, ph, name):
        gt = Buf(sb(name + "_g", [128, D], F32, ph))
        ht = Buf(sb(name + "_h", [128, D], F32, ph))
        with scope() as tmp:
            st = Buf(sb(name + "_s", [128, D], F32, tmp))
            K.dma(sp, [lambda: S.dma_start(out=gt.ap[:], in_=g_row.partition_broadcast(128))], gt, writes=[gt])
            K.dma(sp, [lambda: S.dma_start(out=st.ap[:], in_=MOD[l, modrow, sc_off:sc_off + D].partition_broadcast(128))],
                  st, writes=[st])
            K.dma(sp, [lambda: S.dma_start(out=ht.ap[:], in_=MOD[l, modrow, sh_off:sh_off + D].partition_broadcast(128))],
                  ht, writes=[ht])
            K.do(dve, lambda: V.scalar_tensor_tensor(out=gt.ap[:], in0=st.ap[:], scalar=1.0, in1=gt.ap[:],
                                                     op0=ALU.add, op1=ALU.mult), reads=[st], writes=[gt])
        return gt, ht

    def rstd_from_sumsq(ss):
        K.do(dve, lambda: V.tensor_scalar(ss.ap[:, 0:1], ss.ap[:, 0:1], 1.0 / D, EPS, ALU.mult, ALU.add), writes=[ss])
        K.do(pool, lambda: G.tensor_tensor(ss.ap[:, 1:2], ss.ap[:, 0:1], neg_half[:], ALU.pow),
             reads=[B_const], writes=[ss])

    def rms_mod(xt, Gb, SHb, xn, tmp, ss):
        K.do(dve, lambda: V.tensor_tensor(out=tmp.ap[:], in0=xt.ap[:], in1=xt.ap[:], op=ALU.mult), reads=[xt], writes=[tmp])
        K.do(dve, lambda: V.tensor_reduce(out=ss.ap[:, 0:1], in_=tmp.ap[:], axis=AX.X, op=ALU.add), reads=[tmp], writes=[ss])
        rstd_from_sumsq(ss)
        K.do(dve, lambda: V.scalar_tensor_tensor(out=tmp.ap[:], in0=xt.ap[:], scalar=ss.ap[:, 1:2], in1=Gb.ap[:],
                                                 op0=ALU.mult, op1=ALU.mult), reads=[xt, ss, Gb], writes=[tmp])
        if SHb is None:
            return
        K.do(dve, lambda: V.tensor_tensor(out=xn.ap[:], in0=tmp.ap[:], in1=SHb.ap[:], op=ALU.add),
             reads=[tmp, SHb], writes=[xn])

    def transpose_tile(xn, dest_ap3, dest_buf, pw=False):
        pb = ps8.next()
        pv = pb.ap[:].bitcast(BF16)
        K.do(pe, lambda: [T.transpose(pv[:, kt * 128:(kt + 1) * 128], xn.ap[:, kt * 128:(kt + 1) * 128], ident_bf[:])
                          for kt in range(8)][-1], reads=[xn, B_const], writes=[pb])
        kw = dict(pwrites=[dest_buf]) if pw else dict(writes=[dest_buf])
        K.do(act, lambda: A.copy(dest_ap3, pv.rearrange("p (k t) -> p k t", k=8)), reads=[pb], **kw)

    def seq_src(l, sq):
        return sq["src"] if l == 0 else sq["HB"]

    def phase_front(l, sq, only_cu=False):
        Ls = sq["L"]
        N = min(512, Ls)
        ntt = Ls // N
        nj = Ls // 128
        jpt = N // 128
        grid = not sq["ctx"]
        with scope() as ph:
            xnT = sb("xnT", [128, 8, Ls], BF16, ph)
            B_xn = [Buf() for _ in range(nj)]
            with scope() as p1:
                Gb, SHb = make_mod_tiles(l, sq["modrow"], norm1_g[l], D, 0, p1, "m1")
                xring = mkring("xin", [128, D], F32, 2, p1)
                tmpb = Buf(sb("tmp1", [128, D], F32, p1))
                xnr = mkring("xnr", [128, D], BF16, 2, p1)
                ssr = mkring("ss", [128, 2], F32, 2, p1)
                src = seq_src(l, sq)
                for j in range(nj):
                    xt = xring.next()
                    K.dma(sp, [lambda: S.dma_start(out=xt.ap[:], in_=src[j * 128:(j + 1) * 128, :])], xt, writes=[xt])
                    xn = xnr.next()
                    ss = ssr.next()
                    rms_mod(xt, Gb, SHb, xn, tmpb, ss)
                    transpose_tile(xn, xnT[:, :, j * 128:(j + 1) * 128], B_xn[j])
            if "XNT" in debug and sq["s"] == debug_seq:
                K.dma(sp, [lambda: S.dma_start(out=DBG["XNT"].rearrange("(kt p) t -> p kt t", p=128), in_=xnT[:])],
                      Buf(), reads=B_xn)

            wring = mkring("wblk", [128, 8, 512], BF16, 4, ph)
            tf = mkring("tf", [128, N], F32, 6, ph)
            tb = mkring("tb", [128, N], BF16, 8, ph)

            def load_wblk(cb):
                wb = wring.next()
                K.dma(pool, [lambda: G.dma_start(out=wb.ap[:], in_=w_in[l][:, cb * 512:(cb + 1) * 512]
                                                 .rearrange("(kt p) n -> p kt n", p=128))], wb, writes=[wb])
                return wb

            def proj_fm(pb, wb, mt, tt):
                mm_group(pb.ap[:, 0:N], [(wb.ap[:, kt, mt * 128:(mt + 1) * 128], xnT[:, kt, tt * N:(tt + 1) * N])
                                         for kt in range(8)],
                         reads=[wb] + B_xn[tt * jpt:(tt + 1) * jpt], pbuf=pb)

            def store(dst, t):
                K.dma(pool, [lambda: G.dma_start(out=dst, in_=t.ap[:])], t, reads=[t])

            wb = load_wblk(5)
            for mt in range(4):
                for tt in range(ntt):
                    pb = ps8.next()
                    proj_fm(pb, wb, mt, tt)
                    cs = tb.next()
                    K.do(act, lambda: A.copy(cs.ap[:], pb.ap[:, 0:N]), reads=[pb], writes=[cs])
                    store(sq["CU"][mt * 128:(mt + 1) * 128, tt * N:(tt + 1) * N], cs)
            if only_cu:
                return

            uT = sb("uT", [128, 4, Ls], BF16, ph)
            B_u = [[Buf() for _ in range(ntt)] for _ in range(4)]
            wb = load_wblk(0)
            for mt in range(4):
                for tt in range(ntt):
                    pb = ps8.next()
                    proj_fm(pb, wb, mt, tt)
                    K.do(act, lambda: A.activation(out=uT[:, mt, tt * N:(tt + 1) * N], in_=pb.ap[:, 0:N],
                                                   func=AF.Gelu_apprx_tanh), reads=[pb], writes=[B_u[mt][tt]])

            wsq = Buf(sb("wsq", [128, 4, 128], BF16, ph))
            wsT = Buf(sb("wsT", [128, 4, 128], BF16, ph))
            sguc = Buf(sb("sguc", [128, 4], F32, ph))
            bsrow = Buf(sb("bsrow", [128, 4, 128], F32, ph))
            K.dma(pool, [lambda: G.dma_start(out=wsq.ap[:], in_=sgu_w[l].rearrange("h q k -> q h k"))], wsq, writes=[wsq])
            load_T(sguc, sguc.ap[:], [(0, 4, sgu_norm_g[l].rearrange("(h p) -> h p", p=128))], 4, 128)
            K.dma(sp, [lambda: S.dma_start(out=bsrow.ap[:].rearrange("p h q -> p (h q)"),
                                           in_=sgu_b[l].rearrange("h q -> (h q)").partition_broadcast(128))],
                  bsrow, writes=[bsrow])
            pb = ps8.next()
            pv = pb.ap[:].bitcast(BF16)
            K.do(pe, lambda: [T.transpose(pv[:, h * 128:(h + 1) * 128], wsq.ap[:, h, :], ident_bf[:])
                              for h in range(4)][-1], reads=[wsq, B_const], writes=[pb])
            K.do(act, lambda: A.copy(wsT.ap[:], pv[:, 0:512].rearrange("p (h q) -> p h q", h=4)),
                 reads=[pb], writes=[wsT])
            yaT = sb("yaT", [128, 4, Ls], BF16, ph)
            B_ya = [Buf() for _ in range(ntt)]
            gvr = mkring("gv", [128, 512], F32, 2, ph)
            vnr = mkring("vn", [128, 512], BF16, 2, ph)
            bnr = mkring("bnst", [128, 8], F32, 2, ph)
            wb = load_wblk(1)
            mixps = None
            for j in range(nj):
                tt, jj = divmod(j, jpt)
                if jj == 0:
                    mixps = [psB.next() for _ in range(4)]
                pb = psA.next()
                mm_group(pb.ap[:, :], [(xnT[:, kt, j * 128:(j + 1) * 128], wb.ap[:, kt, :]) for kt in range(8)],
                         reads=[wb, B_xn[j]], pbuf=pb)
                gv = gvr.next()
                K.do(act, lambda: A.activation(out=gv.ap[:], in_=pb.ap[:], func=AF.Gelu_apprx_tanh),
                     reads=[pb], writes=[gv])
                st = bnr.next()
                K.do(dve, lambda: V.bn_stats(out=st.ap[:, 0:6], in_=gv.ap[:]), reads=[gv], writes=[st])
                K.do(dve, lambda: V.bn_aggr(out=st.ap[:, 6:8], in_=st.ap[:, 0:6]), writes=[st])
                K.do(dve, lambda: V.tensor_scalar(st.ap[:, 7:8], st.ap[:, 7:8], EPS, None, ALU.add), writes=[st])
                K.do(pool, lambda: G.tensor_tensor(st.ap[:, 7:8], st.ap[:, 7:8], neg_half[:], ALU.pow),
                     reads=[B_const], writes=[st])
                vn = vnr.next()
                K.do(dve, lambda: V.tensor_scalar(vn.ap[:], gv.ap[:], st.ap[:, 6:7], st.ap[:, 7:8],
                                                  ALU.subtract, ALU.mult), reads=[gv, st], writes=[vn])
                for h in range(4):
                    mm_group(mixps[h].ap[:, jj * 128:(jj + 1) * 128], [(vn.ap[:, h * 128:(h + 1) * 128], wsT.ap[:, h, :])],
                             reads=[vn, wsT], pbuf=mixps[h], pw=(jj > 0))
                if jj == jpt - 1:
                    for h in range(4):
                        tm = tf.next()
                        bsap = bass.AP(bsrow.ap, h * 128, [[512, 128], [0, jpt], [1, 128]])
                        K.do(dve, lambda: V.scalar_tensor_tensor(
                            out=tm.ap[:].rearrange("p (a q) -> p a q", a=jpt),
                            in0=mixps[h].ap[:, 0:N].rearrange("p (a q) -> p a q", a=jpt),
                            scalar=sguc.ap[:, h:h + 1], in1=bsap, op0=ALU.mult, op1=ALU.add),
                            reads=[mixps[h], sguc, bsrow], writes=[tm])
                        K.do(dve, lambda: V.tensor_tensor(out=yaT[:, h, tt * N:(tt + 1) * N], in0=tm.ap[:],
                                                          in1=uT[:, h, tt * N:(tt + 1) * N], op=ALU.mult),
                             reads=[tm, B_u[h][tt]], pwrites=[B_ya[tt]])

            cw = Buf(sb("cw", [128, 3, 4], F32, ph))
            load_T(cw, cw.ap[:].rearrange("p j m -> p (j m)"), [(0, 12, conv_w[l].rearrange("j (m p) -> (j m) p", p=128))], 12, 128)
            ybT = sb("ybT", [128, 4, Ls], BF16, ph)
            B_yb = [Buf() for _ in range(ntt)]
            wbb, wbc, wbh = load_wblk(2), load_wblk(3), load_wblk(4)
            for mt in range(4):
                for tt in range(ntt):
                    pc_, ph_, pb_ = ps8.next(), ps8.next(), ps8.next()
                    proj_fm(pc_, wbc, mt, tt)
                    proj_fm(ph_, wbh, mt, tt)
                    proj_fm(pb_, wbb, mt, tt)
                    bcs = tf.next()
                    K.do(act, lambda: A.copy(bcs.ap[:], pc_.ap[:, 0:N]), reads=[pc_], writes=[bcs])
                    Pt = tf.next()
                    K.do(dve, lambda: V.tensor_tensor(out=Pt.ap[:], in0=bcs.ap[:], in1=ph_.ap[:, 0:N], op=ALU.mult),
                         reads=[bcs, ph_], writes=[Pt])
                    Tt = tf.next()
                    K.do(dve, lambda: V.tensor_scalar(Tt.ap[:], Pt.ap[:], cw.ap[:, 1, mt:mt + 1], None, ALU.mult),
                         reads=[Pt, cw], writes=[Tt])
                    if grid:
                        Tv = Tt.ap[:].rearrange("p (r c) -> p r c", c=64)
                        Pv = Pt.ap[:].rearrange("p (r c) -> p r c", c=64)
                        o1, i1, o2, i2 = Tv[:, :, 1:64], Pv[:, :, 0:63], Tv[:, :, 0:63], Pv[:, :, 1:64]
                    else:
                        o1, i1, o2, i2 = Tt.ap[:, 1:N], Pt.ap[:, 0:N - 1], Tt.ap[:, 0:N - 1], Pt.ap[:, 1:N]
                    K.do(dve, lambda: V.scalar_tensor_tensor(out=o1, in0=i1, scalar=cw.ap[:, 0, mt:mt + 1], in1=o1,
                                                             op0=ALU.mult, op1=ALU.add), reads=[Pt], writes=[Tt])
                    K.do(dve, lambda: V.scalar_tensor_tensor(out=o2, in0=i2, scalar=cw.ap[:, 2, mt:mt + 1], in1=o2,
                                                             op0=ALU.mult, op1=ALU.add), reads=[Pt], writes=[Tt])
                    K.do(dve, lambda: V.tensor_tensor(out=ybT[:, mt, tt * N:(tt + 1) * N], in0=Tt.ap[:],
                                                      in1=pb_.ap[:, 0:N], op=ALU.mult),
                         reads=[Tt, pb_], pwrites=[B_yb[tt]])

            wbr = [Buf(sb(f"wbr{i}", [128, 4, D], BF16, ph)) for i in range(2)]
            for i in range(2):
                K.dma(pool, [lambda: G.dma_start(out=wbr[i].ap[:], in_=w_branch[l, i]
                                                 .rearrange("(kt p) n -> p kt n", p=128))], wbr[i], writes=[wbr[i]])
            for half in range(2):
                wga, wgb, wgc = load_wblk(6 + half), load_wblk(8 + half), load_wblk(10 + half)
                for dtl in range(4):
                    dt_ = half * 4 + dtl
                    for tt in range(ntt):
                        pga, pgb, pgc, pba, pbb = [ps8.next() for _ in range(5)]
                        proj_fm(pga, wga, dtl, tt)
                        proj_fm(pgb, wgb, dtl, tt)
                        proj_fm(pgc, wgc, dtl, tt)
                        mm_group(pba.ap[:, 0:N], [(wbr[0].ap[:, kt, dt_ * 128:(dt_ + 1) * 128],
                                                   yaT[:, kt, tt * N:(tt + 1) * N]) for kt in range(4)],
                                 reads=[wbr[0], B_ya[tt]], pbuf=pba)
                        mm_group(pbb.ap[:, 0:N], [(wbr[1].ap[:, kt, dt_ * 128:(dt_ + 1) * 128],
                                                   ybT[:, kt, tt * N:(tt + 1) * N]) for kt in range(4)],
                                 reads=[wbr[1], B_yb[tt]], pbuf=pbb)
                        ga, gb, gc = tb.next(), tb.next(), tb.next()
                        for gg, pp in ((ga, pga), (gb, pgb), (gc, pgc)):
                            K.do(act, lambda: A.activation(out=gg.ap[:], in_=pp.ap[:, 0:N], func=AF.Sigmoid),
                                 reads=[pp], writes=[gg])
                        store(sq["GC"][dt_ * 128:(dt_ + 1) * 128, tt * N:(tt + 1) * N], gc)
                        m1, m2 = tf.next(), tf.next()
                        K.do(dve, lambda: V.tensor_tensor(out=m1.ap[:], in0=ga.ap[:], in1=pba.ap[:, 0:N], op=ALU.mult),
                             reads=[ga, pba], writes=[m1])
                        K.do(dve, lambda: V.tensor_tensor(out=m2.ap[:], in0=gb.ap[:], in1=pbb.ap[:, 0:N], op=ALU.mult),
                             reads=[gb, pbb], writes=[m2])
                        mab = tb.next()
                        K.do(pool, lambda: G.tensor_tensor(out=mab.ap[:], in0=m1.ap[:], in1=m2.ap[:], op=ALU.add),
                             reads=[m1, m2], writes=[mab])
                        store(sq["MAB"][dt_ * 128:(dt_ + 1) * 128, tt * N:(tt + 1) * N], mab)

    def phase_s5_tables(l):
        with scope() as po:
            Kacc = Buf(sb("Kacc", [128, 32, 128], F32, po))
            maskf = sb("maskf", [128, 128], F32, po)
            maskb = sb("maskb", [128, 128], F32, po)
            dcol = Buf(sb("dcol", [128, 32], F32, po))
            Bm = Buf()

            def emit_masks():
                G.memset(maskf[:], 0.0)
                G.affine_select(out=maskf[:].rearrange("p (t h) -> p t h", h=16), in_=maskf[:].rearrange("p (t h) -> p t h", h=16),
                                compare_op=ALU.is_ge, fill=1.0, base=-16, pattern=[[-16, 8], [0, 16]], channel_multiplier=1)
                G.memset(maskb[:], 1.0)
                return G.affine_select(out=maskb[:].rearrange("p (t h) -> p t h", h=16), in_=maskb[:].rearrange("p (t h) -> p t h", h=16),
                                       compare_op=ALU.is_ge, fill=0.0, base=0, pattern=[[-16, 8], [0, 16]], channel_multiplier=1)
            K.do(pool, emit_masks, writes=[Bm])
            with scope() as tmpd:
                dstg = Buf(sb("dstg", [32, 8, 16], F32, tmpd))
                K.dma(sp, [lambda k=k: S.dma_start(out=dstg.ap[:, k, :], in_=s5_d[l].rearrange("(g h) -> g h", h=16))
                           for k in range(8)], dstg, writes=[dstg])
                pbd = ps8.next()
                K.do(pe, lambda: T.transpose(pbd.ap[:, 0:32], dstg.ap[:].rearrange("g k h -> g (k h)"), ident_f[0:32, 0:32]),
                     reads=[dstg, B_const], writes=[pbd])
                K.do(act, lambda: A.copy(dcol.ap[:], pbd.ap[:, 0:32]), reads=[pbd], writes=[dcol])
            for d in range(2):
                with scope() as ph:
                    def t(name, shape, dt=F32):
                        return Buf(sb(name, shape, dt, ph))
                    lre, lim, dtb = t("lre", [64, 32]), t("lim", [64, 32]), t("dtb", [64, 32])
                    load_T(lre, lre.ap[:], [(0, 32, s5_lam_re[l, d])], 32, 64)
                    load_T(lim, lim.ap[:], [(0, 32, s5_lam_im[l, d])], 32, 64)
                    K.dma(sp, [lambda: S.dma_start(out=dtb.ap[:], in_=s5_log_dt[l, d].partition_broadcast(64))],
                          dtb, writes=[dtb])
                    K.do(act, lambda: A.activation(out=dtb.ap[:], in_=dtb.ap[:], func=AF.Exp), writes=[dtb])
                    X, ANG = t("X", [64, 32]), t("ANG", [64, 32])
                    K.do(dve, lambda: V.tensor_tensor(out=X.ap[:], in0=lre.ap[:], in1=dtb.ap[:], op=ALU.mult),
                         reads=[lre, dtb], writes=[X])
                    K.do(dve, lambda: V.tensor_tensor(out=ANG.ap[:], in0=lim.ap[:], in1=dtb.ap[:], op=ALU.mult),
                         reads=[lim, dtb], writes=[ANG])
                    EV = t("EV", [64, 16])
                    K.do(pool, lambda: [G.memset(EV.ap[:, i:i + 1], float(i - 7)) for i in range(16)][-1], writes=[EV])

                    def outer(dst, src):
                        in0 = bass.AP(src.ap, 0, [[32, 64], [1, 32], [0, 16]])
                        in1 = bass.AP(EV.ap, 0, [[16, 64], [0, 32], [1, 16]])
                        K.do(dve, lambda: V.tensor_tensor(out=dst.ap[:], in0=in0, in1=in1, op=ALU.mult),
                             reads=[src, EV], writes=[dst])
                    XE, AE = t("XE", [64, 32, 16]), t("AE", [64, 32, 16])
                    outer(XE, X)
                    outer(AE, ANG)
                    MAG = t("MAG", [64, 32, 16])
                    K.do(act, lambda: A.activation(out=MAG.ap[:], in_=XE.ap[:], func=AF.Exp), reads=[XE], writes=[MAG])
                    PRE, PIM = t("PRE", [64, 32, 16]), t("PIM", [64, 32, 16])
                    YI = t("YI", [64, 32, 16], I32)
                    YF, FR, MK = t("YF", [64, 32, 16]), t("FR", [64, 32, 16]), t("MK", [64, 32, 16])
                    for (dst, off) in ((PIM, 64.0), (PRE, 64.25)):
                        K.do(dve, lambda: V.tensor_scalar(FR.ap[:], AE.ap[:], 1.0 / TWO_PI, off, ALU.mult, ALU.add),
                             reads=[AE], writes=[FR])
                        K.do(dve, lambda: V.tensor_copy(YI.ap[:], FR.ap[:]), reads=[FR], writes=[YI])
                        K.do(dve, lambda: V.tensor_copy(YF.ap[:], YI.ap[:]), reads=[YI], writes=[YF])
                        K.do(dve, lambda: V.tensor_tensor(out=FR.ap[:], in0=FR.ap[:], in1=YF.ap[:], op=ALU.subtract),
                             reads=[YF], writes=[FR])
                        K.do(dve, lambda: V.tensor_scalar(MK.ap[:], FR.ap[:], 0.5, None, ALU.is_gt), reads=[FR], writes=[MK])
                        K.do(dve, lambda: V.tensor_tensor(out=FR.ap[:], in0=FR.ap[:], in1=MK.ap[:], op=ALU.subtract),
                             reads=[MK], writes=[FR])
                        K.do(dve, lambda: V.tensor_scalar(FR.ap[:], FR.ap[:], -0.5, 0.5, ALU.max, ALU.min), writes=[FR])
                        K.do(act, lambda: A.activation(out=dst.ap[:], in_=FR.ap[:], func=AF.Sin, scale=TWO_PI),
                             reads=[FR], writes=[dst])
                        K.do(dve, lambda: V.tensor_tensor(out=dst.ap[:], in0=dst.ap[:], in1=MAG.ap[:], op=ALU.mult),
                             reads=[MAG], writes=[dst])
                    ld = l * 2 + d
                    K.do(dve, lambda: [V.tensor_copy(a8_ar[:, ld, 0, :], PRE.ap[:, :, 15]),
                                       V.tensor_copy(a8_ar[:, ld, 1, :], PRE.ap[:, :, 15]),
                                       V.tensor_scalar(a8_ai[:, ld, 0, :], PIM.ap[:, :, 15], -1.0, None, ALU.mult),
                                       V.tensor_copy(a8_ai[:, ld, 1, :], PIM.ap[:, :, 15])][-1],
                         reads=[PRE, PIM], pwrites=[B_a8])
                    den, nr, fre, fim, t1, t2 = (t(n, [64, 32]) for n in ("den", "nr", "fre", "fim", "t1s", "t2s"))

                    def tt_(out, a, b, op, rd=(), wr=()):
                        K.do(dve, lambda: V.tensor_tensor(out=out, in0=a, in1=b, op=op), reads=list(rd), writes=list(wr))
                    tt_(den.ap[:], lre.ap[:], lre.ap[:], ALU.mult, [lre], [den])
                    tt_(t1.ap[:], lim.ap[:], lim.ap[:], ALU.mult, [lim], [t1])
                    tt_(den.ap[:], den.ap[:], t1.ap[:], ALU.add, [t1], [den])
                    K.do(dve, lambda: V.reciprocal(den.ap[:], den.ap[:]), writes=[den])
                    K.do(dve, lambda: V.tensor_scalar(nr.ap[:], PRE.ap[:, :, 8], -1.0, None, ALU.add), reads=[PRE], writes=[nr])
                    tt_(fre.ap[:], nr.ap[:], lre.ap[:], ALU.mult, [nr, lre], [fre])
                    tt_(t1.ap[:], PIM.ap[:, :, 8], lim.ap[:], ALU.mult, [PIM, lim], [t1])
                    tt_(fre.ap[:], fre.ap[:], t1.ap[:], ALU.add, [t1], [fre])
                    tt_(fre.ap[:], fre.ap[:], den.ap[:], ALU.mult, [den], [fre])
                    tt_(fim.ap[:], PIM.ap[:, :, 8], lre.ap[:], ALU.mult, [PIM, lre], [fim])
                    tt_(t2.ap[:], nr.ap[:], lim.ap[:], ALU.mult, [nr, lim], [t2])
                    tt_(fim.ap[:], fim.ap[:], t2.ap[:], ALU.subtract, [t2], [fim])
                    tt_(fim.ap[:], fim.ap[:], den.ap[:], ALU.mult, [den], [fim])
                    Bre, Bim = t("Bre", [64, 32, 16]), t("Bim", [64, 32, 16])
                    K.dma(sp, [lambda: S.dma_start(out=Bre.ap[:], in_=s5_b_re[l, d].rearrange("g p h -> p g h"))], Bre, writes=[Bre])
                    K.dma(sp, [lambda: S.dma_start(out=Bim.ap[:], in_=s5_b_im[l, d].rearrange("g p h -> p g h"))], Bim, writes=[Bim])
                    BBre, BBim, tq = t("BBre", [64, 32, 16]), t("BBim", [64, 32, 16]), t("tq", [64, 32, 16])

                    def fb(fsrc):
                        return bass.AP(fsrc.ap, 0, [[32, 64], [1, 32], [0, 16]])

                    def cmul(ore, oim, are, aim, bre, bim, tmp, rd):
                        tt_(ore[1], are, bre, ALU.mult, rd, [ore[0]])
                        tt_(tmp[1], aim, bim, ALU.mult, rd, [tmp[0]])
                        tt_(ore[1], ore[1], tmp[1], ALU.subtract, [tmp[0]], [ore[0]])
                        tt_(oim[1], are, bim, ALU.mult, rd, [oim[0]])
                        tt_(tmp[1], aim, bre, ALU.mult, rd, [tmp[0]])
                        tt_(oim[1], oim[1], tmp[1], ALU.add, [tmp[0]], [oim[0]])
                    cmul((BBre, BBre.ap[:]), (BBim, BBim.ap[:]), fb(fre), fb(fim), Bre.ap[:], Bim.ap[:], (tq, tq.ap[:]),
                         [fre, fim, Bre, Bim])
                    CTre, CTim = t("CTre", [64, 512]), t("CTim", [64, 512])
                    cin = t("cin", [128, 4, 64])
                    for (csrc, cdst) in ((s5_c_re, CTre), (s5_c_im, CTim)):
                        K.dma(sp, [lambda: S.dma_start(out=cin.ap[:], in_=csrc[l, d].rearrange("g h p -> (g h) p")
                                                       .rearrange("(a r) p -> r a p", r=128))], cin, writes=[cin])
                        pb = ps8.next()
                        K.do(pe, lambda: [T.transpose(pb.ap[0:64, a * 128:(a + 1) * 128], cin.ap[:, a, :], ident_f[:])
                                          for a in range(4)][-1], reads=[cin, B_const], writes=[pb])
                        K.do(act, lambda: A.copy(cdst.ap[:], pb.ap[0:64, :]), reads=[pb], writes=[cdst])
                    W1Tre, W1Tim, tw = t("W1Tre", [64, 32, 8, 16]), t("W1Tim", [64, 32, 8, 16]), t("tw", [64, 32, 8, 16])

                    def pview(Psrc, start, step):
                        return bass.AP(Psrc.ap, start, [[512, 64], [16, 32], [step, 8], [0, 16]])

                    def bview(Bsrc):
                        return bass.AP(Bsrc.ap, 0, [[512, 64], [16, 32], [0, 8], [1, 16]])
                    s1, st1 = (14, -1) if d == 0 else (7, 1)
                    cmul((W1Tre, W1Tre.ap[:]), (W1Tim, W1Tim.ap[:]), pview(PRE, s1, st1), pview(PIM, s1, st1),
                         bview(BBre), bview(BBim), (tw, tw.ap[:]), [PRE, PIM, BBre, BBim])
                    W2f = t("W2f", [64, 32, 2, 8, 16])
                    W2m = t("W2m", [64, 32, 2, 8, 16])

                    def w2build(dst, s2, st2):
                        ore = bass.AP(dst.ap, 0, [[8192, 64], [256, 32], [16, 8], [1, 16]])
                        oim = bass.AP(dst.ap, 128, [[8192, 64], [256, 32], [16, 8], [1, 16]])
                        cmul((dst, ore), (dst, oim), pview(PRE, s2, st2), pview(PIM, s2, st2),
                             bview(CTre), bview(CTim), (tw, tw.ap[:]), [PRE, PIM, CTre, CTim])
                        K.do(dve, lambda: V.tensor_scalar(oim, oim, -1.0, None, ALU.mult), writes=[dst])
                    s2, st2 = (8, 1) if d == 0 else (15, -1)
                    w2build(W2f, s2, st2)
                    s3, st3 = (0, 1) if d == 0 else (7, -1)
                    w2build(W2m, s3, st3)
                    W2b = t("W2b", [64, 32, 2, 128], BF16)
                    K.do(act, lambda: A.copy(W2b.ap[:].rearrange("p g r x -> p (g r x)"),
                                             W2f.ap[:].rearrange("p g r t h -> p (g r t h)")), reads=[W2f], writes=[W2b])
                    K.dma(sp, [lambda: S.dma_start(out=W2D[l, d], in_=W2b.ap[:])], W2b, reads=[W2b])
                    mask = maskf if d == 0 else maskb
                    tkr = mkring("tk", [128, 4, 128], F32, 2, ph)
                    for g4 in range(8):
                        pb = ps8.next()
                        for gg in range(4):
                            g = g4 * 4 + gg
                            mm_group(pb.ap[:, gg * 128:(gg + 1) * 128],
                                     [(W1Tre.ap[:, g].rearrange("p k h -> p (k h)"), W2m.ap[:, g, 0].rearrange("p t h -> p (t h)")),
                                      (W1Tim.ap[:, g].rearrange("p k h -> p (k h)"), W2m.ap[:, g, 1].rearrange("p t h -> p (t h)"))],
                                     reads=[W1Tre, W1Tim, W2m], pbuf=pb, pw=(gg > 0))
                        mk = bass.AP(mask, 0, [[128, 128], [0, 4], [1, 128]])
                        kv = Kacc.ap[:, g4 * 4:(g4 + 1) * 4, :]
                        pv3 = pb.ap[:].rearrange("p (a x) -> p a x", a=4)
                        if d == 0:
                            K.do(dve, lambda: V.tensor_tensor(out=kv, in0=pv3, in1=mk, op=ALU.mult),
                                 reads=[pb, Bm], pwrites=[Kacc])
                        else:
                            tk = tkr.next()
                            K.do(dve, lambda: V.tensor_tensor(out=tk.ap[:], in0=pv3, in1=mk, op=ALU.mult),
                                 reads=[pb, Bm], writes=[tk])
                            K.do(dve, lambda: V.tensor_tensor(out=kv, in0=kv, in1=tk.ap[:], op=ALU.add),
                                 reads=[tk, Kacc], pwrites=[Kacc])
                    W1b = t("W1b", [128, 32, 128], BF16)
                    for g4 in range(8):
                        pb = ps8.next()

                        def emit_tr():
                            ins = None
                            for gg in range(4):
                                g = g4 * 4 + gg
                                for ri, src in enumerate((W1Tre, W1Tim)):
                                    ins = T.transpose(pb.ap[:, gg * 128 + ri * 64: gg * 128 + ri * 64 + 64],
                                                      src.ap[:, g].rearrange("p k h -> p (k h)"), ident_f[0:64, 0:64])
                            return ins
                        K.do(pe, emit_tr, reads=[W1Tre, W1Tim, B_const], writes=[pb])
                        K.do(act, lambda: A.copy(W1b.ap[:, g4 * 4:(g4 + 1) * 4, :], pb.ap[:].rearrange("p (a x) -> p a x", a=4)),
                             reads=[pb], pwrites=[W1b])
                    K.dma(sp, [lambda: S.dma_start(out=W1D[l, d], in_=W1b.ap[:])], W1b, reads=[W1b])
            Kb = Buf(sb("Kb", [128, 32, 128], BF16, po))
            for g in range(32):
                K.do(dve, lambda: V.scalar_tensor_tensor(out=Kacc.ap[:, g, :], in0=ident_f[:], scalar=dcol.ap[:, g:g + 1],
                                                         in1=Kacc.ap[:, g, :], op0=ALU.mult, op1=ALU.add),
                     reads=[B_const, dcol, Kacc], pwrites=[Kacc])
            K.do(act, lambda: A.copy(Kb.ap[:], Kacc.ap[:]), reads=[Kacc], writes=[Kb])
            K.dma(sp, [lambda: S.dma_start(out=KMD[l], in_=Kb.ap[:])], Kb, reads=[Kb])

    def phase_s5(l, sq):
        Ls = sq["L"]
        C = Ls // 8
        b = sq["b"]
        with scope() as ph:
            U = sb("U", [128, 32, C], BF16, ph)
            B_U = Buf()
            EBm = sb("EBm", [64, 2, C, 2, 32], BF16, ph)
            EB = [EBm[:, d] for d in range(2)]
            B_E = [Buf(), Buf()]
            B_H = [Buf(), Buf()]
            with scope() as pa:
                cuT = Buf(sb("cuT", [128, 4, Ls], BF16, pa))
                W1 = Buf(sb("W1", [128, 2, 32, 128], BF16, pa))
                SEL = Buf(sb("SEL", [128, 8, 8, 128], BF16, pa))
                K.dma(sp, [lambda: S.dma_start(out=cuT.ap[:], in_=sq["CU"].rearrange("(m p) t -> p m t", p=128))], cuT, writes=[cuT])
                K.dma(sp, [lambda d=d: S.dma_start(out=W1.ap[:, d], in_=W1D[l, d]) for d in range(2)], W1, writes=[W1])
                K.dma(sp, [lambda: S.dma_start(out=SEL.ap[:], in_=SELD)], SEL, writes=[SEL])
                for g in range(32):
                    mt, gl = divmod(g, 8)
                    pb = ps8.next()
                    mm_group(pb.ap[:, 0:C], [(SEL.ap[:, gl, k, :], cuT.ap[:, mt, k::8]) for k in range(8)],
                             reads=[SEL, cuT], pbuf=pb)
                    if g % 2 == 0:
                        K.do(act, lambda: A.copy(U[:, g, :], pb.ap[:, 0:C]), reads=[pb], pwrites=[B_U])
                    else:
                        K.do(dve, lambda: V.tensor_copy(U[:, g, :], pb.ap[:, 0:C]), reads=[pb], pwrites=[B_U])
                for d in range(2):
                    for g in range(32):
                        pb = ps8.next()
                        mm_group(pb.ap[:, 0:C], [(W1.ap[:, d, g, :], U[:, g, :])], reads=[W1, B_U], pbuf=pb)
                        K.do(act, lambda: A.copy(EB[d][:, :, 0, g], pb.ap[0:64, 0:C]), reads=[pb], pwrites=[B_E[d]])
                        K.do(dve, lambda: V.tensor_copy(EB[d][:, :, 1, g], pb.ap[64:128, 0:C]), reads=[pb], pwrites=[B_E[d]])
            with scope() as pscan:
                sring = mkring("sst", [64, 128], F32, 4, pscan)
                t1 = Buf(sb("sc_t1", [64, 128], F32, pscan))
                t2 = Buf(sb("sc_t2", [64, 128], F32, pscan))
                ar = a8_ar[:, 2 * l:2 * l + 2].rearrange("p d a g -> p (d a g)")
                ai4 = a8_ai[:, 2 * l:2 * l + 2]
                h0v = h0st[:, 2 * b:2 * b + 2].rearrange("p d a g -> p (d a g)")
                sprev = sring.next()
                if sq["ctx"]:
                    K.do(dve, lambda: V.memset(sprev.ap[:], 0.0), writes=[sprev])
                else:
                    K.do(dve, lambda: V.tensor_copy(sprev.ap[:], h0v), reads=[B_h0], writes=[sprev])
                for i in range(C):
                    snew = sring.next()
                    ev_ap = bass.AP(EBm, i * 64, [[2 * C * 64, 64], [(2 * C - 1 - 2 * i) * 64, 2], [1, 64]])
                    swp = bass.AP(sprev.ap, 32, [[128, 64], [64, 2], [-32, 2], [1, 32]])

                    def step():
                        V.tensor_tensor(out=t1.ap[:], in0=sprev.ap[:], in1=ar, op=ALU.mult)
                        V.tensor_tensor(out=t2.ap[:].rearrange("p (d a g) -> p d a g", d=2, a=2), in0=swp, in1=ai4, op=ALU.mult)
                        V.tensor_tensor(out=t1.ap[:], in0=t1.ap[:], in1=t2.ap[:], op=ALU.add)
                        return V.tensor_tensor(out=snew.ap[:].rearrange("p (d x) -> p d x", d=2),
                                               in0=t1.ap[:].rearrange("p (d x) -> p d x", d=2), in1=ev_ap, op=ALU.add)
                    ev4 = K.do(dve, step, reads=[sprev, B_a8, B_E[0], B_E[1]], writes=[snew, t1, t2])
                    K.do(act, lambda: A.copy(ev_ap, sprev.ap[:].rearrange("p (d x) -> p d x", d=2)),
                         reads=[sprev], pwrites=[B_H[0], B_H[1]], after=[ev4])
                    sprev = snew
                if sq["ctx"]:
                    K.do(dve, lambda: V.tensor_copy(h0v, sprev.ap[:]), reads=[sprev], pwrites=[B_h0])
            with scope() as rd:
                W2 = Buf(sb("W2", [64, 2, 32, 2, 128], BF16, rd))
                KM = Buf(sb("KM", [128, 32, 128], BF16, rd))
                SELT = Buf(sb("SELT", [128, 8, 8, 128], BF16, rd))
                Y = sb("Y", [128, 32, C], BF16, rd)
                B_Y = Buf()
                zT = Buf(sb("zT", [128, 4, Ls], BF16, rd))
                K.dma(sp, [lambda d=d: S.dma_start(out=W2.ap[:, d], in_=W2D[l, d]) for d in range(2)], W2, writes=[W2])
                K.dma(sp, [lambda: S.dma_start(out=KM.ap[:], in_=KMD[l])], KM, writes=[KM])
                K.dma(sp, [lambda: S.dma_start(out=SELT.ap[:], in_=SELTD)], SELT, writes=[SELT])
                for g in range(32):
                    pb = ps8.next()
                    pairs = [(KM.ap[:, g, :], U[:, g, :])]
                    for d in range(2):
                        for ri in range(2):
                            pairs.append((W2.ap[:, d, g, ri, :], EB[d][:, :, ri, g]))
                    mm_group(pb.ap[:, 0:C], pairs, reads=[KM, W2, B_U, B_H[0], B_H[1]], pbuf=pb)
                    if g % 2 == 0:
                        K.do(act, lambda: A.copy(Y[:, g, :], pb.ap[:, 0:C]), reads=[pb], pwrites=[B_Y])
                    else:
                        K.do(dve, lambda: V.tensor_copy(Y[:, g, :], pb.ap[:, 0:C]), reads=[pb], pwrites=[B_Y])
                for mt in range(4):
                    for tau in range(8):
                        pb = ps8.next()
                        mm_group(pb.ap[:, 0:C], [(SELT.ap[:, gl, tau, :], Y[:, mt * 8 + gl, :]) for gl in range(8)],
                                 reads=[SELT, B_Y], pbuf=pb)
                        K.do(act, lambda: A.activation(out=zT.ap[:, mt, tau::8], in_=pb.ap[:, 0:C], func=AF.Gelu_apprx_tanh),
                             reads=[pb], pwrites=[zT])
                K.dma(sp, [lambda: S.dma_start(out=sq["YC"].rearrange("(m p) t -> p m t", p=128), in_=zT.ap[:])],
                      zT, reads=[zT])

    def phase_back(l, sq):
        Ls = sq["L"]
        N = min(512, Ls)
        ntt = Ls // N
        jpt = N // 128
        src = seq_src(l, sq)
        with scope() as ph:
            G1b = Buf(sb("g1b", [128, D], F32, ph))
            K.dma(sp, [lambda: S.dma_start(out=G1b.ap[:], in_=MOD[l, sq["modrow"], 2 * D:3 * D].partition_broadcast(128))],
                  G1b, writes=[G1b])
            G2b, SH2b = make_mod_tiles(l, sq["modrow"], norm2_g[l], 4 * D, 3 * D, ph, "m2")
            gluw = Buf(sb("gluw", [128, 4, WM], BF16, ph))
            wbr2 = Buf(sb("wbr2", [128, 4, D], BF16, ph))
            wout = Buf(sb("wout", [128, 8, D], BF16, ph))
            glub = Buf(sb("glub", [128, 4], F32, ph))
            K.dma(pool, [lambda: G.dma_start(out=gluw.ap[:], in_=glu_w[l].rearrange("(kt p) n -> p kt n", p=128))], gluw, writes=[gluw])
            K.dma(pool, [lambda: G.dma_start(out=wbr2.ap[:], in_=w_branch[l, 2].rearrange("(kt p) n -> p kt n", p=128))], wbr2, writes=[wbr2])
            K.dma(pool, [lambda: G.dma_start(out=wout.ap[:], in_=w_out[l].rearrange("(kt p) n -> p kt n", p=128))], wout, writes=[wout])
            load_T(glub, glub.ap[:], [(0, 4, glu_b[l].rearrange("(m p) -> m p", p=128))], 4, 128)
            zr = mkring("zin", [128, 4, N], BF16, 2, ph)
            mabr = mkring("mabin", [128, 8, N], BF16, 2, ph)
            gcr = mkring("gcin", [128, 8, N], BF16, 2, ph)
            ycr = mkring("ycT", [128, 4, N], BF16, 2, ph)
            mgr = mkring("mgT", [128, 8, N], BF16, 2, ph)
            tf = mkring("tfb", [128, N], F32, 3, ph)
            tb = mkring("tbb", [128, N], BF16, 3, ph)
            hr = mkring("hin", [128, D], F32, 2, ph)
            hnr = mkring("hn", [128, D], F32, 2, ph)
            tmpb = Buf(sb("tmp2", [128, D], F32, ph))
            xnr = mkring("xn2", [128, D], BF16, 2, ph)
            ssr = mkring("ss2", [128, 2], F32, 2, ph)
            xstr = mkring("xst", [128, 8, N], BF16, 2, ph)
            for tt in range(ntt):
                ts = slice(tt * N, (tt + 1) * N)
                zt, mabt, gct = zr.next(), mabr.next(), gcr.next()
                K.dma(sp, [lambda: S.dma_start(out=zt.ap[:], in_=sq["YC"][:, ts].rearrange("(m p) t -> p m t", p=128))], zt, writes=[zt])
                K.dma(sp, [lambda: S.dma_start(out=mabt.ap[:], in_=sq["MAB"][:, ts].rearrange("(m p) t -> p m t", p=128))], mabt, writes=[mabt])
                K.dma(sp, [lambda: S.dma_start(out=gct.ap[:], in_=sq["GC"][:, ts].rearrange("(m p) t -> p m t", p=128))], gct, writes=[gct])
                yc = ycr.next()
                for mt in range(4):
                    pb = ps8.next()
                    mm_group(pb.ap[:, 0:N], [(gluw.ap[:, kt, mt * 128:(mt + 1) * 128], zt.ap[:, kt, :]) for kt in range(4)],
                             reads=[gluw, zt], pbuf=pb)
                    sg = tb.next()
                    K.do(act, lambda: A.activation(out=sg.ap[:], in_=pb.ap[:, 0:N], func=AF.Sigmoid, bias=glub.ap[:, mt:mt + 1]),
                         reads=[pb, glub], writes=[sg])
                    K.do(dve, lambda: V.tensor_tensor(out=yc.ap[:, mt, :], in0=zt.ap[:, mt, :], in1=sg.ap[:], op=ALU.mult),
                         reads=[zt, sg], **(dict(writes=[yc]) if mt == 0 else dict(pwrites=[yc])))
                mg = mgr.next()
                for dt_ in range(8):
                    pb = ps8.next()
                    mm_group(pb.ap[:, 0:N], [(wbr2.ap[:, kt, dt_ * 128:(dt_ + 1) * 128], yc.ap[:, kt, :]) for kt in range(4)],
                             reads=[wbr2, yc], pbuf=pb)
                    m = tf.next()
                    K.do(dve, lambda: V.tensor_tensor(out=m.ap[:], in0=gct.ap[:, dt_, :], in1=pb.ap[:, 0:N], op=ALU.mult),
                         reads=[gct, pb], writes=[m])
                    K.do(pool, lambda: G.tensor_tensor(out=mg.ap[:, dt_, :], in0=m.ap[:], in1=mabt.ap[:, dt_, :], op=ALU.add),
                         reads=[m, mabt], **(dict(writes=[mg]) if dt_ == 0 else dict(pwrites=[mg])))
                xst = xstr.next()
                for jj in range(jpt):
                    r0 = tt * N + jj * 128
                    ht = hr.next()
                    K.dma(sp, [lambda: S.dma_start(out=ht.ap[:], in_=src[r0:r0 + 128, :])], ht, writes=[ht])
                    hn = hnr.next()
                    for half in range(2):
                        pb = ps8.next()
                        mm_group(pb.ap[:, :], [(mg.ap[:, kt, jj * 128:(jj + 1) * 128], wout.ap[:, kt, half * 512:(half + 1) * 512])
                                               for kt in range(8)], reads=[mg, wout], pbuf=pb)
                        hs = slice(half * 512, (half + 1) * 512)
                        K.do(dve, lambda: V.tensor_tensor(out=hn.ap[:, hs], in0=pb.ap[:, :], in1=G1b.ap[:, hs], op=ALU.mult),
                             reads=[pb, G1b], **(dict(writes=[hn]) if half == 0 else dict(pwrites=[hn])))
                    K.do(dve, lambda: V.tensor_tensor(out=hn.ap[:], in0=hn.ap[:], in1=ht.ap[:], op=ALU.add),
                         reads=[ht], writes=[hn])
                    K.dma(pool, [lambda: G.dma_start(out=sq["HA"][r0:r0 + 128, :], in_=hn.ap[:])], hn, reads=[hn])
                    xn = xnr.next()
                    ss = ssr.next()
                    rms_mod(hn, G2b, SH2b, xn, tmpb, ss)
                    K.dma(pool, [lambda: G.dma_start(out=sq["XN2R"][r0:r0 + 128, :], in_=xn.ap[:])], xn, reads=[xn])
                    transpose_tile(xn, xst.ap[:, :, jj * 128:(jj + 1) * 128], xst, pw=(jj > 0))
                K.dma(pool, [lambda: G.dma_start(out=sq["XN2T"][:, ts].rearrange("(k p) t -> p k t", p=128), in_=xst.ap[:])],
                      xst, reads=[xst])

    def phase_moe_sparse(l, seq_list):
        last = (l == DEPTH - 1)
        blocks = []
        ctxs = [sq for sq in seq_list if sq["ctx"]]
        if ctxs and not last:
            blocks.append([(sq, 0, LC) for sq in ctxs])
        for sq in seq_list:
            if not sq["ctx"]:
                for tt in range(LS // 512):
                    blocks.append([(sq, tt * 512, 512)])
        tiles = []
        for blk in blocks:
            for (sq, t0, n) in blk:
                for jj in range(n // 128):
                    tiles.append((sq, t0 + jj * 128))
        NJ = len(tiles)
        TL = (2 * NJ * 128 + NE * 511) // 512
        assert TL <= TMAX

        def bl(ap_t, off, pstride, dims):
            return bass.AP(ap_t, off, [[pstride, 128]] + dims)

        with scope() as ph:
            Q1 = Buf(sb("Q1", [128, NJ, NE], F32, ph))
            Q2 = Buf(sb("Q2", [128, NJ, NE], F32, ph))
            GW = Buf(sb("GW", [128, NJ, NE], F32, ph))
            slot_i = Buf(sb("slot_i", [128, 2, NJ], I32, ph))
            wsel = Buf(sb("wsel", [128, 2, NJ], F32, ph))
            idxW = Buf(sb("idxW", [128, TL], I32, ph))
            with scope() as p1:
                rw = Buf(sb("rw", [128, 8, NE], BF16, p1))
                rbb = Buf(sb("rbb", [128, 4, NE], F32, p1))
                K.dma(pool, [lambda: G.dma_start(out=rw.ap[:], in_=router_w.rearrange("(kt p) e -> p kt e", p=128))], rw, writes=[rw])
                K.dma(sp, [lambda j=j: S.dma_start(out=rbb.ap[:, j, :], in_=router_b.partition_broadcast(128)) for j in range(4)],
                      rbb, writes=[rbb])
                xTr = mkring("xT", [128, 8, 512], BF16, 2, p1)
                sc = Buf(sb("r_sc", [128, 4, NE], F32, p1))
                bb = Buf(sb("r_b", [128, 4, NE], F32, p1))
                b2 = Buf(sb("r_b2", [128, 4, NE], F32, p1))
                q1 = Buf(sb("r_q1", [128, 4, NE], F32, p1))
                m16 = Buf(sb("r_m16", [128, 16], F32, p1))
                m16b = Buf(sb("r_m16b", [128, 16], F32, p1))
                m4 = Buf(sb("r_m4", [128, 4], F32, p1))
                j0 = 0
                for blk in blocks:
                    xT = xTr.next()
                    off = 0
                    ems = []
                    for (sq, t0, n) in blk:
                        ems.append(lambda sq=sq, t0=t0, n=n, off=off: S.dma_start(
                            out=xT.ap[:, :, off:off + n], in_=sq["XN2T"][:, t0:t0 + n].rearrange("(k p) t -> p k t", p=128)))
                        off += n
                    nt = off // 128
                    K.dma(sp, ems, xT, writes=[xT])
                    pr = ps8.next()
                    for j in range(nt):
                        mm_group(pr.ap[:, j * NE:(j + 1) * NE], [(xT.ap[:, kt, j * 128:(j + 1) * 128], rw.ap[:, kt, :]) for kt in range(8)],
                                 reads=[xT, rw], pbuf=pr, pw=(j > 0))
                    if nt < 4:
                        K.do(dve, lambda: V.memset(sc.ap[:], 0.0), writes=[sc])
                    K.do(act, lambda: A.activation(out=sc.ap[:, 0:nt, :].rearrange("p j e -> p (j e)"), in_=pr.ap[:, 0:nt * NE], func=AF.Sigmoid),
                         reads=[pr], **(dict(writes=[sc]) if nt == 4 else dict(pwrites=[sc])))

                    def vv(fn, rd, wr, pw=()):
                        K.do(dve, fn, reads=rd, writes=wr, pwrites=pw)
                    flat = lambda t_: t_.ap[:].rearrange("p j e -> p (j e)")
                    g44 = lambda t_: t_.ap[:].rearrange("p j (g e) -> p (j g) e", e=4)
                    vv(lambda: V.tensor_tensor(out=flat(bb), in0=flat(sc), in1=flat(rbb), op=ALU.add), [sc, rbb], [bb])
                    vv(lambda: V.tensor_reduce(out=m16.ap[:], in_=g44(bb), axis=AX.X, op=ALU.max), [bb], [m16])
                    m16bc = bl(m16.ap, 0, 16, [[1, 16], [0, 4]])
                    vv(lambda: V.tensor_tensor(out=g44(q1), in0=g44(bb), in1=m16bc, op=ALU.is_equal), [bb, m16], [q1])
                    vv(lambda: V.scalar_tensor_tensor(out=flat(b2), in0=flat(q1), scalar=-BIG, in1=flat(bb), op0=ALU.mult, op1=ALU.add),
                       [q1, bb], [b2])
                    vv(lambda: V.tensor_reduce(out=m16b.ap[:], in_=g44(b2), axis=AX.X, op=ALU.max), [b2], [m16b])
                    vv(lambda: V.tensor_tensor(out=m16.ap[:], in0=m16.ap[:], in1=m16b.ap[:], op=ALU.add), [m16b], [m16])
                    vv(lambda: V.tensor_reduce(out=m4.ap[:], in_=m16.ap[:].rearrange("p (j g) -> p j g", g=4), axis=AX.X, op=ALU.max),
                       [m16], [m4])
                    m4bc = bl(m4.ap, 0, 4, [[1, 4], [0, 4]])
                    vv(lambda: V.tensor_tensor(out=m16b.ap[:].rearrange("p (j g) -> p j g", g=4),
                                               in0=m16.ap[:].rearrange("p (j g) -> p j g", g=4), in1=m4bc, op=ALU.is_equal),
                       [m16, m4], [m16b])
                    vv(lambda: V.tensor_scalar(m16b.ap[:], m16b.ap[:], -1.0, BIG, ALU.add, ALU.mult), [], [m16b])
                    penbc = bl(m16b.ap, 0, 16, [[1, 16], [0, 4]])
                    vv(lambda: V.tensor_tensor(out=g44(b2), in0=g44(bb), in1=penbc, op=ALU.add), [bb, m16b], [b2])
                    vv(lambda: V.tensor_reduce(out=m4.ap[:], in_=b2.ap[:], axis=AX.X, op=ALU.max), [b2], [m4])
                    m4e = bl(m4.ap, 0, 4, [[1, 4], [0, NE]])
                    vv(lambda: V.tensor_tensor(out=q1.ap[:], in0=b2.ap[:], in1=m4e, op=ALU.is_equal), [b2, m4], [q1])
                    vv(lambda: V.tensor_copy(Q1.ap[:, j0:j0 + nt, :], q1.ap[:, 0:nt, :]), [q1], [], [Q1])
                    vv(lambda: V.scalar_tensor_tensor(out=flat(b2), in0=flat(q1), scalar=-BIG, in1=flat(b2), op0=ALU.mult, op1=ALU.add),
                       [q1], [b2])
                    vv(lambda: V.tensor_reduce(out=m4.ap[:], in_=b2.ap[:], axis=AX.X, op=ALU.max), [b2], [m4])
                    vv(lambda: V.tensor_tensor(out=bb.ap[:], in0=b2.ap[:], in1=m4e, op=ALU.is_equal), [b2, m4], [bb])
                    vv(lambda: V.tensor_copy(Q2.ap[:, j0:j0 + nt, :], bb.ap[:, 0:nt, :]), [bb], [], [Q2])
                    vv(lambda: V.tensor_tensor(out=flat(q1), in0=flat(q1), in1=flat(bb), op=ALU.add), [bb], [q1])
                    vv(lambda: V.tensor_tensor(out=flat(q1), in0=flat(q1), in1=flat(sc), op=ALU.mult), [sc], [q1])
                    vv(lambda: V.tensor_reduce(out=m4.ap[:], in_=q1.ap[:], axis=AX.X, op=ALU.add), [q1], [m4])
                    vv(lambda: V.tensor_scalar(m4.ap[:], m4.ap[:], 1e-30, None, ALU.max), [], [m4])
                    vv(lambda: V.reciprocal(m4.ap[:], m4.ap[:]), [], [m4])
                    vv(lambda: V.tensor_tensor(out=GW.ap[:, j0:j0 + nt, :], in0=q1.ap[:, 0:nt, :],
                                               in1=bl(m4.ap, 0, 4, [[1, nt], [0, NE]]), op=ALU.mult), [q1, m4], [], [GW])
                    j0 += nt
                Mb = Buf(sb("Mb", [128, NJ, NE], BF16, p1))
                Macc = Buf(sb("Macc", [128, NJ + 1, NE], BF16, p1))
                Rsb = Buf(sb("Rsb", [128, NJ, NE], F32, p1))
                cnt = Buf(sb("cnt", [128, NE], F32, p1))
                tli = Buf(sb("tli", [128, NE], I32, p1))
                tl = Buf(sb("tl", [128, NE], F32, p1))
                offT = Buf(sb("offT", [128, NE], F32, p1))
                endT = Buf(sb("endT", [128, NE], F32, p1))
                cmp = Buf(sb("cmp", [128, TL, NE], F32, p1))
                eidr = Buf(sb("eidr", [128, TL], F32, p1))
                idf = Buf(sb("idf", [128, TL], F32, p1))
                slf = Buf(sb("slf", [128, 2, NJ], F32, p1))
                K.do(dve, lambda: V.tensor_tensor(out=Mb.ap[:], in0=Q1.ap[:], in1=Q2.ap[:], op=ALU.add), reads=[Q1, Q2], writes=[Mb])

                K.do(dve, lambda: V.memset(Macc.ap[:, 0, :], 0.0), writes=[Macc])
                for j in range(NJ):
                    K.do(dve, lambda: V.tensor_tensor(out=Macc.ap[:, j + 1, :], in0=Macc.ap[:, j, :], in1=Mb.ap[:, j, :], op=ALU.add),
                         reads=[Mb], writes=[Macc])
                for c0 in range(0, NJ, 32):
                    c1 = min(NJ, c0 + 32)
                    pb = ps8.next()

                    def emit_rank():
                        ins = None
                        for j in range(c0, c1):
                            o = pb.ap[:, (j - c0) * NE:(j - c0 + 1) * NE]
                            T.matmul(o, tri_bf[:], Mb.ap[:, j, :], start=True, stop=False)
                            ins = T.matmul(o, ones_bf[:], Macc.ap[:, j, :], start=False, stop=True)
                        return ins
                    K.do(pe, emit_rank, reads=[Mb, Macc, B_const], writes=[pb])
                    K.do(act, lambda: A.copy(Rsb.ap[:, c0:c1, :].rearrange("p j e -> p (j e)"), pb.ap[:, 0:(c1 - c0) * NE]),
                         reads=[pb], pwrites=[Rsb])
                pbc = ps8.next()
                K.do(pe, lambda: T.matmul(pbc.ap[:, 0:NE], ones_bf[:], Macc.ap[:, NJ, :], start=True, stop=True),
                     reads=[Macc, B_const], writes=[pbc])
                K.do(act, lambda: A.copy(cnt.ap[:], pbc.ap[:, 0:NE]), reads=[pbc], writes=[cnt])
                K.do(dve, lambda: V.tensor_scalar(tl.ap[:], cnt.ap[:], 1.0 / 512.0, 511.0 / 512.0 - 0.5 + 1.0 / 1024.0, ALU.mult, ALU.add),
                     reads=[cnt], writes=[tl])
                K.do(dve, lambda: V.tensor_copy(tli.ap[:], tl.ap[:]), reads=[tl], writes=[tli])
                K.do(dve, lambda: V.tensor_copy(tl.ap[:], tli.ap[:]), reads=[tli], writes=[tl])

                K.do(dve, lambda: V.memset(offT.ap[:, 0:1], 0.0), writes=[offT])
                for e in range(1, NE):
                    K.do(dve, lambda: V.tensor_tensor(out=offT.ap[:, e:e + 1], in0=offT.ap[:, e - 1:e], in1=tl.ap[:, e - 1:e], op=ALU.add),
                         reads=[tl], writes=[offT])
                K.do(dve, lambda: V.tensor_tensor(out=endT.ap[:], in0=offT.ap[:], in1=tl.ap[:], op=ALU.add), reads=[offT, tl], writes=[endT])
                K.do(dve, lambda: V.scalar_tensor_tensor(out=Rsb.ap[:], in0=bl(offT.ap, 0, NE, [[0, NJ], [1, NE]]), scalar=512.0,
                                                         in1=Rsb.ap[:], op0=ALU.mult, op1=ALU.add), reads=[offT], writes=[Rsb])
                for k, Q in enumerate((Q1, Q2)):
                    tmpq = Buf(sb(f"tmpq{k}", [128, NJ, NE], F32, p1))
                    K.do(dve, lambda: V.tensor_tensor(out=tmpq.ap[:], in0=Q.ap[:], in1=Rsb.ap[:], op=ALU.mult), reads=[Q, Rsb], writes=[tmpq])
                    K.do(dve, lambda: V.tensor_reduce(out=slf.ap[:, k, :], in_=tmpq.ap[:], axis=AX.X, op=ALU.add), reads=[tmpq], pwrites=[slf])
                    K.do(dve, lambda: V.tensor_tensor(out=tmpq.ap[:], in0=Q.ap[:], in1=GW.ap[:], op=ALU.mult), reads=[Q, GW], writes=[tmpq])
                    K.do(dve, lambda: V.tensor_reduce(out=wsel.ap[:, k, :], in_=tmpq.ap[:], axis=AX.X, op=ALU.add), reads=[tmpq], pwrites=[wsel])
                K.do(dve, lambda: V.tensor_copy(slot_i.ap[:], slf.ap[:]), reads=[slf], writes=[slot_i])
                K.do(dve, lambda: V.tensor_tensor(out=cmp.ap[:], in0=bl(endT.ap, 0, NE, [[0, TL], [1, NE]]),
                                                  in1=bl(iot, 0, TMAX, [[1, TL], [0, NE]]), op=ALU.is_le),
                     reads=[endT, B_const], writes=[cmp])
                K.do(dve, lambda: V.tensor_reduce(out=eidr.ap[:], in_=cmp.ap[:], axis=AX.X, op=ALU.add), reads=[cmp], writes=[eidr])
                K.do(dve, lambda: V.tensor_scalar(eidr.ap[:], eidr.ap[:], float(NE - 1), None, ALU.min), writes=[eidr])
                K.do(dve, lambda: V.scalar_tensor_tensor(out=idf.ap[:], in0=eidr.ap[:], scalar=128.0,
                                                         in1=bl(basek, 0, 8, [[0, TL]]), op0=ALU.mult, op1=ALU.add),
                     reads=[eidr, B_const], writes=[idf])
                K.do(dve, lambda: V.tensor_copy(idxW.ap[:], idf.ap[:]), reads=[idf], writes=[idxW])
                if "SLOT" in debug:
                    for nm, bf, shp in (("CNT", cnt, [128, NE]), ("TLD", tl, [128, NE]), ("OFFT", offT, [128, NE]), ("ENDT", endT, [128, NE]),
                                        ("RSB", Rsb, [128, NJ, NE]), ("Q1D", Q1, [128, NJ, NE]), ("Q2D", Q2, [128, NJ, NE])):
                        dd = nc.dram_tensor(nm, shp, F32, kind="ExternalOutput").ap()
                        K.dma(sp, [lambda dd=dd, bf=bf: S.dma_start(out=dd, in_=bf.ap[:])], bf, reads=[bf])
                    K.dma(sp, [lambda: S.dma_start(out=DBG["SLOT"], in_=slf.ap[:])], slf, reads=[slf])
                    K.dma(sp, [lambda: S.dma_start(out=DBG["EID"], in_=eidr.ap[:])], eidr, reads=[eidr])
                    K.dma(sp, [lambda: S.dma_start(out=DBG["WSEL"], in_=wsel.ap[:])], wsel, reads=[wsel])
            with scope() as p2:
                xrr = mkring("xrow", [128, D], BF16, 3, p2)
                for j, (sq, r0) in enumerate(tiles):
                    xt = xrr.next()
                    K.dma(sp, [lambda: S.dma_start(out=xt.ap[:], in_=sq["XN2R"][r0:r0 + 128, :])], xt, writes=[xt])
                    K.dma(pool, [lambda k=k: G.indirect_dma_start(out=XS, out_offset=bass.IndirectOffsetOnAxis(ap=slot_i.ap[:, k, j:j + 1], axis=0),
                                                                  in_=xt.ap[:], in_offset=None) for k in range(2)],
                          xt, reads=[xt, slot_i], pwrites=[B_XS])
            with scope() as p3:
                wgr = mkring("wg", [128, 8, DFF], BF16, 2, p3)
                wur = mkring("wu", [128, 8, DFF], BF16, 2, p3)
                wdr = mkring("wd", [128, 4, D], BF16, 2, p3)
                xrr = mkring("xs_rows", [128, 4, D], BF16, 2, p3)
                xTr = mkring("xsT", [128, 8, 512], BF16, 2, p3)
                her = mkring("heT", [128, 4, 512], BF16, 2, p3)
                sr = mkring("sl", [128, 512], BF16, 2, p3)
                ysr = mkring("ysb", [128, D], BF16, 3, p3)
                for i in range(TL):
                    wg, wu, wd, xr, xT, he = wgr.next(), wur.next(), wdr.next(), xrr.next(), xTr.next(), her.next()
                    K.dma(sp, [lambda: S.dma_start(out=xr.ap[:], in_=XS[i * 512:(i + 1) * 512, :].rearrange("(a p) d -> p a d", p=128))],
                          xr, reads=[B_XS], writes=[xr])
                    for (wt_, src_) in ((wg, WG[l]), (wu, WU[l]), (wd, WD[l])):
                        K.dma(pool, [lambda wt_=wt_, src_=src_: G.indirect_dma_start(
                            out=wt_.ap[:].rearrange("p a b -> p (a b)"), out_offset=None, in_=src_,
                            in_offset=bass.IndirectOffsetOnAxis(ap=idxW.ap[:, i:i + 1], axis=0))],
                              wt_, reads=[idxW, B_expw[l]], writes=[wt_])
                    for js in range(4):
                        pb = ps8.next()
                        pv = pb.ap[:].bitcast(BF16)
                        K.do(pe, lambda: [T.transpose(pv[:, kt * 128:(kt + 1) * 128], xr.ap[:, js, kt::8], ident_bf[:])
                                          for kt in range(8)][-1], reads=[xr, B_const], writes=[pb])
                        K.do(act if js % 2 == 0 else dve,
                             (lambda: A.copy(xT.ap[:, :, js * 128:(js + 1) * 128], pv.rearrange("p (k t) -> p k t", k=8))) if js % 2 == 0 else
                             (lambda: V.tensor_copy(xT.ap[:, :, js * 128:(js + 1) * 128], pv.rearrange("p (k t) -> p k t", k=8))),
                             reads=[pb], **(dict(writes=[xT]) if js == 0 else dict(pwrites=[xT])))
                    for ft in range(4):
                        pgt, put = ps8.next(), ps8.next()
                        mm_group(pgt.ap[:, :], [(wg.ap[:, kt, ft::4], xT.ap[:, kt, :]) for kt in range(8)],
                                 reads=[wg, xT], pbuf=pgt)
                        mm_group(put.ap[:, :], [(wu.ap[:, kt, ft::4], xT.ap[:, kt, :]) for kt in range(8)],
                                 reads=[wu, xT], pbuf=put)
                        sl = sr.next()
                        K.do(act, lambda: A.activation(out=sl.ap[:], in_=pgt.ap[:, :], func=AF.Silu), reads=[pgt], writes=[sl])
                        K.do(dve, lambda: V.tensor_tensor(out=he.ap[:, ft, :], in0=sl.ap[:], in1=put.ap[:, :], op=ALU.mult),
                             reads=[sl, put], **(dict(writes=[he]) if ft == 0 else dict(pwrites=[he])))
                    for js in range(4):
                        ys = ysr.next()
                        for half in range(2):
                            po = ps8.next()
                            mm_group(po.ap[:, :], [(he.ap[:, kt, js * 128:(js + 1) * 128], wd.ap[:, kt, half * 512:(half + 1) * 512])
                                                   for kt in range(4)], reads=[he, wd], pbuf=po)
                            hs = slice(half * 512, (half + 1) * 512)
                            kw = dict(writes=[ys]) if half == 0 else dict(pwrites=[ys])
                            if half == 0:
                                K.do(act, lambda: A.copy(ys.ap[:, hs], po.ap[:, :]), reads=[po], **kw)
                            else:
                                K.do(dve, lambda: V.tensor_copy(ys.ap[:, hs], po.ap[:, :]), reads=[po], **kw)
                        r0 = i * 512 + js * 128
                        K.dma(sp, [lambda: S.dma_start(out=YS[r0:r0 + 128, :], in_=ys.ap[:])], ys, reads=[ys], pwrites=[B_YS])
            with scope() as p4:
                G2 = {}
                for mr in sorted({sq["modrow"] for (sq, _) in tiles}):
                    G2[mr] = Buf(sb(f"g2b{mr}", [128, D], F32, p4))
                    K.dma(sp, [lambda mr=mr: S.dma_start(out=G2[mr].ap[:], in_=MOD[l, mr, 5 * D:6 * D].partition_broadcast(128))],
                          G2[mr], writes=[G2[mr]])
                G3b = None
                if last:
                    G3b = Buf(sb("g3b", [128, D], F32, p4))
                    K.dma(sp, [lambda: S.dma_start(out=G3b.ap[:], in_=final_norm_g.partition_broadcast(128))], G3b, writes=[G3b])
                y1r = mkring("y1", [128, D], BF16, 2, p4)
                y2r = mkring("y2", [128, D], BF16, 2, p4)
                hr = mkring("hin2", [128, D], F32, 2, p4)
                hnr = mkring("hn2", [128, D], F32, 2, p4)
                mr_ = mkring("mix", [128, D], F32, 2, p4)
                tmpb = Buf(sb("tmp3", [128, D], F32, p4))
                ssr = mkring("ss3", [128, 2], F32, 2, p4)
                for j, (sq, r0) in enumerate(tiles):
                    y1, y2, ht, hn, mx = y1r.next(), y2r.next(), hr.next(), hnr.next(), mr_.next()
                    K.dma(pool, [lambda: G.indirect_dma_start(out=y1.ap[:], out_offset=None, in_=YS,
                                                              in_offset=bass.IndirectOffsetOnAxis(ap=slot_i.ap[:, 0, j:j + 1], axis=0))],
                          y1, reads=[slot_i, B_YS], writes=[y1])
                    K.dma(pool, [lambda: G.indirect_dma_start(out=y2.ap[:], out_offset=None, in_=YS,
                                                              in_offset=bass.IndirectOffsetOnAxis(ap=slot_i.ap[:, 1, j:j + 1], axis=0))],
                          y2, reads=[slot_i, B_YS], writes=[y2])
                    K.dma(sp, [lambda: S.dma_start(out=ht.ap[:], in_=sq["HA"][r0:r0 + 128, :])], ht, writes=[ht])
                    K.do(act, lambda: A.activation(out=mx.ap[:], in_=y1.ap[:], func=AF.Copy, scale=wsel.ap[:, 0, j:j + 1]),
                         reads=[y1, wsel], writes=[mx])
                    K.do(dve, lambda: V.scalar_tensor_tensor(out=mx.ap[:], in0=y2.ap[:], scalar=wsel.ap[:, 1, j:j + 1], in1=mx.ap[:],
                                                             op0=ALU.mult, op1=ALU.add), reads=[y2, wsel], writes=[mx])
                    K.do(dve, lambda: V.tensor_tensor(out=mx.ap[:], in0=mx.ap[:], in1=G2[sq["modrow"]].ap[:], op=ALU.mult),
                         reads=[G2[sq["modrow"]]], writes=[mx])
                    K.do(dve, lambda: V.tensor_tensor(out=hn.ap[:], in0=mx.ap[:], in1=ht.ap[:], op=ALU.add), reads=[mx, ht], writes=[hn])
                    if not last:
                        K.dma(sp, [lambda: S.dma_start(out=sq["HB"][r0:r0 + 128, :], in_=hn.ap[:])], hn, reads=[hn])
                    else:
                        ss = ssr.next()
                        rms_mod(hn, G3b, None, None, tmpb, ss)
                        K.dma(sp, [lambda: S.dma_start(out=out_d[sq["b"], r0:r0 + 128, :], in_=tmpb.ap[:])], tmpb, reads=[tmpb])

    def phase_moe(l, seq_list):
        last = (l == DEPTH - 1)
        blocks = []
        ctxs = [sq for sq in seq_list if sq["ctx"]]
        if ctxs and not last:
            blocks.append([(sq, 0, LC) for sq in ctxs])
        for sq in seq_list:
            if not sq["ctx"]:
                for tt in range(LS // 512):
                    blocks.append([(sq, tt * 512, 512)])
        with scope() as ph:
            rw = Buf(sb("rw", [128, 8, NE], BF16, ph))
            rbb = Buf(sb("rbb", [128, 4, NE], F32, ph))
            K.dma(pool, [lambda: G.dma_start(out=rw.ap[:], in_=router_w.rearrange("(kt p) e -> p kt e", p=128))], rw, writes=[rw])
            K.dma(sp, [lambda j=j: S.dma_start(out=rbb.ap[:, j, :], in_=router_b.partition_broadcast(128)) for j in range(4)],
                  rbb, writes=[rbb])
            g2c = Buf(sb("g2c", [128, 3, 8], F32, ph))
            for r in range(3):
                load_T(g2c, g2c.ap[:, r, :], [(0, 8, MOD[l, r, 5 * D:6 * D].rearrange("(k p) -> k p", p=128))], 8, 128,
                       partial=(r > 0))
            G3b = None
            if last:
                G3b = Buf(sb("g3b", [128, D], F32, ph))
                K.dma(sp, [lambda: S.dma_start(out=G3b.ap[:], in_=final_norm_g.partition_broadcast(128))], G3b, writes=[G3b])
            xTr = mkring("xT", [128, 8, 512], BF16, 2, ph)
            heT = sb("heT", [128, 64, 512], BF16, ph)
            B_he = Buf()
            WR = mkring("wstream", [128, 16384], BF16, 2, ph)
            moT = sb("moT", [128, 8, 512], F32, ph)
            B_mo = Buf()
            gbr = mkring("gb", [128, 512], BF16, 2, ph)
            sr = mkring("sl", [128, 512], BF16, 2, ph)
            t1r = mkring("t1", [128, 512], BF16, 2, ph)
            hr = mkring("hin2", [128, D], F32, 2, ph)
            hnr = mkring("hn2", [128, D], F32, 2, ph)
            tmpb = Buf(sb("tmp3", [128, D], F32, ph))
            ssr = mkring("ss3", [128, 2], F32, 2, ph)
            sc = Buf(sb("r_sc", [128, 4, NE], F32, ph))
            bb = Buf(sb("r_b", [128, 4, NE], F32, ph))
            b2 = Buf(sb("r_b2", [128, 4, NE], F32, ph))
            q1 = Buf(sb("r_q1", [128, 4, NE], F32, ph))
            m16 = Buf(sb("r_m16", [128, 16], F32, ph))
            m16b = Buf(sb("r_m16b", [128, 16], F32, ph))
            m4 = Buf(sb("r_m4", [128, 4], F32, ph))
            gts = Buf(sb("r_gts", [128, 4, NE], BF16, ph))
            gT = Buf(sb("r_gT", [16, 512], BF16, ph))

            def bl(ap_t, off, pstride, dims):
                return bass.AP(ap_t, off, [[pstride, 128]] + dims)

            for blk in blocks:
                xT = xTr.next()
                off = 0
                ems = []
                for (sq, t0, n) in blk:
                    ems.append(lambda sq=sq, t0=t0, n=n, off=off: S.dma_start(
                        out=xT.ap[:, :, off:off + n], in_=sq["XN2T"][:, t0:t0 + n].rearrange("(k p) t -> p k t", p=128)))
                    off += n
                K.dma(sp, ems, xT, writes=[xT])
                pr = ps8.next()
                for j in range(4):
                    mm_group(pr.ap[:, j * NE:(j + 1) * NE], [(xT.ap[:, kt, j * 128:(j + 1) * 128], rw.ap[:, kt, :]) for kt in range(8)],
                             reads=[xT, rw], pbuf=pr, pw=(j > 0))
                K.do(act, lambda: A.activation(out=sc.ap[:].rearrange("p j e -> p (j e)"), in_=pr.ap[:, 0:4 * NE], func=AF.Sigmoid),
                     reads=[pr], writes=[sc])

                def vv(fn, rd, wr):
                    K.do(dve, fn, reads=rd, writes=wr)
                flat = lambda t_: t_.ap[:].rearrange("p j e -> p (j e)")
                g44 = lambda t_: t_.ap[:].rearrange("p j (g e) -> p (j g) e", e=4)
                vv(lambda: V.tensor_tensor(out=flat(bb), in0=flat(sc), in1=flat(rbb), op=ALU.add), [sc, rbb], [bb])
                vv(lambda: V.tensor_reduce(out=m16.ap[:], in_=g44(bb), axis=AX.X, op=ALU.max), [bb], [m16])
                m16bc = bl(m16.ap, 0, 16, [[1, 16], [0, 4]])
                vv(lambda: V.tensor_tensor(out=g44(q1), in0=g44(bb), in1=m16bc, op=ALU.is_equal), [bb, m16], [q1])
                vv(lambda: V.scalar_tensor_tensor(out=flat(b2), in0=flat(q1), scalar=-BIG, in1=flat(bb), op0=ALU.mult, op1=ALU.add),
                   [q1, bb], [b2])
                vv(lambda: V.tensor_reduce(out=m16b.ap[:], in_=g44(b2), axis=AX.X, op=ALU.max), [b2], [m16b])
                vv(lambda: V.tensor_tensor(out=m16.ap[:], in0=m16.ap[:], in1=m16b.ap[:], op=ALU.add), [m16b], [m16])
                vv(lambda: V.tensor_reduce(out=m4.ap[:], in_=m16.ap[:].rearrange("p (j g) -> p j g", g=4), axis=AX.X, op=ALU.max),
                   [m16], [m4])
                m4bc = bl(m4.ap, 0, 4, [[1, 4], [0, 4]])
                vv(lambda: V.tensor_tensor(out=m16b.ap[:].rearrange("p (j g) -> p j g", g=4),
                                           in0=m16.ap[:].rearrange("p (j g) -> p j g", g=4), in1=m4bc, op=ALU.is_equal),
                   [m16, m4], [m16b])
                vv(lambda: V.tensor_scalar(m16b.ap[:], m16b.ap[:], -1.0, BIG, ALU.add, ALU.mult), [], [m16b])
                penbc = bl(m16b.ap, 0, 16, [[1, 16], [0, 4]])
                vv(lambda: V.tensor_tensor(out=g44(b2), in0=g44(bb), in1=penbc, op=ALU.add), [bb, m16b], [b2])
                vv(lambda: V.tensor_reduce(out=m4.ap[:], in_=b2.ap[:], axis=AX.X, op=ALU.max), [b2], [m4])
                m4e = bl(m4.ap, 0, 4, [[1, 4], [0, NE]])
                vv(lambda: V.tensor_tensor(out=q1.ap[:], in0=b2.ap[:], in1=m4e, op=ALU.is_equal), [b2, m4], [q1])
                vv(lambda: V.scalar_tensor_tensor(out=flat(b2), in0=flat(q1), scalar=-BIG, in1=flat(b2), op0=ALU.mult, op1=ALU.add),
                   [q1], [b2])
                vv(lambda: V.tensor_reduce(out=m4.ap[:], in_=b2.ap[:], axis=AX.X, op=ALU.max), [b2], [m4])
                vv(lambda: V.tensor_tensor(out=bb.ap[:], in0=b2.ap[:], in1=m4e, op=ALU.is_equal), [b2, m4], [bb])
                vv(lambda: V.tensor_tensor(out=flat(q1), in0=flat(q1), in1=flat(bb), op=ALU.add), [bb], [q1])
                vv(lambda: V.tensor_tensor(out=flat(q1), in0=flat(q1), in1=flat(sc), op=ALU.mult), [sc], [q1])
                vv(lambda: V.tensor_reduce(out=m4.ap[:], in_=q1.ap[:], axis=AX.X, op=ALU.add), [q1], [m4])
                vv(lambda: V.reciprocal(m4.ap[:], m4.ap[:]), [], [m4])
                vv(lambda: V.tensor_tensor(out=gts.ap[:], in0=q1.ap[:], in1=m4e, op=ALU.mult), [q1, m4], [gts])
                if "GATES" in debug:
                    pass
                pg = ps8.next()
                pgv = pg.ap[:].bitcast(BF16)
                K.do(pe, lambda: [T.transpose(pgv[0:NE, j * 128:(j + 1) * 128], gts.ap[:, j, :], ident_bf[:])
                                  for j in range(4)][-1], reads=[gts, B_const], writes=[pg])
                K.do(act, lambda: A.copy(gT.ap[:], pgv[0:NE, 0:512]), reads=[pg], writes=[gT])
                for e in range(NE):
                    ws = WR.next()
                    after = []
                    K.dma(sp, [lambda: S.dma_start(out=ws.ap[:, 0:4096].rearrange("p (k f) -> p k f", k=8),
                                                   in_=WG[l, e].rearrange("(k p) f -> p k f", p=128)),
                               lambda: S.dma_start(out=ws.ap[:, 4096:8192].rearrange("p (k f) -> p k f", k=8),
                                                   in_=WU[l, e].rearrange("(k p) f -> p k f", p=128))],
                          ws, reads=[B_expw[l]], writes=[ws])
                    wgv = ws.ap[:, 0:4096].rearrange("p (k f) -> p k f", k=8)
                    wuv = ws.ap[:, 4096:8192].rearrange("p (k f) -> p k f", k=8)
                    pbc = ps8.next()
                    mm_group(pbc.ap[:, :], [(onesel[:, e, :], gT.ap[:, :])], reads=[gT, B_const], pbuf=pbc)
                    gb = gbr.next()
                    K.do(act, lambda: A.copy(gb.ap[:], pbc.ap[:, :]), reads=[pbc], writes=[gb])
                    for ft in range(4):
                        pgt, put = ps8.next(), ps8.next()
                        mm_group(pgt.ap[:, :], [(wgv[:, kt, ft * 128:(ft + 1) * 128], xT.ap[:, kt, :]) for kt in range(8)],
                                 reads=[ws, xT], pbuf=pgt)
                        mm_group(put.ap[:, :], [(wuv[:, kt, ft * 128:(ft + 1) * 128], xT.ap[:, kt, :]) for kt in range(8)],
                                 reads=[ws, xT], pbuf=put)
                        sl = sr.next()
                        K.do(act, lambda: A.activation(out=sl.ap[:], in_=pgt.ap[:, :], func=AF.Silu), reads=[pgt], writes=[sl])
                        t1 = t1r.next()
                        K.do(dve, lambda: V.tensor_tensor(out=t1.ap[:], in0=sl.ap[:], in1=put.ap[:, :], op=ALU.mult),
                             reads=[sl, put], writes=[t1])
                        first = (e == 0 and ft == 0)
                        K.do(pool, lambda: G.tensor_tensor(out=heT[:, e * 4 + ft, :], in0=t1.ap[:], in1=gb.ap[:], op=ALU.mult),
                             reads=[t1, gb], **(dict(writes=[B_he]) if first else dict(pwrites=[B_he])))
                for dt2 in range(4):
                    ws = WR.next()
                    wdv = ws.ap[:].rearrange("p (i c) -> p i c", c=256)
                    K.dma(sp, [lambda: S.dma_start(out=wdv, in_=WD[l][:, dt2 * 256:(dt2 + 1) * 256].rearrange("(i p) c -> p i c", p=128))],
                          ws, reads=[B_expw[l]], writes=[ws])
                    for dtl in range(2):
                        dt_ = dt2 * 2 + dtl
                        po = ps8.next()
                        mm_group(po.ap[:, :], [(wdv[:, i, dtl * 128:(dtl + 1) * 128], heT[:, i, :]) for i in range(64)],
                                 reads=[ws, B_he], pbuf=po)
                        mr = blk[0][0]["modrow"]
                        if len(blk) == 1:
                            K.do(act, lambda: A.activation(out=moT[:, dt_, :], in_=po.ap[:, :], func=AF.Copy,
                                                           scale=g2c.ap[:, mr, dt_:dt_ + 1]),
                                 reads=[po, g2c], **(dict(writes=[B_mo]) if dt_ == 0 else dict(pwrites=[B_mo])))
                        else:
                            o2 = 0
                            for pi, (sq, t0, n) in enumerate(blk):
                                K.do(act, lambda: A.activation(out=moT[:, dt_, o2:o2 + n], in_=po.ap[:, o2:o2 + n], func=AF.Copy,
                                                               scale=g2c.ap[:, sq["modrow"], dt_:dt_ + 1]),
                                     reads=[po, g2c], **(dict(writes=[B_mo]) if (dt_ == 0 and pi == 0) else dict(pwrites=[B_mo])))
                                o2 += n
                off = 0
                for (sq, t0, n) in blk:
                    for jj in range(n // 128):
                        c0 = off + jj * 128
                        r0 = t0 + jj * 128
                        ht = hr.next()
                        K.dma(sp, [lambda: S.dma_start(out=ht.ap[:], in_=sq["HA"][r0:r0 + 128, :])], ht, writes=[ht])
                        hn = hnr.next()
                        for half in range(2):
                            pt = ps8.next()
                            K.do(pe, lambda: [T.transpose(pt.ap[:, k * 128:(k + 1) * 128], moT[:, half * 4 + k, c0:c0 + 128], ident_f[:])
                                              for k in range(4)][-1], reads=[B_mo, B_const], writes=[pt])
                            hs = slice(half * 512, (half + 1) * 512)
                            K.do(dve, lambda: V.tensor_tensor(out=hn.ap[:, hs], in0=pt.ap[:, :], in1=ht.ap[:, hs], op=ALU.add),
                                 reads=[pt, ht], **(dict(writes=[hn]) if half == 0 else dict(pwrites=[hn])))
                        if not last:
                            K.dma(pool, [lambda: G.dma_start(out=sq["HB"][r0:r0 + 128, :], in_=hn.ap[:])], hn, reads=[hn])
                        else:
                            ss = ssr.next()
                            rms_mod(hn, G3b, None, None, tmpb, ss)
                            K.dma(pool, [lambda: G.dma_start(out=out_d[sq["b"], r0:r0 + 128, :], in_=tmpb.ap[:])], tmpb, reads=[tmpb])
                    off += n

    DBG = {}
    debug_seq = 2
    if "XNT" in debug:
        DBG["XNT"] = nc.dram_tensor("XNT", [D, LS], BF16, kind="ExternalOutput").ap()
    if "SLOT" in debug:
        njd = sum((SEQ[s_]["L"] // 128) for s_ in seqs)
        DBG["SLOT"] = nc.dram_tensor("SLOT", [128, 2, njd], F32, kind="ExternalOutput").ap()
        DBG["WSEL"] = nc.dram_tensor("WSEL", [128, 2, njd], F32, kind="ExternalOutput").ap()
        DBG["EID"] = nc.dram_tensor("EID", [128, (2 * njd * 128 + NE * 511) // 512], F32, kind="ExternalOutput").ap()

    def stop(name):
        return stop_after == name

    def finish():
        K.barrier()
        es.close()
        return nc

    init_consts()
    seq_list = [SEQ[s] for s in seqs]
    for l in range(nlayers):
        convert_experts(l)
    for l in range(nlayers):
        phase_adaln(l)
    if stop("adaln"):
        return finish()
    for l in range(nlayers):
        phase_s5_tables(l)
    if stop("tables"):
        return finish()
    for l in range(nlayers):
        last = (l == DEPTH - 1)
        for sq in seq_list:
            phase_front(l, sq, only_cu=(last and sq["ctx"]))
        if stop(f"front{l}"):
            return finish()
        for sq in seq_list:
            phase_s5(l, sq)
        if stop(f"s5{l}"):
            return finish()
        for sq in seq_list:
            if not (last and sq["ctx"]):
                phase_back(l, sq)
        if stop(f"back{l}"):
            return finish()
        phase_moe_sparse(l, seq_list)
        if stop(f"moe{l}"):
            return finish()
    return finish()


_PROG = {}


def _inputs_for_core(inputs, c):
    m = {}
    b0 = 2 * c
    f = lambda a: np.ascontiguousarray(np.asarray(a, dtype=np.float32))
    m["x"] = f(inputs["x"][b0:b0 + 2])
    m["ctx"] = f(inputs["ctx"][b0:b0 + 2])
    m["cvec"] = f(np.stack([inputs["c"][b0], inputs["c"][b0 + 1], inputs["c_ctx"]], axis=0))
    for k in ("w_mod", "b_mod", "norm1_g", "norm2_g", "w_in", "sgu_norm_g", "sgu_w", "sgu_b", "conv_w",
              "s5_lam_re", "s5_lam_im", "s5_log_dt", "s5_b_re", "s5_b_im", "s5_c_re", "s5_c_im", "s5_d",
              "glu_w", "glu_b", "w_branch", "w_out", "router_w", "router_b", "exp_w_gate", "exp_w_up",
              "exp_w_down", "final_norm_g"):
        m[k] = f(inputs[k])
    return m


def kernel(**inputs):
    if "nc" not in _PROG:
        _PROG["nc"] = build_program()
    nc = _PROG["nc"]
    shared = _inputs_for_core(inputs, 0)
    in_maps = []
    for c in range(NCORES):
        m = dict(shared)
        b0 = 2 * c
        m["x"] = np.ascontiguousarray(np.asarray(inputs["x"][b0:b0 + 2], dtype=np.float32))
        m["ctx"] = np.ascontiguousarray(np.asarray(inputs["ctx"][b0:b0 + 2], dtype=np.float32))
        m["cvec"] = np.ascontiguousarray(np.stack([inputs["c"][b0], inputs["c"][b0 + 1], inputs["c_ctx"]], axis=0).astype(np.float32))
        in_maps.append(m)
    res = run_bass_kernel_spmd(nc, in_maps, core_ids=list(range(NCORES)))
    out = np.concatenate([np.asarray(r["out"]) for r in res.results], axis=0)
    return out.astype(np.float32)
```

```python
import math
import numpy as np
from contextlib import ExitStack, contextmanager
import concourse.bass as bass
import concourse.mybir as mybir
from concourse.bass_utils import run_bass_kernel_spmd

F32 = mybir.dt.float32
BF16 = mybir.dt.bfloat16
I32 = mybir.dt.int32
AF = mybir.ActivationFunctionType
ALU = mybir.AluOpType
AX = mybir.AxisListType

D = 1024
LS = 2048
LC = 256
DEPTH = 2
DIN = 6144
WM = 512
NE = 16
DFF = 512
NCORES = 8
EPS = 1e-6
BIG = 1.0e4
TWO_PI = 2.0 * math.pi


class Eng:
    def __init__(self, nc, e, name):
        self.e = e
        self.sem = nc.alloc_semaphore(name)
        self.n = 0
        self.seen = {}

    def wait(self, evs):
        for sem, val in evs.items():
            if self.seen.get(sem, 0) >= val:
                continue
            self.e.wait_ge(sem, val)
            self.seen[sem] = val

    def sig(self, ins):
        self.n += 1
        ins.then_inc(self.sem, 1)
        return (self.sem, self.n)


class Buf:
    def __init__(self, ap=None):
        self.ap = ap
        self.w = {}
        self.r = {}
        self.dsem = None
        self.slot = None
        self.dv = 0


def _merge(d, ev):
    s, v = ev
    if d.get(s, 0) < v:
        d[s] = v


class Ring:
    def __init__(self, bufs):
        self.bufs = bufs
        self.i = 0

    def next(self):
        b = self.bufs[self.i]
        self.i = (self.i + 1) % len(self.bufs)
        return b


class Ker:
    def __init__(self, nc):
        self.nc = nc
        self.pe = Eng(nc, nc.tensor, "s_pe")
        self.act = Eng(nc, nc.scalar, "s_act")
        self.dve = Eng(nc, nc.vector, "s_dve")
        self.pool = Eng(nc, nc.gpsimd, "s_pool")
        self.sp = Eng(nc, nc.sync, "s_sp")
        self.engs = [self.pe, self.act, self.dve, self.pool, self.sp]
        self.dbufs = []
        self.uid = 0
        self.free_slots = []
        self.scope_bufs = [[]]

    def _pre(self, eng, reads, writes, after):
        evs = {}
        for b in reads:
            for s, v in b.w.items():
                _merge(evs, (s, v))
        for b in writes:
            for s, v in b.w.items():
                _merge(evs, (s, v))
            for s, v in b.r.items():
                _merge(evs, (s, v))
        for ev in after:
            _merge(evs, ev)
        eng.wait(evs)

    def _post(self, ev, reads, writes, pwrites):
        for b in reads:
            _merge(b.r, ev)
        for b in writes:
            b.w = {ev[0]: ev[1]}
            b.r = {}
        for b in pwrites:
            _merge(b.w, ev)

    def do(self, eng, emit, reads=(), writes=(), pwrites=(), after=()):
        self._pre(eng, reads, writes, after)
        ins = emit()
        ev = eng.sig(ins)
        self._post(ev, reads, writes, pwrites)
        return ev

    def dma(self, q, emits, owner, reads=(), writes=(), pwrites=(), after=()):
        self._pre(q, reads, writes, after)
        if owner.dsem is None:
            if self.free_slots:
                owner.slot = self.free_slots.pop()
            else:
                self.uid += 1
                owner.slot = [self.nc.alloc_semaphore(f"dq{self.uid}"), 0]
            owner.dsem = owner.slot[0]
            owner.dv = owner.slot[1]
            self.dbufs.append(owner)
            self.scope_bufs[-1].append(owner)
        for em in emits:
            ins = em()
            ins.then_inc(owner.dsem, 16)
            owner.dv += 16
        owner.slot[1] = owner.dv
        ev = (owner.dsem, owner.dv)
        self._post(ev, reads, writes, pwrites)
        return ev

    def barrier(self):
        evs = {}
        for e in self.engs:
            if e.n > 0:
                evs[e.sem] = e.n
        for b in self.dbufs:
            if b.dv > 0:
                evs[b.dsem] = b.dv
        for e in self.engs:
            e.wait(evs)

    def open_scope(self):
        self.scope_bufs.append([])

    def close_scope(self):
        for b in self.scope_bufs.pop():
            self.free_slots.append(b.slot)
            self.dbufs.remove(b)
            b.dsem = None
            b.slot = None


def build_program(debug=(), seqs=(0, 1, 2, 3), stop_after=None, nlayers=DEPTH):
    debug = set(debug)
    nc = bass.Bass("TRN2", target_bir_lowering=False)
    K = Ker(nc)
    es = ExitStack()

    def dram_in(name, shape):
        return nc.dram_tensor(name, list(shape), F32, kind="ExternalInput").ap()

    def dram_scr(name, shape, dt):
        if name in debug:
            return nc.dram_tensor(name, list(shape), dt, kind="ExternalOutput").ap()
        return nc.dram_tensor(name, list(shape), dt).ap()

    x_in = dram_in("x", [2, LS, D])
    ctx_in = dram_in("ctx", [2, LC, D])
    cvec = dram_in("cvec", [3, D])
    w_mod = dram_in("w_mod", [DEPTH, D, 6 * D])
    b_mod = dram_in("b_mod", [DEPTH, 6 * D])
    norm1_g = dram_in("norm1_g", [DEPTH, D])
    norm2_g = dram_in("norm2_g", [DEPTH, D])
    w_in = dram_in("w_in", [DEPTH, D, DIN])
    sgu_norm_g = dram_in("sgu_norm_g", [DEPTH, WM])
    sgu_w = dram_in("sgu_w", [DEPTH, 4, 128, 128])
    sgu_b = dram_in("sgu_b", [DEPTH, 4, 128])
    conv_w = dram_in("conv_w", [DEPTH, 3, WM])
    s5_lam_re = dram_in("s5_lam_re", [DEPTH, 2, 32, 64])
    s5_lam_im = dram_in("s5_lam_im", [DEPTH, 2, 32, 64])
    s5_log_dt = dram_in("s5_log_dt", [DEPTH, 2, 32])
    s5_b_re = dram_in("s5_b_re", [DEPTH, 2, 32, 64, 16])
    s5_b_im = dram_in("s5_b_im", [DEPTH, 2, 32, 64, 16])
    s5_c_re = dram_in("s5_c_re", [DEPTH, 2, 32, 16, 64])
    s5_c_im = dram_in("s5_c_im", [DEPTH, 2, 32, 16, 64])
    s5_d = dram_in("s5_d", [DEPTH, WM])
    glu_w = dram_in("glu_w", [DEPTH, WM, WM])
    glu_b = dram_in("glu_b", [DEPTH, WM])
    w_branch = dram_in("w_branch", [DEPTH, 3, WM, D])
    w_out = dram_in("w_out", [DEPTH, D, D])
    router_w = dram_in("router_w", [D, NE])
    router_b = dram_in("router_b", [NE])
    exp_w_gate = dram_in("exp_w_gate", [DEPTH, NE, D, DFF])
    exp_w_up = dram_in("exp_w_up", [DEPTH, NE, D, DFF])
    exp_w_down = dram_in("exp_w_down", [DEPTH, NE, DFF, D])
    final_norm_g = dram_in("final_norm_g", [D])
    out_d = nc.dram_tensor("out", [2, LS, D], F32, kind="ExternalOutput").ap()

    MOD = dram_scr("MOD", [DEPTH, 3, 6 * D], F32)
    SEQ = []
    for s in range(4):
        isctx = s < 2
        b = s % 2
        Ls = LC if isctx else LS
        SEQ.append(dict(
            s=s, b=b, ctx=isctx, L=Ls, modrow=(2 if isctx else b),
            src=(ctx_in[b] if isctx else x_in[b]),
            CU=dram_scr(f"CU{s}", [WM, Ls], BF16),
            YC=dram_scr(f"YC{s}", [WM, Ls], BF16),
            MAB=dram_scr(f"MAB{s}", [D, Ls], BF16),
            GC=dram_scr(f"GC{s}", [D, Ls], BF16),
            XN2T=dram_scr(f"XN2T{s}", [D, Ls], BF16),
            XN2R=dram_scr(f"XN2R{s}", [Ls, D], BF16),
            HA=dram_scr(f"HA{s}", [Ls, D], F32),
            HB=dram_scr(f"HB{s}", [Ls, D], F32),
        ))
    WG = [dram_scr(f"WG{l}", [NE * 128, 8 * DFF], BF16) for l in range(DEPTH)]
    WU = [dram_scr(f"WU{l}", [NE * 128, 8 * DFF], BF16) for l in range(DEPTH)]
    WD = [dram_scr(f"WD{l}", [NE * 128, 4 * D], BF16) for l in range(DEPTH)]
    TMAX = (2 * (2 * LS + 2 * LC) + NE * 511) // 512
    XS = dram_scr("XS", [TMAX * 512, D], BF16)
    YS = dram_scr("YS", [TMAX * 512, D], BF16)
    W1D = dram_scr("W1D", [DEPTH, 2, 128, 32, 128], BF16)
    W2D = dram_scr("W2D", [DEPTH, 2, 64, 32, 2, 128], BF16)
    KMD = dram_scr("KMD", [DEPTH, 128, 32, 128], BF16)
    SELD = dram_scr("SELD", [128, 8, 8, 128], BF16)
    SELTD = dram_scr("SELTD", [128, 8, 8, 128], BF16)

    def sb(name, shape, dt, stack=None):
        K.uid += 1
        return (stack or es).enter_context(nc.sbuf_tensor(f"{name}_{K.uid}", list(shape), dt))

    @contextmanager
    def scope():
        st = ExitStack()
        K.open_scope()
        try:
            yield st
        finally:
            K.barrier()
            K.close_scope()
            st.close()

    def mkring(name, shape, dt, n, stack):
        return Ring([Buf(sb(f"{name}{i}", shape, dt, stack)) for i in range(n)])

    ident_bf = sb("ident_bf", [128, 128], BF16)
    ident_f = sb("ident_f", [128, 128], F32)
    neg_half = sb("neg_half", [128, 1], F32)
    onesel = sb("onesel", [16, NE, 128], BF16)
    a8_ar = sb("a8_ar", [64, DEPTH * 2, 2, 32], F32)
    a8_ai = sb("a8_ai", [64, DEPTH * 2, 2, 32], F32)
    h0st = sb("h0st", [64, 4, 2, 32], F32)
    ones_bf = sb("ones_bf", [128, 128], BF16)
    tri_bf = sb("tri_bf", [128, 128], BF16)
    iot = sb("iot", [128, TMAX], F32)
    basek = sb("basek", [128, 8], F32)
    B_XS = Buf()
    B_YS = Buf()
    B_const = Buf()
    B_a8 = Buf()
    B_h0 = Buf()

    ps_t = [es.enter_context(nc.psum_tensor(f"ps{i}", [128, 512], F32)) for i in range(8)]
    PS = [Buf(ps_t[i]) for i in range(8)]
    ps8 = Ring(PS)
    psA = Ring(PS[0:4])
    psB = Ring(PS[4:8])

    pe, act, dve, pool, sp = K.pe, K.act, K.dve, K.pool, K.sp
    V, A, T, G, S = nc.vector, nc.scalar, nc.tensor, nc.gpsimd, nc.sync

    def mm_group(out_ap, pairs, reads, pbuf, pw=False):
        def emit():
            n = len(pairs)
            ins = None
            for i, (lh, rh) in enumerate(pairs):
                ins = T.matmul(out_ap, lh, rh, start=(i == 0), stop=(i == n - 1))
            return ins
        if pw:
            return K.do(pe, emit, reads=reads, pwrites=[pbuf])
        return K.do(pe, emit, reads=reads, writes=[pbuf])

    def load_T(dst, dst_ap, srcs, F, P, partial=False):
        with scope() as tmp:
            stg = Buf(sb("ldT", [F, P], F32, tmp))
            K.dma(sp, [lambda r0=r0, n=n, a=a: S.dma_start(out=stg.ap[r0:r0 + n, :], in_=a) for (r0, n, a) in srcs],
                  stg, writes=[stg])
            pb = ps8.next()
            K.do(pe, lambda: T.transpose(pb.ap[0:P, 0:F], stg.ap[:], ident_f[0:F, 0:F]), reads=[stg, B_const], writes=[pb])
            kw = dict(pwrites=[dst]) if partial else dict(writes=[dst])
            K.do(act, lambda: A.copy(dst_ap, pb.ap[0:P, 0:F]), reads=[pb], **kw)

    def init_consts():
        def emit():
            G.memset(ident_bf[:], 0.0)
            G.affine_select(out=ident_bf[:], in_=ident_bf[:], compare_op=ALU.not_equal, fill=1.0,
                            base=0, pattern=[[-1, 128]], channel_multiplier=1)
            G.memset(ident_f[:], 0.0)
            G.affine_select(out=ident_f[:], in_=ident_f[:], compare_op=ALU.not_equal, fill=1.0,
                            base=0, pattern=[[-1, 128]], channel_multiplier=1)
            G.memset(neg_half[:], -0.5)
            G.memset(onesel[:], 0.0)
            G.memset(h0st[:], 0.0)
            return G.affine_select(out=onesel[:], in_=onesel[:], compare_op=ALU.not_equal, fill=1.0,
                                   base=0, pattern=[[-1, NE], [0, 128]], channel_multiplier=1)
        K.do(pool, emit, writes=[B_const, B_h0])

        def emit2():
            G.memset(ones_bf[:], 1.0)
            G.memset(tri_bf[:], 0.0)
            G.affine_select(out=tri_bf[:], in_=tri_bf[:], compare_op=ALU.is_ge, fill=1.0,
                            base=0, pattern=[[-1, 128]], channel_multiplier=1)
            ins = None
            for i in range(TMAX):
                ins = G.memset(iot[:, i:i + 1], float(i))
            return ins
        K.do(pool, emit2, pwrites=[B_const])
        pbp = ps8.next()
        K.do(pe, lambda: T.matmul(pbp.ap[:, 0:1], tri_bf[:], ones_bf[:, 0:1], start=True, stop=True), reads=[B_const], writes=[pbp])
        K.do(dve, lambda: [V.tensor_scalar(basek[:, kt:kt + 1], pbp.ap[:, 0:1], float(kt * 128), None, ALU.add)
                           for kt in range(8)][-1], reads=[pbp], pwrites=[B_const])
        with scope() as pz:
            zt = Buf(sb("zfill", [128, 8192], BF16, pz))
            K.do(dve, lambda: V.memset(zt.ap[:], 0.0), writes=[zt])
            nrows = TMAX * 512
            ems = []
            for r0 in range(0, nrows, 1024):
                nr = min(1024, nrows - r0)
                ems.append(lambda r0=r0, nr=nr: G.dma_start(out=XS[r0:r0 + nr, :].rearrange("(p a) d -> p (a d)", p=128),
                                                            in_=zt.ap[:, 0:(nr // 128) * D]))
            K.dma(pool, ems, zt, reads=[zt], pwrites=[B_XS])
        with scope() as ph:
            selt = sb("selt", [128, 8, 8, 128], BF16, ph)
            Bs = Buf()

            def emit_sel(transposed):
                def f():
                    ins = G.memset(selt[:], 0.0)
                    idv = ident_bf[:].rearrange("p (a b) -> p a b", b=16)
                    for j in range(8):
                        if not transposed:
                            ins = G.tensor_copy(selt[:, :, j, 16 * j:16 * j + 16], idv)
                        else:
                            ins = G.tensor_copy(selt[:, j, :, 16 * j:16 * j + 16], idv)
                    return ins
                return f
            K.do(pool, emit_sel(False), reads=[B_const], writes=[Bs])
            K.dma(sp, [lambda: S.dma_start(out=SELD, in_=selt[:])], Bs, reads=[Bs])
            K.do(pool, emit_sel(True), reads=[B_const], writes=[Bs])
            K.dma(sp, [lambda: S.dma_start(out=SELTD, in_=selt[:])], Bs, reads=[Bs])

    B_expw = [Buf() for _ in range(DEPTH)]

    def convert_experts(l):
        ems = []
        for e in range(NE):
            for (dst, src) in ((WG[l], exp_w_gate[l, e]), (WU[l], exp_w_up[l, e]), (WD[l], exp_w_down[l, e])):
                ems.append(lambda dst=dst, src=src, e=e: G.dma_start(
                    out=dst[e * 128:(e + 1) * 128, :].rearrange("(r a) c -> r (a c)", r=64),
                    in_=src.rearrange("(r a) f -> r (a f)", r=64)))
        K.dma(pool, ems, B_expw[l], pwrites=[B_expw[l]])

    def phase_adaln(l):
        with scope() as ph:
            csb = Buf(sb("csb", [3, D], F32, ph))
            scT = Buf(sb("scT", [128, 8, 3], F32, ph))
            bm = Buf(sb("bm", [3, 6 * D], F32, ph))
            modsb = Buf(sb("modsb", [3, 6 * D], F32, ph))
            wmr = mkring("wm", [128, 8, 512], F32, 4, ph)
            K.dma(sp, [lambda: S.dma_start(out=csb.ap[:], in_=cvec)], csb, writes=[csb])
            K.dma(sp, [lambda: S.dma_start(out=bm.ap[:], in_=b_mod[l].partition_broadcast(3))], bm, writes=[bm])
            pb = ps8.next()
            K.do(pe, lambda: [T.transpose(pb.ap[:, kt * 3:kt * 3 + 3], csb.ap[:, kt * 128:(kt + 1) * 128],
                                          ident_f[0:3, 0:3]) for kt in range(8)][-1],
                 reads=[csb, B_const], writes=[pb])
            K.do(act, lambda: A.activation(out=scT.ap[:], in_=pb.ap[:, 0:24].rearrange("p (k j) -> p k j", k=8),
                                           func=AF.Silu), reads=[pb], writes=[scT])
            for cc in range(12):
                wm = wmr.next()
                qe, QE = sp, S
                K.dma(qe, [lambda h=h: QE.dma_start(out=wm.ap[:, h * 4:(h + 1) * 4, :], in_=w_mod[l][h * 512:(h + 1) * 512, cc * 512:(cc + 1) * 512]
                                                    .rearrange("(kt p) n -> p kt n", p=128)) for h in range(2)], wm, writes=[wm])
                pb = ps8.next()
                mm_group(pb.ap[0:3, :], [(scT.ap[:, kt, :], wm.ap[:, kt, :]) for kt in range(8)],
                         reads=[scT, wm], pbuf=pb)
                K.do(dve, lambda: V.tensor_tensor(out=modsb.ap[:, cc * 512:(cc + 1) * 512], in0=pb.ap[0:3, :],
                                                  in1=bm.ap[:, cc * 512:(cc + 1) * 512], op=ALU.add),
                     reads=[pb, bm], pwrites=[modsb])
            K.dma(sp, [lambda: S.dma_start(out=MOD[l], in_=modsb.ap[:])], modsb, reads=[modsb])

    def make_mod_tiles(l, modrow, g_row, sc_off, sh_off, ph, name):
        gt = Buf(sb(name + "_g", [128, D], F32, ph))
        ht = Buf(sb(name + "_h", [128, D], F32, ph))
        with scope() as tmp:
            st = Buf(sb(name + "_s", [128, D], F32, tmp))
            K.dma(sp, [lambda: S.dma_start(out=gt.ap[:], in_=g_row.partition_broadcast(128))], gt, writes=[gt])
            K.dma(sp, [lambda: S.dma_start(out=st.ap[:], in_=MOD[l, modrow, sc_off:sc_off + D].partition_broadcast(128))],
                  st, writes=[st])
            K.dma(sp, [lambda: S.dma_start(out=ht.ap[:], in_=MOD[l, modrow, sh_off:sh_off + D].partition_broadcast(128))],
                  ht, writes=[ht])
            K.do(dve, lambda: V.scalar_tensor_tensor(out=gt.ap[:], in0=st.ap[:], scalar=1.0, in1=gt.ap[:],
                                                     op0=ALU.add, op1=ALU.mult), reads=[st], writes=[gt])
        return gt, ht

    def rstd_from_sumsq(ss):
        K.do(dve, lambda: V.tensor_scalar(ss.ap[:, 0:1], ss.ap[:, 0:1], 1.0 / D, EPS, ALU.mult, ALU.add), writes=[ss])
        K.do(pool, lambda: G.tensor_tensor(ss.ap[:, 1:2], ss.ap[:, 0:1], neg_half[:], ALU.pow),
             reads=[B_const], writes=[ss])

    def rms_mod(xt, Gb, SHb, xn, tmp, ss):
        K.do(dve, lambda: V.tensor_tensor(out=tmp.ap[:], in0=xt.ap[:], in1=xt.ap[:], op=ALU.mult), reads=[xt], writes=[tmp])
        K.do(dve, lambda: V.tensor_reduce(out=ss.ap[:, 0:1], in_=tmp.ap[:], axis=AX.X, op=ALU.add), reads=[tmp], writes=[ss])
        rstd_from_sumsq(ss)
        K.do(dve, lambda: V.scalar_tensor_tensor(out=tmp.ap[:], in0=xt.ap[:], scalar=ss.ap[:, 1:2], in1=Gb.ap[:],
                                                 op0=ALU.mult, op1=ALU.mult), reads=[xt, ss, Gb], writes=[tmp])
        if SHb is None:
            return
        K.do(dve, lambda: V.tensor_tensor(out=xn.ap[:], in0=tmp.ap[:], in1=SHb.ap[:], op=ALU.add),
             reads=[tmp, SHb], writes=[xn])

    def transpose_tile(xn, dest_ap3, dest_buf, pw=False):
        pb = ps8.next()
        pv = pb.ap[:].bitcast(BF16)
        K.do(pe, lambda: [T.transpose(pv[:, kt * 128:(kt + 1) * 128], xn.ap[:, kt * 128:(kt + 1) * 128], ident_bf[:])
                          for kt in range(8)][-1], reads=[xn, B_const], writes=[pb])
        kw = dict(pwrites=[dest_buf]) if pw else dict(writes=[dest_buf])
        K.do(act, lambda: A.copy(dest_ap3, pv.rearrange("p (k t) -> p k t", k=8)), reads=[pb], **kw)

    def seq_src(l, sq):
        return sq["src"] if l == 0 else sq["HB"]

    def phase_front(l, sq, only_cu=False):
        Ls = sq["L"]
        N = min(512, Ls)
        ntt = Ls // N
        nj = Ls // 128
        jpt = N // 128
        grid = not sq["ctx"]
        with scope() as ph:
            xnT = sb("xnT", [128, 8, Ls], BF16, ph)
            B_xn = [Buf() for _ in range(nj)]
            with scope() as p1:
                Gb, SHb = make_mod_tiles(l, sq["modrow"], norm1_g[l], D, 0, p1, "m1")
                xring = mkring("xin", [128, D], F32, 2, p1)
                tmpb = Buf(sb("tmp1", [128, D], F32, p1))
                xnr = mkring("xnr", [128, D], BF16, 2, p1)
                ssr = mkring("ss", [128, 2], F32, 2, p1)
                src = seq_src(l, sq)
                for j in range(nj):
                    xt = xring.next()
                    K.dma(sp, [lambda: S.dma_start(out=xt.ap[:], in_=src[j * 128:(j + 1) * 128, :])], xt, writes=[xt])
                    xn = xnr.next()
                    ss = ssr.next()
                    rms_mod(xt, Gb, SHb, xn, tmpb, ss)
                    transpose_tile(xn, xnT[:, :, j * 128:(j + 1) * 128], B_xn[j])
            if "XNT" in debug and sq["s"] == debug_seq:
                K.dma(sp, [lambda: S.dma_start(out=DBG["XNT"].rearrange("(kt p) t -> p kt t", p=128), in_=xnT[:])],
                      Buf(), reads=B_xn)

            wring = mkring("wblk", [128, 8, 512], BF16, 4, ph)
            tf = mkring("tf", [128, N], F32, 6, ph)
            tb = mkring("tb", [128, N], BF16, 8, ph)

            def load_wblk(cb):
                wb = wring.next()
                K.dma(pool, [lambda: G.dma_start(out=wb.ap[:], in_=w_in[l][:, cb * 512:(cb + 1) * 512]
                                                 .rearrange("(kt p) n -> p kt n", p=128))], wb, writes=[wb])
                return wb

            def proj_fm(pb, wb, mt, tt):
                mm_group(pb.ap[:, 0:N], [(wb.ap[:, kt, mt * 128:(mt + 1) * 128], xnT[:, kt, tt * N:(tt + 1) * N])
                                         for kt in range(8)],
                         reads=[wb] + B_xn[tt * jpt:(tt + 1) * jpt], pbuf=pb)

            def store(dst, t):
                K.dma(pool, [lambda: G.dma_start(out=dst, in_=t.ap[:])], t, reads=[t])

            wb = load_wblk(5)
            for mt in range(4):
                for tt in range(ntt):
                    pb = ps8.next()
                    proj_fm(pb, wb, mt, tt)
                    cs = tb.next()
                    K.do(act, lambda: A.copy(cs.ap[:], pb.ap[:, 0:N]), reads=[pb], writes=[cs])
                    store(sq["CU"][mt * 128:(mt + 1) * 128, tt * N:(tt + 1) * N], cs)
            if only_cu:
                return

            uT = sb("uT", [128, 4, Ls], BF16, ph)
            B_u = [[Buf() for _ in range(ntt)] for _ in range(4)]
            wb = load_wblk(0)
            for mt in range(4):
                for tt in range(ntt):
                    pb = ps8.next()
                    proj_fm(pb, wb, mt, tt)
                    K.do(act, lambda: A.activation(out=uT[:, mt, tt * N:(tt + 1) * N], in_=pb.ap[:, 0:N],
                                                   func=AF.Gelu_apprx_tanh), reads=[pb], writes=[B_u[mt][tt]])

            wsq = Buf(sb("wsq", [128, 4, 128], BF16, ph))
            wsT = Buf(sb("wsT", [128, 4, 128], BF16, ph))
            sguc = Buf(sb("sguc", [128, 4], F32, ph))
            bsrow = Buf(sb("bsrow", [128, 4, 128], F32, ph))
            K.dma(pool, [lambda: G.dma_start(out=wsq.ap[:], in_=sgu_w[l].rearrange("h q k -> q h k"))], wsq, writes=[wsq])
            load_T(sguc, sguc.ap[:], [(0, 4, sgu_norm_g[l].rearrange("(h p) -> h p", p=128))], 4, 128)
            K.dma(sp, [lambda: S.dma_start(out=bsrow.ap[:].rearrange("p h q -> p (h q)"),
                                           in_=sgu_b[l].rearrange("h q -> (h q)").partition_broadcast(128))],
                  bsrow, writes=[bsrow])
            pb = ps8.next()
            pv = pb.ap[:].bitcast(BF16)
            K.do(pe, lambda: [T.transpose(pv[:, h * 128:(h + 1) * 128], wsq.ap[:, h, :], ident_bf[:])
                              for h in range(4)][-1], reads=[wsq, B_const], writes=[pb])
            K.do(act, lambda: A.copy(wsT.ap[:], pv[:, 0:512].rearrange("p (h q) -> p h q", h=4)),
                 reads=[pb], writes=[wsT])
            yaT = sb("yaT", [128, 4, Ls], BF16, ph)
            B_ya = [Buf() for _ in range(ntt)]
            gvr = mkring("gv", [128, 512], F32, 2, ph)
            vnr = mkring("vn", [128, 512], BF16, 2, ph)
            bnr = mkring("bnst", [128, 8], F32, 2, ph)
            wb = load_wblk(1)
            mixps = None
            for j in range(nj):
                tt, jj = divmod(j, jpt)
                if jj == 0:
                    mixps = [psB.next() for _ in range(4)]
                pb = psA.next()
                mm_group(pb.ap[:, :], [(xnT[:, kt, j * 128:(j + 1) * 128], wb.ap[:, kt, :]) for kt in range(8)],
                         reads=[wb, B_xn[j]], pbuf=pb)
                gv = gvr.next()
                K.do(act, lambda: A.activation(out=gv.ap[:], in_=pb.ap[:], func=AF.Gelu_apprx_tanh),
                     reads=[pb], writes=[gv])
                st = bnr.next()
                K.do(dve, lambda: V.bn_stats(out=st.ap[:, 0:6], in_=gv.ap[:]), reads=[gv], writes=[st])
                K.do(dve, lambda: V.bn_aggr(out=st.ap[:, 6:8], in_=st.ap[:, 0:6]), writes=[st])
                K.do(dve, lambda: V.tensor_scalar(st.ap[:, 7:8], st.ap[:, 7:8], EPS, None, ALU.add), writes=[st])
                K.do(pool, lambda: G.tensor_tensor(st.ap[:, 7:8], st.ap[:, 7:8], neg_half[:], ALU.pow),
                     reads=[B_const], writes=[st])
                vn = vnr.next()
                K.do(dve, lambda: V.tensor_scalar(vn.ap[:], gv.ap[:], st.ap[:, 6:7], st.ap[:, 7:8],
                                                  ALU.subtract, ALU.mult), reads=[gv, st], writes=[vn])
                for h in range(4):
                    mm_group(mixps[h].ap[:, jj * 128:(jj + 1) * 128], [(vn.ap[:, h * 128:(h + 1) * 128], wsT.ap[:, h, :])],
                             reads=[vn, wsT], pbuf=mixps[h], pw=(jj > 0))
                if jj == jpt - 1:
                    for h in range(4):
                        tm = tf.next()
                        bsap = bass.AP(bsrow.ap, h * 128, [[512, 128], [0, jpt], [1, 128]])
                        K.do(dve, lambda: V.scalar_tensor_tensor(
                            out=tm.ap[:].rearrange("p (a q) -> p a q", a=jpt),
                            in0=mixps[h].ap[:, 0:N].rearrange("p (a q) -> p a q", a=jpt),
                            scalar=sguc.ap[:, h:h + 1], in1=bsap, op0=ALU.mult, op1=ALU.add),
                            reads=[mixps[h], sguc, bsrow], writes=[tm])
                        K.do(dve, lambda: V.tensor_tensor(out=yaT[:, h, tt * N:(tt + 1) * N], in0=tm.ap[:],
                                                          in1=uT[:, h, tt * N:(tt + 1) * N], op=ALU.mult),
                             reads=[tm, B_u[h][tt]], pwrites=[B_ya[tt]])

            cw = Buf(sb("cw", [128, 3, 4], F32, ph))
            load_T(cw, cw.ap[:].rearrange("p j m -> p (j m)"), [(0, 12, conv_w[l].rearrange("j (m p) -> (j m) p", p=128))], 12, 128)
            ybT = sb("ybT", [128, 4, Ls], BF16, ph)
            B_yb = [Buf() for _ in range(ntt)]
            wbb, wbc, wbh = load_wblk(2), load_wblk(3), load_wblk(4)
            for mt in range(4):
                for tt in range(ntt):
                    pc_, ph_, pb_ = ps8.next(), ps8.next(), ps8.next()
                    proj_fm(pc_, wbc, mt, tt)
                    proj_fm(ph_, wbh, mt, tt)
                    proj_fm(pb_, wbb, mt, tt)
                    bcs = tf.next()
                    K.do(act, lambda: A.copy(bcs.ap[:], pc_.ap[:, 0:N]), reads=[pc_], writes=[bcs])
                    Pt = tf.next()
                    K.do(dve, lambda: V.tensor_tensor(out=Pt.ap[:], in0=bcs.ap[:], in1=ph_.ap[:, 0:N], op=ALU.mult),
                         reads=[bcs, ph_], writes=[Pt])
                    Tt = tf.next()
                    K.do(dve, lambda: V.tensor_scalar(Tt.ap[:], Pt.ap[:], cw.ap[:, 1, mt:mt + 1], None, ALU.mult),
                         reads=[Pt, cw], writes=[Tt])
                    if grid:
                        Tv = Tt.ap[:].rearrange("p (r c) -> p r c", c=64)
                        Pv = Pt.ap[:].rearrange("p (r c) -> p r c", c=64)
                        o1, i1, o2, i2 = Tv[:, :, 1:64], Pv[:, :, 0:63], Tv[:, :, 0:63], Pv[:, :, 1:64]
                    else:
                        o1, i1, o2, i2 = Tt.ap[:, 1:N], Pt.ap[:, 0:N - 1], Tt.ap[:, 0:N - 1], Pt.ap[:, 1:N]
                    K.do(dve, lambda: V.scalar_tensor_tensor(out=o1, in0=i1, scalar=cw.ap[:, 0, mt:mt + 1], in1=o1,
                                                             op0=ALU.mult, op1=ALU.add), reads=[Pt], writes=[Tt])
                    K.do(dve, lambda: V.scalar_tensor_tensor(out=o2, in0=i2, scalar=cw.ap[:, 2, mt:mt + 1], in1=o2,
                                                             op0=ALU.mult, op1=ALU.add), reads=[Pt], writes=[Tt])
                    K.do(dve, lambda: V.tensor_tensor(out=ybT[:, mt, tt * N:(tt + 1) * N], in0=Tt.ap[:],
                                                      in1=pb_.ap[:, 0:N], op=ALU.mult),
                         reads=[Tt, pb_], pwrites=[B_yb[tt]])

            wbr = [Buf(sb(f"wbr{i}", [128, 4, D], BF16, ph)) for i in range(2)]
            for i in range(2):
                K.dma(pool, [lambda: G.dma_start(out=wbr[i].ap[:], in_=w_branch[l, i]
                                                 .rearrange("(kt p) n -> p kt n", p=128))], wbr[i], writes=[wbr[i]])
            for half in range(2):
                wga, wgb, wgc = load_wblk(6 + half), load_wblk(8 + half), load_wblk(10 + half)
                for dtl in range(4):
                    dt_ = half * 4 + dtl
                    for tt in range(ntt):
                        pga, pgb, pgc, pba, pbb = [ps8.next() for _ in range(5)]
                        proj_fm(pga, wga, dtl, tt)
                        proj_fm(pgb, wgb, dtl, tt)
                        proj_fm(pgc, wgc, dtl, tt)
                        mm_group(pba.ap[:, 0:N], [(wbr[0].ap[:, kt, dt_ * 128:(dt_ + 1) * 128],
                                                   yaT[:, kt, tt * N:(tt + 1) * N]) for kt in range(4)],
                                 reads=[wbr[0], B_ya[tt]], pbuf=pba)
                        mm_group(pbb.ap[:, 0:N], [(wbr[1].ap[:, kt, dt_ * 128:(dt_ + 1) * 128],
                                                   ybT[:, kt, tt * N:(tt + 1) * N]) for kt in range(4)],
                                 reads=[wbr[1], B_yb[tt]], pbuf=pbb)
                        ga, gb, gc = tb.next(), tb.next(), tb.next()
                        for gg, pp in ((ga, pga), (gb, pgb), (gc, pgc)):
                            K.do(act, lambda: A.activation(out=gg.ap[:], in_=pp.ap[:, 0:N], func=AF.Sigmoid),
                                 reads=[pp], writes=[gg])
                        store(sq["GC"][dt_ * 128:(dt_ + 1) * 128, tt * N:(tt + 1) * N], gc)
                        m1, m2 = tf.next(), tf.next()
                        K.do(dve, lambda: V.tensor_tensor(out=m1.ap[:], in0=ga.ap[:], in1=pba.ap[:, 0:N], op=ALU.mult),
                             reads=[ga, pba], writes=[m1])
                        K.do(dve, lambda: V.tensor_tensor(out=m2.ap[:], in0=gb.ap[:], in1=pbb.ap[:, 0:N], op=ALU.mult),
                             reads=[gb, pbb], writes=[m2])
                        mab = tb.next()
                        K.do(pool, lambda: G.tensor_tensor(out=mab.ap[:], in0=m1.ap[:], in1=m2.ap[:], op=ALU.add),
                             reads=[m1, m2], writes=[mab])
                        store(sq["MAB"][dt_ * 128:(dt_ + 1) * 128, tt * N:(tt + 1) * N], mab)

    def phase_s5_tables(l):
        with scope() as po:
            Kacc = Buf(sb("Kacc", [128, 32, 128], F32, po))
            maskf = sb("maskf", [128, 128], F32, po)
            maskb = sb("maskb", [128, 128], F32, po)
            dcol = Buf(sb("dcol", [128, 32], F32, po))
            Bm = Buf()

            def emit_masks():
                G.memset(maskf[:], 0.0)
                G.affine_select(out=maskf[:].rearrange("p (t h) -> p t h", h=16), in_=maskf[:].rearrange("p (t h) -> p t h", h=16),
                                compare_op=ALU.is_ge, fill=1.0, base=-16, pattern=[[-16, 8], [0, 16]], channel_multiplier=1)
                G.memset(maskb[:], 1.0)
                return G.affine_select(out=maskb[:].rearrange("p (t h) -> p t h", h=16), in_=maskb[:].rearrange("p (t h) -> p t h", h=16),
                                       compare_op=ALU.is_ge, fill=0.0, base=0, pattern=[[-16, 8], [0, 16]], channel_multiplier=1)
            K.do(pool, emit_masks, writes=[Bm])
            with scope() as tmpd:
                dstg = Buf(sb("dstg", [32, 8, 16], F32, tmpd))
                K.dma(sp, [lambda k=k: S.dma_start(out=dstg.ap[:, k, :], in_=s5_d[l].rearrange("(g h) -> g h", h=16))
                           for k in range(8)], dstg, writes=[dstg])
                pbd = ps8.next()
                K.do(pe, lambda: T.transpose(pbd.ap[:, 0:32], dstg.ap[:].rearrange("g k h -> g (k h)"), ident_f[0:32, 0:32]),
                     reads=[dstg, B_const], writes=[pbd])
                K.do(act, lambda: A.copy(dcol.ap[:], pbd.ap[:, 0:32]), reads=[pbd], writes=[dcol])
            for d in range(2):
                with scope() as ph:
                    def t(name, shape, dt=F32):
                        return Buf(sb(name, shape, dt, ph))
                    lre, lim, dtb = t("lre", [64, 32]), t("lim", [64, 32]), t("dtb", [64, 32])
                    load_T(lre, lre.ap[:], [(0, 32, s5_lam_re[l, d])], 32, 64)
                    load_T(lim, lim.ap[:], [(0, 32, s5_lam_im[l, d])], 32, 64)
                    K.dma(sp, [lambda: S.dma_start(out=dtb.ap[:], in_=s5_log_dt[l, d].partition_broadcast(64))],
                          dtb, writes=[dtb])
                    K.do(act, lambda: A.activation(out=dtb.ap[:], in_=dtb.ap[:], func=AF.Exp), writes=[dtb])
                    X, ANG = t("X", [64, 32]), t("ANG", [64, 32])
                    K.do(dve, lambda: V.tensor_tensor(out=X.ap[:], in0=lre.ap[:], in1=dtb.ap[:], op=ALU.mult),
                         reads=[lre, dtb], writes=[X])
                    K.do(dve, lambda: V.tensor_tensor(out=ANG.ap[:], in0=lim.ap[:], in1=dtb.ap[:], op=ALU.mult),
                         reads=[lim, dtb], writes=[ANG])
                    EV = t("EV", [64, 16])
                    K.do(pool, lambda: [G.memset(EV.ap[:, i:i + 1], float(i - 7)) for i in range(16)][-1], writes=[EV])

                    def outer(dst, src):
                        in0 = bass.AP(src.ap, 0, [[32, 64], [1, 32], [0, 16]])
                        in1 = bass.AP(EV.ap, 0, [[16, 64], [0, 32], [1, 16]])
                        K.do(dve, lambda: V.tensor_tensor(out=dst.ap[:], in0=in0, in1=in1, op=ALU.mult),
                             reads=[src, EV], writes=[dst])
                    XE, AE = t("XE", [64, 32, 16]), t("AE", [64, 32, 16])
                    outer(XE, X)
                    outer(AE, ANG)
                    MAG = t("MAG", [64, 32, 16])
                    K.do(act, lambda: A.activation(out=MAG.ap[:], in_=XE.ap[:], func=AF.Exp), reads=[XE], writes=[MAG])
                    PRE, PIM = t("PRE", [64, 32, 16]), t("PIM", [64, 32, 16])
                    YI = t("YI", [64, 32, 16], I32)
                    YF, FR, MK = t("YF", [64, 32, 16]), t("FR", [64, 32, 16]), t("MK", [64, 32, 16])
                    for (dst, off) in ((PIM, 64.0), (PRE, 64.25)):
                        K.do(dve, lambda: V.tensor_scalar(FR.ap[:], AE.ap[:], 1.0 / TWO_PI, off, ALU.mult, ALU.add),
                             reads=[AE], writes=[FR])
                        K.do(dve, lambda: V.tensor_copy(YI.ap[:], FR.ap[:]), reads=[FR], writes=[YI])
                        K.do(dve, lambda: V.tensor_copy(YF.ap[:], YI.ap[:]), reads=[YI], writes=[YF])
                        K.do(dve, lambda: V.tensor_tensor(out=FR.ap[:], in0=FR.ap[:], in1=YF.ap[:], op=ALU.subtract),
                             reads=[YF], writes=[FR])
                        K.do(dve, lambda: V.tensor_scalar(MK.ap[:], FR.ap[:], 0.5, None, ALU.is_gt), reads=[FR], writes=[MK])
                        K.do(dve, lambda: V.tensor_tensor(out=FR.ap[:], in0=FR.ap[:], in1=MK.ap[:], op=ALU.subtract),
                             reads=[MK], writes=[FR])
                        K.do(dve, lambda: V.tensor_scalar(FR.ap[:], FR.ap[:], -0.5, 0.5, ALU.max, ALU.min), writes=[FR])
                        K.do(act, lambda: A.activation(out=dst.ap[:], in_=FR.ap[:], func=AF.Sin, scale=TWO_PI),
                             reads=[FR], writes=[dst])
                        K.do(dve, lambda: V.tensor_tensor(out=dst.ap[:], in0=dst.ap[:], in1=MAG.ap[:], op=ALU.mult),
                             reads=[MAG], writes=[dst])
                    ld = l * 2 + d
                    K.do(dve, lambda: [V.tensor_copy(a8_ar[:, ld, 0, :], PRE.ap[:, :, 15]),
                                       V.tensor_copy(a8_ar[:, ld, 1, :], PRE.ap[:, :, 15]),
                                       V.tensor_scalar(a8_ai[:, ld, 0, :], PIM.ap[:, :, 15], -1.0, None, ALU.mult),
                                       V.tensor_copy(a8_ai[:, ld, 1, :], PIM.ap[:, :, 15])][-1],
                         reads=[PRE, PIM], pwrites=[B_a8])
                    den, nr, fre, fim, t1, t2 = (t(n, [64, 32]) for n in ("den", "nr", "fre", "fim", "t1s", "t2s"))

                    def tt_(out, a, b, op, rd=(), wr=()):
                        K.do(dve, lambda: V.tensor_tensor(out=out, in0=a, in1=b, op=op), reads=list(rd), writes=list(wr))
                    tt_(den.ap[:], lre.ap[:], lre.ap[:], ALU.mult, [lre], [den])
                    tt_(t1.ap[:], lim.ap[:], lim.ap[:], ALU.mult, [lim], [t1])
                    tt_(den.ap[:], den.ap[:], t1.ap[:], ALU.add, [t1], [den])
                    K.do(dve, lambda: V.reciprocal(den.ap[:], den.ap[:]), writes=[den])
                    K.do(dve, lambda: V.tensor_scalar(nr.ap[:], PRE.ap[:, :, 8], -1.0, None, ALU.add), reads=[PRE], writes=[nr])
                    tt_(fre.ap[:], nr.ap[:], lre.ap[:], ALU.mult, [nr, lre], [fre])
                    tt_(t1.ap[:], PIM.ap[:, :, 8], lim.ap[:], ALU.mult, [PIM, lim], [t1])
                    tt_(fre.ap[:], fre.ap[:], t1.ap[:], ALU.add, [t1], [fre])
                    tt_(fre.ap[:], fre.ap[:], den.ap[:], ALU.mult, [den], [fre])
                    tt_(fim.ap[:], PIM.ap[:, :, 8], lre.ap[:], ALU.mult, [PIM, lre], [fim])
                    tt_(t2.ap[:], nr.ap[:], lim.ap[:], ALU.mult, [nr, lim], [t2])
                    tt_(fim.ap[:], fim.ap[:], t2.ap[:], ALU.subtract, [t2], [fim])
                    tt_(fim.ap[:], fim.ap[:], den.ap[:], ALU.mult, [den], [fim])
                    Bre, Bim = t("Bre", [64, 32, 16]), t("Bim", [64, 32, 16])
                    K.dma(sp, [lambda: S.dma_start(out=Bre.ap[:], in_=s5_b_re[l, d].rearrange("g p h -> p g h"))], Bre, writes=[Bre])
                    K.dma(sp, [lambda: S.dma_start(out=Bim.ap[:], in_=s5_b_im[l, d].rearrange("g p h -> p g h"))], Bim, writes=[Bim])
                    BBre, BBim, tq = t("BBre", [64, 32, 16]), t("BBim", [64, 32, 16]), t("tq", [64, 32, 16])

                    def fb(fsrc):
                        return bass.AP(fsrc.ap, 0, [[32, 64], [1, 32], [0, 16]])

                    def cmul(ore, oim, are, aim, bre, bim, tmp, rd):
                        tt_(ore[1], are, bre, ALU.mult, rd, [ore[0]])
                        tt_(tmp[1], aim, bim, ALU.mult, rd, [tmp[0]])
                        tt_(ore[1], ore[1], tmp[1], ALU.subtract, [tmp[0]], [ore[0]])
                        tt_(oim[1], are, bim, ALU.mult, rd, [oim[0]])
                        tt_(tmp[1], aim, bre, ALU.mult, rd, [tmp[0]])
                        tt_(oim[1], oim[1], tmp[1], ALU.add, [tmp[0]], [oim[0]])
                    cmul((BBre, BBre.ap[:]), (BBim, BBim.ap[:]), fb(fre), fb(fim), Bre.ap[:], Bim.ap[:], (tq, tq.ap[:]),
                         [fre, fim, Bre, Bim])
                    CTre, CTim = t("CTre", [64, 512]), t("CTim", [64, 512])
                    cin = t("cin", [128, 4, 64])
                    for (csrc, cdst) in ((s5_c_re, CTre), (s5_c_im, CTim)):
                        K.dma(sp, [lambda: S.dma_start(out=cin.ap[:], in_=csrc[l, d].rearrange("g h p -> (g h) p")
                                                       .rearrange("(a r) p -> r a p", r=128))], cin, writes=[cin])
                        pb = ps8.next()
                        K.do(pe, lambda: [T.transpose(pb.ap[0:64, a * 128:(a + 1) * 128], cin.ap[:, a, :], ident_f[:])
                                          for a in range(4)][-1], reads=[cin, B_const], writes=[pb])
                        K.do(act, lambda: A.copy(cdst.ap[:], pb.ap[0:64, :]), reads=[pb], writes=[cdst])
                    W1Tre, W1Tim, tw = t("W1Tre", [64, 32, 8, 16]), t("W1Tim", [64, 32, 8, 16]), t("tw", [64, 32, 8, 16])

                    def pview(Psrc, start, step):
                        return bass.AP(Psrc.ap, start, [[512, 64], [16, 32], [step, 8], [0, 16]])

                    def bview(Bsrc):
                        return bass.AP(Bsrc.ap, 0, [[512, 64], [16, 32], [0, 8], [1, 16]])
                    s1, st1 = (14, -1) if d == 0 else (7, 1)
                    cmul((W1Tre, W1Tre.ap[:]), (W1Tim, W1Tim.ap[:]), pview(PRE, s1, st1), pview(PIM, s1, st1),
                         bview(BBre), bview(BBim), (tw, tw.ap[:]), [PRE, PIM, BBre, BBim])
                    W2f = t("W2f", [64, 32, 2, 8, 16])
                    W2m = t("W2m", [64, 32, 2, 8, 16])

                    def w2build(dst, s2, st2):
                        ore = bass.AP(dst.ap, 0, [[8192, 64], [256, 32], [16, 8], [1, 16]])
                        oim = bass.AP(dst.ap, 128, [[8192, 64], [256, 32], [16, 8], [1, 16]])
                        cmul((dst, ore), (dst, oim), pview(PRE, s2, st2), pview(PIM, s2, st2),
                             bview(CTre), bview(CTim), (tw, tw.ap[:]), [PRE, PIM, CTre, CTim])
                        K.do(dve, lambda: V.tensor_scalar(oim, oim, -1.0, None, ALU.mult), writes=[dst])
                    s2, st2 = (8, 1) if d == 0 else (15, -1)
                    w2build(W2f, s2, st2)
                    s3, st3 = (0, 1) if d == 0 else (7, -1)
                    w2build(W2m, s3, st3)
                    W2b = t("W2b", [64, 32, 2, 128], BF16)
                    K.do(act, lambda: A.copy(W2b.ap[:].rearrange("p g r x -> p (g r x)"),
                                             W2f.ap[:].rearrange("p g r t h -> p (g r t h)")), reads=[W2f], writes=[W2b])
                    K.dma(sp, [lambda: S.dma_start(out=W2D[l, d], in_=W2b.ap[:])], W2b, reads=[W2b])
                    mask = maskf if d == 0 else maskb
                    tkr = mkring("tk", [128, 4, 128], F32, 2, ph)
                    for g4 in range(8):
                        pb = ps8.next()
                        for gg in range(4):
                            g = g4 * 4 + gg
                            mm_group(pb.ap[:, gg * 128:(gg + 1) * 128],
                                     [(W1Tre.ap[:, g].rearrange("p k h -> p (k h)"), W2m.ap[:, g, 0].rearrange("p t h -> p (t h)")),
                                      (W1Tim.ap[:, g].rearrange("p k h -> p (k h)"), W2m.ap[:, g, 1].rearrange("p t h -> p (t h)"))],
                                     reads=[W1Tre, W1Tim, W2m], pbuf=pb, pw=(gg > 0))
                        mk = bass.AP(mask, 0, [[128, 128], [0, 4], [1, 128]])
                        kv = Kacc.ap[:, g4 * 4:(g4 + 1) * 4, :]
                        pv3 = pb.ap[:].rearrange("p (a x) -> p a x", a=4)
                        if d == 0:
                            K.do(dve, lambda: V.tensor_tensor(out=kv, in0=pv3, in1=mk, op=ALU.mult),
                                 reads=[pb, Bm], pwrites=[Kacc])
                        else:
                            tk = tkr.next()
                            K.do(dve, lambda: V.tensor_tensor(out=tk.ap[:], in0=pv3, in1=mk, op=ALU.mult),
                                 reads=[pb, Bm], writes=[tk])
                            K.do(dve, lambda: V.tensor_tensor(out=kv, in0=kv, in1=tk.ap[:], op=ALU.add),
                                 reads=[tk, Kacc], pwrites=[Kacc])
                    W1b = t("W1b", [128, 32, 128], BF16)
                    for g4 in range(8):
                        pb = ps8.next()

                        def emit_tr():
                            ins = None
                            for gg in range(4):
                                g = g4 * 4 + gg
                                for ri, src in enumerate((W1Tre, W1Tim)):
                                    ins = T.transpose(pb.ap[:, gg * 128 + ri * 64: gg * 128 + ri * 64 + 64],
                                                      src.ap[:, g].rearrange("p k h -> p (k h)"), ident_f[0:64, 0:64])
                            return ins
                        K.do(pe, emit_tr, reads=[W1Tre, W1Tim, B_const], writes=[pb])
                        K.do(act, lambda: A.copy(W1b.ap[:, g4 * 4:(g4 + 1) * 4, :], pb.ap[:].rearrange("p (a x) -> p a x", a=4)),
                             reads=[pb], pwrites=[W1b])
                    K.dma(sp, [lambda: S.dma_start(out=W1D[l, d], in_=W1b.ap[:])], W1b, reads=[W1b])
            Kb = Buf(sb("Kb", [128, 32, 128], BF16, po))
            for g in range(32):
                K.do(dve, lambda: V.scalar_tensor_tensor(out=Kacc.ap[:, g, :], in0=ident_f[:], scalar=dcol.ap[:, g:g + 1],
                                                         in1=Kacc.ap[:, g, :], op0=ALU.mult, op1=ALU.add),
                     reads=[B_const, dcol, Kacc], pwrites=[Kacc])
            K.do(act, lambda: A.copy(Kb.ap[:], Kacc.ap[:]), reads=[Kacc], writes=[Kb])
            K.dma(sp, [lambda: S.dma_start(out=KMD[l], in_=Kb.ap[:])], Kb, reads=[Kb])

    def phase_s5(l, sq):
        Ls = sq["L"]
        C = Ls // 8
        b = sq["b"]
        with scope() as ph:
            U = sb("U", [128, 32, C], BF16, ph)
            B_U = Buf()
            EBm = sb("EBm", [64, 2, C, 2, 32], BF16, ph)
            EB = [EBm[:, d] for d in range(2)]
            B_E = [Buf(), Buf()]
            B_H = [Buf(), Buf()]
            with scope() as pa:
                cuT = Buf(sb("cuT", [128, 4, Ls], BF16, pa))
                W1 = Buf(sb("W1", [128, 2, 32, 128], BF16, pa))
                SEL = Buf(sb("SEL", [128, 8, 8, 128], BF16, pa))
                K.dma(sp, [lambda: S.dma_start(out=cuT.ap[:], in_=sq["CU"].rearrange("(m p) t -> p m t", p=128))], cuT, writes=[cuT])
                K.dma(sp, [lambda d=d: S.dma_start(out=W1.ap[:, d], in_=W1D[l, d]) for d in range(2)], W1, writes=[W1])
                K.dma(sp, [lambda: S.dma_start(out=SEL.ap[:], in_=SELD)], SEL, writes=[SEL])
                for g in range(32):
                    mt, gl = divmod(g, 8)
                    pb = ps8.next()
                    mm_group(pb.ap[:, 0:C], [(SEL.ap[:, gl, k, :], cuT.ap[:, mt, k::8]) for k in range(8)],
                             reads=[SEL, cuT], pbuf=pb)
                    if g % 2 == 0:
                        K.do(act, lambda: A.copy(U[:, g, :], pb.ap[:, 0:C]), reads=[pb], pwrites=[B_U])
                    else:
                        K.do(dve, lambda: V.tensor_copy(U[:, g, :], pb.ap[:, 0:C]), reads=[pb], pwrites=[B_U])
                for d in range(2):
                    for g in range(32):
                        pb = ps8.next()
                        mm_group(pb.ap[:, 0:C], [(W1.ap[:, d, g, :], U[:, g, :])], reads=[W1, B_U], pbuf=pb)
                        K.do(act, lambda: A.copy(EB[d][:, :, 0, g], pb.ap[0:64, 0:C]), reads=[pb], pwrites=[B_E[d]])
                        K.do(dve, lambda: V.tensor_copy(EB[d][:, :, 1, g], pb.ap[64:128, 0:C]), reads=[pb], pwrites=[B_E[d]])
            with scope() as pscan:
                sring = mkring("sst", [64, 128], F32, 4, pscan)
                t1 = Buf(sb("sc_t1", [64, 128], F32, pscan))
                t2 = Buf(sb("sc_t2", [64, 128], F32, pscan))
                ar = a8_ar[:, 2 * l:2 * l + 2].rearrange("p d a g -> p (d a g)")
                ai4 = a8_ai[:, 2 * l:2 * l + 2]
                h0v = h0st[:, 2 * b:2 * b + 2].rearrange("p d a g -> p (d a g)")
                sprev = sring.next()
                if sq["ctx"]:
                    K.do(dve, lambda: V.memset(sprev.ap[:], 0.0), writes=[sprev])
                else:
                    K.do(dve, lambda: V.tensor_copy(sprev.ap[:], h0v), reads=[B_h0], writes=[sprev])
                for i in range(C):
                    snew = sring.next()
                    ev_ap = bass.AP(EBm, i * 64, [[2 * C * 64, 64], [(2 * C - 1 - 2 * i) * 64, 2], [1, 64]])
                    swp = bass.AP(sprev.ap, 32, [[128, 64], [64, 2], [-32, 2], [1, 32]])

                    def step():
                        V.tensor_tensor(out=t1.ap[:], in0=sprev.ap[:], in1=ar, op=ALU.mult)
                        V.tensor_tensor(out=t2.ap[:].rearrange("p (d a g) -> p d a g", d=2, a=2), in0=swp, in1=ai4, op=ALU.mult)
                        V.tensor_tensor(out=t1.ap[:], in0=t1.ap[:], in1=t2.ap[:], op=ALU.add)
                        return V.tensor_tensor(out=snew.ap[:].rearrange("p (d x) -> p d x", d=2),
                                               in0=t1.ap[:].rearrange("p (d x) -> p d x", d=2), in1=ev_ap, op=ALU.add)
                    ev4 = K.do(dve, step, reads=[sprev, B_a8, B_E[0], B_E[1]], writes=[snew, t1, t2])
                    K.do(act, lambda: A.copy(ev_ap, sprev.ap[:].rearrange("p (d x) -> p d x", d=2)),
                         reads=[sprev], pwrites=[B_H[0], B_H[1]], after=[ev4])
                    sprev = snew
                if sq["ctx"]:
                    K.do(dve, lambda: V.tensor_copy(h0v, sprev.ap[:]), reads=[sprev], pwrites=[B_h0])
            with scope() as rd:
                W2 = Buf(sb("W2", [64, 2, 32, 2, 128], BF16, rd))
                KM = Buf(sb("KM", [128, 32, 128], BF16, rd))
                SELT = Buf(sb("SELT", [128, 8, 8, 128], BF16, rd))
                Y = sb("Y", [128, 32, C], BF16, rd)
                B_Y = Buf()
                zT = Buf(sb("zT", [128, 4, Ls], BF16, rd))
                K.dma(sp, [lambda d=d: S.dma_start(out=W2.ap[:, d], in_=W2D[l, d]) for d in range(2)], W2, writes=[W2])
                K.dma(sp, [lambda: S.dma_start(out=KM.ap[:], in_=KMD[l])], KM, writes=[KM])
                K.dma(sp, [lambda: S.dma_start(out=SELT.ap[:], in_=SELTD)], SELT, writes=[SELT])
                for g in range(32):
                    pb = ps8.next()
                    pairs = [(KM.ap[:, g, :], U[:, g, :])]
                    for d in range(2):
                        for ri in range(2):
                            pairs.append((W2.ap[:, d, g, ri, :], EB[d][:, :, ri, g]))
                    mm_group(pb.ap[:, 0:C], pairs, reads=[KM, W2, B_U, B_H[0], B_H[1]], pbuf=pb)
                    if g % 2 == 0:
                        K.do(act, lambda: A.copy(Y[:, g, :], pb.ap[:, 0:C]), reads=[pb], pwrites=[B_Y])
                    else:
                        K.do(dve, lambda: V.tensor_copy(Y[:, g, :], pb.ap[:, 0:C]), reads=[pb], pwrites=[B_Y])
                for mt in range(4):
                    for tau in range(8):
                        pb = ps8.next()
                        mm_group(pb.ap[:, 0:C], [(SELT.ap[:, gl, tau, :], Y[:, mt * 8 + gl, :]) for gl in range(8)],
                                 reads=[SELT, B_Y], pbuf=pb)
                        K.do(act, lambda: A.activation(out=zT.ap[:, mt, tau::8], in_=pb.ap[:, 0:C], func=AF.Gelu_apprx_tanh),
                             reads=[pb], pwrites=[zT])
                K.dma(sp, [lambda: S.dma_start(out=sq["YC"].rearrange("(m p) t -> p m t", p=128), in_=zT.ap[:])],
                      zT, reads=[zT])

    def phase_back(l, sq):
        Ls = sq["L"]
        N = min(512, Ls)
        ntt = Ls // N
        jpt = N // 128
        src = seq_src(l, sq)
        with scope() as ph:
            G1b = Buf(sb("g1b", [128, D], F32, ph))
            K.dma(sp, [lambda: S.dma_start(out=G1b.ap[:], in_=MOD[l, sq["modrow"], 2 * D:3 * D].partition_broadcast(128))],
                  G1b, writes=[G1b])
            G2b, SH2b = make_mod_tiles(l, sq["modrow"], norm2_g[l], 4 * D, 3 * D, ph, "m2")
            gluw = Buf(sb("gluw", [128, 4, WM], BF16, ph))
            wbr2 = Buf(sb("wbr2", [128, 4, D], BF16, ph))
            wout = Buf(sb("wout", [128, 8, D], BF16, ph))
            glub = Buf(sb("glub", [128, 4], F32, ph))
            K.dma(pool, [lambda: G.dma_start(out=gluw.ap[:], in_=glu_w[l].rearrange("(kt p) n -> p kt n", p=128))], gluw, writes=[gluw])
            K.dma(pool, [lambda: G.dma_start(out=wbr2.ap[:], in_=w_branch[l, 2].rearrange("(kt p) n -> p kt n", p=128))], wbr2, writes=[wbr2])
            K.dma(pool, [lambda: G.dma_start(out=wout.ap[:], in_=w_out[l].rearrange("(kt p) n -> p kt n", p=128))], wout, writes=[wout])
            load_T(glub, glub.ap[:], [(0, 4, glu_b[l].rearrange("(m p) -> m p", p=128))], 4, 128)
            zr = mkring("zin", [128, 4, N], BF16, 2, ph)
            mabr = mkring("mabin", [128, 8, N], BF16, 2, ph)
            gcr = mkring("gcin", [128, 8, N], BF16, 2, ph)
            ycr = mkring("ycT", [128, 4, N], BF16, 2, ph)
            mgr = mkring("mgT", [128, 8, N], BF16, 2, ph)
            tf = mkring("tfb", [128, N], F32, 3, ph)
            tb = mkring("tbb", [128, N], BF16, 3, ph)
            hr = mkring("hin", [128, D], F32, 2, ph)
            hnr = mkring("hn", [128, D], F32, 2, ph)
            tmpb = Buf(sb("tmp2", [128, D], F32, ph))
            xnr = mkring("xn2", [128, D], BF16, 2, ph)
            ssr = mkring("ss2", [128, 2], F32, 2, ph)
            xstr = mkring("xst", [128, 8, N], BF16, 2, ph)
            for tt in range(ntt):
                ts = slice(tt * N, (tt + 1) * N)
                zt, mabt, gct = zr.next(), mabr.next(), gcr.next()
                K.dma(sp, [lambda: S.dma_start(out=zt.ap[:], in_=sq["YC"][:, ts].rearrange("(m p) t -> p m t", p=128))], zt, writes=[zt])
                K.dma(sp, [lambda: S.dma_start(out=mabt.ap[:], in_=sq["MAB"][:, ts].rearrange("(m p) t -> p m t", p=128))], mabt, writes=[mabt])
                K.dma(sp, [lambda: S.dma_start(out=gct.ap[:], in_=sq["GC"][:, ts].rearrange("(m p) t -> p m t", p=128))], gct, writes=[gct])
                yc = ycr.next()
                for mt in range(4):
                    pb = ps8.next()
                    mm_group(pb.ap[:, 0:N], [(gluw.ap[:, kt, mt * 128:(mt + 1) * 128], zt.ap[:, kt, :]) for kt in range(4)],
                             reads=[gluw, zt], pbuf=pb)
                    sg = tb.next()
                    K.do(act, lambda: A.activation(out=sg.ap[:], in_=pb.ap[:, 0:N], func=AF.Sigmoid, bias=glub.ap[:, mt:mt + 1]),
                         reads=[pb, glub], writes=[sg])
                    K.do(dve, lambda: V.tensor_tensor(out=yc.ap[:, mt, :], in0=zt.ap[:, mt, :], in1=sg.ap[:], op=ALU.mult),
                         reads=[zt, sg], **(dict(writes=[yc]) if mt == 0 else dict(pwrites=[yc])))
                mg = mgr.next()
                for dt_ in range(8):
                    pb = ps8.next()
                    mm_group(pb.ap[:, 0:N], [(wbr2.ap[:, kt, dt_ * 128:(dt_ + 1) * 128], yc.ap[:, kt, :]) for kt in range(4)],
                             reads=[wbr2, yc], pbuf=pb)
                    m = tf.next()
                    K.do(dve, lambda: V.tensor_tensor(out=m.ap[:], in0=gct.ap[:, dt_, :], in1=pb.ap[:, 0:N], op=ALU.mult),
                         reads=[gct, pb], writes=[m])
                    K.do(pool, lambda: G.tensor_tensor(out=mg.ap[:, dt_, :], in0=m.ap[:], in1=mabt.ap[:, dt_, :], op=ALU.add),
                         reads=[m, mabt], **(dict(writes=[mg]) if dt_ == 0 else dict(pwrites=[mg])))
                xst = xstr.next()
                for jj in range(jpt):
                    r0 = tt * N + jj * 128
                    ht = hr.next()
                    K.dma(sp, [lambda: S.dma_start(out=ht.ap[:], in_=src[r0:r0 + 128, :])], ht, writes=[ht])
                    hn = hnr.next()
                    for half in range(2):
                        pb = ps8.next()
                        mm_group(pb.ap[:, :], [(mg.ap[:, kt, jj * 128:(jj + 1) * 128], wout.ap[:, kt, half * 512:(half + 1) * 512])
                                               for kt in range(8)], reads=[mg, wout], pbuf=pb)
                        hs = slice(half * 512, (half + 1) * 512)
                        K.do(dve, lambda: V.tensor_tensor(out=hn.ap[:, hs], in0=pb.ap[:, :], in1=G1b.ap[:, hs], op=ALU.mult),
                             reads=[pb, G1b], **(dict(writes=[hn]) if half == 0 else dict(pwrites=[hn])))
                    K.do(dve, lambda: V.tensor_tensor(out=hn.ap[:], in0=hn.ap[:], in1=ht.ap[:], op=ALU.add),
                         reads=[ht], writes=[hn])
                    K.dma(pool, [lambda: G.dma_start(out=sq["HA"][r0:r0 + 128, :], in_=hn.ap[:])], hn, reads=[hn])
                    xn = xnr.next()
                    ss = ssr.next()
                    rms_mod(hn, G2b, SH2b, xn, tmpb, ss)
                    K.dma(pool, [lambda: G.dma_start(out=sq["XN2R"][r0:r0 + 128, :], in_=xn.ap[:])], xn, reads=[xn])
                    transpose_tile(xn, xst.ap[:, :, jj * 128:(jj + 1) * 128], xst, pw=(jj > 0))
                K.dma(pool, [lambda: G.dma_start(out=sq["XN2T"][:, ts].rearrange("(k p) t -> p k t", p=128), in_=xst.ap[:])],
                      xst, reads=[xst])

    def phase_moe_sparse(l, seq_list):
        last = (l == DEPTH - 1)
        blocks = []
        ctxs = [sq for sq in seq_list if sq["ctx"]]
        if ctxs and not last:
            blocks.append([(sq, 0, LC) for sq in ctxs])
        for sq in seq_list:
            if not sq["ctx"]:
                for tt in range(LS // 512):
                    blocks.append([(sq, tt * 512, 512)])
        tiles = []
        for blk in blocks:
            for (sq, t0, n) in blk:
                for jj in range(n // 128):
                    tiles.append((sq, t0 + jj * 128))
        NJ = len(tiles)
        TL = (2 * NJ * 128 + NE * 511) // 512
        assert TL <= TMAX

        def bl(ap_t, off, pstride, dims):
            return bass.AP(ap_t, off, [[pstride, 128]] + dims)

        with scope() as ph:
            Q1 = Buf(sb("Q1", [128, NJ, NE], F32, ph))
            Q2 = Buf(sb("Q2", [128, NJ, NE], F32, ph))
            GW = Buf(sb("GW", [128, NJ, NE], F32, ph))
            slot_i = Buf(sb("slot_i", [128, 2, NJ], I32, ph))
            wsel = Buf(sb("wsel", [128, 2, NJ], F32, ph))
            idxW = Buf(sb("idxW", [128, TL], I32, ph))
            with scope() as p1:
                rw = Buf(sb("rw", [128, 8, NE], BF16, p1))
                rbb = Buf(sb("rbb", [128, 4, NE], F32, p1))
                K.dma(pool, [lambda: G.dma_start(out=rw.ap[:], in_=router_w.rearrange("(kt p) e -> p kt e", p=128))], rw, writes=[rw])
                K.dma(sp, [lambda j=j: S.dma_start(out=rbb.ap[:, j, :], in_=router_b.partition_broadcast(128)) for j in range(4)],
                      rbb, writes=[rbb])
                xTr = mkring("xT", [128, 8, 512], BF16, 2, p1)
                sc = Buf(sb("r_sc", [128, 4, NE], F32, p1))
                bb = Buf(sb("r_b", [128, 4, NE], F32, p1))
                b2 = Buf(sb("r_b2", [128, 4, NE], F32, p1))
                q1 = Buf(sb("r_q1", [128, 4, NE], F32, p1))
                m16 = Buf(sb("r_m16", [128, 16], F32, p1))
                m16b = Buf(sb("r_m16b", [128, 16], F32, p1))
                m4 = Buf(sb("r_m4", [128, 4], F32, p1))
                j0 = 0
                for blk in blocks:
                    xT = xTr.next()
                    off = 0
                    ems = []
                    for (sq, t0, n) in blk:
                        ems.append(lambda sq=sq, t0=t0, n=n, off=off: S.dma_start(
                            out=xT.ap[:, :, off:off + n], in_=sq["XN2T"][:, t0:t0 + n].rearrange("(k p) t -> p k t", p=128)))
                        off += n
                    nt = off // 128
                    K.dma(sp, ems, xT, writes=[xT])
                    pr = ps8.next()
                    for j in range(nt):
                        mm_group(pr.ap[:, j * NE:(j + 1) * NE], [(xT.ap[:, kt, j * 128:(j + 1) * 128], rw.ap[:, kt, :]) for kt in range(8)],
                                 reads=[xT, rw], pbuf=pr, pw=(j > 0))
                    if nt < 4:
                        K.do(dve, lambda: V.memset(sc.ap[:], 0.0), writes=[sc])
                    K.do(act, lambda: A.activation(out=sc.ap[:, 0:nt, :].rearrange("p j e -> p (j e)"), in_=pr.ap[:, 0:nt * NE], func=AF.Sigmoid),
                         reads=[pr], **(dict(writes=[sc]) if nt == 4 else dict(pwrites=[sc])))

                    def vv(fn, rd, wr, pw=()):
                        K.do(dve, fn, reads=rd, writes=wr, pwrites=pw)
                    flat = lambda t_: t_.ap[:].rearrange("p j e -> p (j e)")
                    g44 = lambda t_: t_.ap[:].rearrange("p j (g e) -> p (j g) e", e=4)
                    vv(lambda: V.tensor_tensor(out=flat(bb), in0=flat(sc), in1=flat(rbb), op=ALU.add), [sc, rbb], [bb])
                    vv(lambda: V.tensor_reduce(out=m16.ap[:], in_=g44(bb), axis=AX.X, op=ALU.max), [bb], [m16])
                    m16bc = bl(m16.ap, 0, 16, [[1, 16], [0, 4]])
                    vv(lambda: V.tensor_tensor(out=g44(q1), in0=g44(bb), in1=m16bc, op=ALU.is_equal), [bb, m16], [q1])
                    vv(lambda: V.scalar_tensor_tensor(out=flat(b2), in0=flat(q1), scalar=-BIG, in1=flat(bb), op0=ALU.mult, op1=ALU.add),
                       [q1, bb], [b2])
                    vv(lambda: V.tensor_reduce(out=m16b.ap[:], in_=g44(b2), axis=AX.X, op=ALU.max), [b2], [m16b])
                    vv(lambda: V.tensor_tensor(out=m16.ap[:], in0=m16.ap[:], in1=m16b.ap[:], op=ALU.add), [m16b], [m16])
                    vv(lambda: V.tensor_reduce(out=m4.ap[:], in_=m16.ap[:].rearrange("p (j g) -> p j g", g=4), axis=AX.X, op=ALU.max),
                       [m16], [m4])
                    m4bc = bl(m4.ap, 0, 4, [[1, 4], [0, 4]])
                    vv(lambda: V.tensor_tensor(out=m16b.ap[:].rearrange("p (j g) -> p j g", g=4),
                                               in0=m16.ap[:].rearrange("p (j g) -> p j g", g=4), in1=m4bc, op=ALU.is_equal),
                       [m16, m4], [m16b])
                    vv(lambda: V.tensor_scalar(m16b.ap[:], m16b.ap[:], -1.0, BIG, ALU.add, ALU.mult), [], [m16b])
                    penbc = bl(m16b.ap, 0, 16, [[1, 16], [0, 4]])
                    vv(lambda: V.tensor_tensor(out=g44(b2), in0=g44(bb), in1=penbc, op=ALU.add), [bb, m16b], [b2])
                    vv(lambda: V.tensor_reduce(out=m4.ap[:], in_=b2.ap[:], axis=AX.X, op=ALU.max), [b2], [m4])
                    m4e = bl(m4.ap, 0, 4, [[1, 4], [0, NE]])
                    vv(lambda: V.tensor_tensor(out=q1.ap[:], in0=b2.ap[:], in1=m4e, op=ALU.is_equal), [b2, m4], [q1])
                    vv(lambda: V.tensor_copy(Q1.ap[:, j0:j0 + nt, :], q1.ap[:, 0:nt, :]), [q1], [], [Q1])
                    vv(lambda: V.scalar_tensor_tensor(out=flat(b2), in0=flat(q1), scalar=-BIG, in1=flat(b2), op0=ALU.mult, op1=ALU.add),
                       [q1], [b2])
                    vv(lambda: V.tensor_reduce(out=m4.ap[:], in_=b2.ap[:], axis=AX.X, op=ALU.max), [b2], [m4])
                    vv(lambda: V.tensor_tensor(out=bb.ap[:], in0=b2.ap[:], in1=m4e, op=ALU.is_equal), [b2, m4], [bb])
                    vv(lambda: V.tensor_copy(Q2.ap[:, j0:j0 + nt, :], bb.ap[:, 0:nt, :]), [bb], [], [Q2])
                    vv(lambda: V.tensor_tensor(out=flat(q1), in0=flat(q1), in1=flat(bb), op=ALU.add), [bb], [q1])
                    vv(lambda: V.tensor_tensor(out=flat(q1), in0=flat(q1), in1=flat(sc), op=ALU.mult), [sc], [q1])
                    vv(lambda: V.tensor_reduce(out=m4.ap[:], in_=q1.ap[:], axis=AX.X, op=ALU.add), [q1], [m4])
                    vv(lambda: V.tensor_scalar(m4.ap[:], m4.ap[:], 1e-30, None, ALU.max), [], [m4])
                    vv(lambda: V.reciprocal(m4.ap[:], m4.ap[:]), [], [m4])
                    vv(lambda: V.tensor_tensor(out=GW.ap[:, j0:j0 + nt, :], in0=q1.ap[:, 0:nt, :],
                                               in1=bl(m4.ap, 0, 4, [[1, nt], [0, NE]]), op=ALU.mult), [q1, m4], [], [GW])
                    j0 += nt
                Mb = Buf(sb("Mb", [128, NJ, NE], BF16, p1))
                Macc = Buf(sb("Macc", [128, NJ + 1, NE], BF16, p1))
                Rsb = Buf(sb("Rsb", [128, NJ, NE], F32, p1))
                cnt = Buf(sb("cnt", [128, NE], F32, p1))
                tli = Buf(sb("tli", [128, NE], I32, p1))
                tl = Buf(sb("tl", [128, NE], F32, p1))
                offT = Buf(sb("offT", [128, NE], F32, p1))
                endT = Buf(sb("endT", [128, NE], F32, p1))
                cmp = Buf(sb("cmp", [128, TL, NE], F32, p1))
                eidr = Buf(sb("eidr", [128, TL], F32, p1))
                idf = Buf(sb("idf", [128, TL], F32, p1))
                slf = Buf(sb("slf", [128, 2, NJ], F32, p1))
                K.do(dve, lambda: V.tensor_tensor(out=Mb.ap[:], in0=Q1.ap[:], in1=Q2.ap[:], op=ALU.add), reads=[Q1, Q2], writes=[Mb])

                K.do(dve, lambda: V.memset(Macc.ap[:, 0, :], 0.0), writes=[Macc])
                for j in range(NJ):
                    K.do(dve, lambda: V.tensor_tensor(out=Macc.ap[:, j + 1, :], in0=Macc.ap[:, j, :], in1=Mb.ap[:, j, :], op=ALU.add),
                         reads=[Mb], writes=[Macc])
                for c0 in range(0, NJ, 32):
                    c1 = min(NJ, c0 + 32)
                    pb = ps8.next()

                    def emit_rank():
                        ins = None
                        for j in range(c0, c1):
                            o = pb.ap[:, (j - c0) * NE:(j - c0 + 1) * NE]
                            T.matmul(o, tri_bf[:], Mb.ap[:, j, :], start=True, stop=False)
                            ins = T.matmul(o, ones_bf[:], Macc.ap[:, j, :], start=False, stop=True)
                        return ins
                    K.do(pe, emit_rank, reads=[Mb, Macc, B_const], writes=[pb])
                    K.do(act, lambda: A.copy(Rsb.ap[:, c0:c1, :].rearrange("p j e -> p (j e)"), pb.ap[:, 0:(c1 - c0) * NE]),
                         reads=[pb], pwrites=[Rsb])
                pbc = ps8.next()
                K.do(pe, lambda: T.matmul(pbc.ap[:, 0:NE], ones_bf[:], Macc.ap[:, NJ, :], start=True, stop=True),
                     reads=[Macc, B_const], writes=[pbc])
                K.do(act, lambda: A.copy(cnt.ap[:], pbc.ap[:, 0:NE]), reads=[pbc], writes=[cnt])
                K.do(dve, lambda: V.tensor_scalar(tl.ap[:], cnt.ap[:], 1.0 / 512.0, 511.0 / 512.0 - 0.5 + 1.0 / 1024.0, ALU.mult, ALU.add),
                     reads=[cnt], writes=[tl])
                K.do(dve, lambda: V.tensor_copy(tli.ap[:], tl.ap[:]), reads=[tl], writes=[tli])
                K.do(dve, lambda: V.tensor_copy(tl.ap[:], tli.ap[:]), reads=[tli], writes=[tl])

                K.do(dve, lambda: V.memset(offT.ap[:, 0:1], 0.0), writes=[offT])
                for e in range(1, NE):
                    K.do(dve, lambda: V.tensor_tensor(out=offT.ap[:, e:e + 1], in0=offT.ap[:, e - 1:e], in1=tl.ap[:, e - 1:e], op=ALU.add),
                         reads=[tl], writes=[offT])
                K.do(dve, lambda: V.tensor_tensor(out=endT.ap[:], in0=offT.ap[:], in1=tl.ap[:], op=ALU.add), reads=[offT, tl], writes=[endT])
                K.do(dve, lambda: V.scalar_tensor_tensor(out=Rsb.ap[:], in0=bl(offT.ap, 0, NE, [[0, NJ], [1, NE]]), scalar=512.0,
                                                         in1=Rsb.ap[:], op0=ALU.mult, op1=ALU.add), reads=[offT], writes=[Rsb])
                for k, Q in enumerate((Q1, Q2)):
                    tmpq = Buf(sb(f"tmpq{k}", [128, NJ, NE], F32, p1))
                    K.do(dve, lambda: V.tensor_tensor(out=tmpq.ap[:], in0=Q.ap[:], in1=Rsb.ap[:], op=ALU.mult), reads=[Q, Rsb], writes=[tmpq])
                    K.do(dve, lambda: V.tensor_reduce(out=slf.ap[:, k, :], in_=tmpq.ap[:], axis=AX.X, op=ALU.add), reads=[tmpq], pwrites=[slf])
                    K.do(dve, lambda: V.tensor_tensor(out=tmpq.ap[:], in0=Q.ap[:], in1=GW.ap[:], op=ALU.mult), reads=[Q, GW], writes=[tmpq])
                    K.do(dve, lambda: V.tensor_reduce(out=wsel.ap[:, k, :], in_=tmpq.ap[:], axis=AX.X, op=ALU.add), reads=[tmpq], pwrites=[wsel])
                K.do(dve, lambda: V.tensor_scalar(slf.ap[:], slf.ap[:], float(TL * 512 - 1), None, ALU.min), writes=[slf])
                K.do(dve, lambda: V.tensor_copy(slot_i.ap[:], slf.ap[:]), reads=[slf], writes=[slot_i])
                K.do(dve, lambda: V.tensor_tensor(out=cmp.ap[:], in0=bl(endT.ap, 0, NE, [[0, TL], [1, NE]]),
                                                  in1=bl(iot, 0, TMAX, [[1, TL], [0, NE]]), op=ALU.is_le),
                     reads=[endT, B_const], writes=[cmp])
                K.do(dve, lambda: V.tensor_reduce(out=eidr.ap[:], in_=cmp.ap[:], axis=AX.X, op=ALU.add), reads=[cmp], writes=[eidr])
                K.do(dve, lambda: V.tensor_scalar(eidr.ap[:], eidr.ap[:], float(NE - 1), None, ALU.min), writes=[eidr])
                K.do(dve, lambda: V.scalar_tensor_tensor(out=idf.ap[:], in0=eidr.ap[:], scalar=128.0,
                                                         in1=bl(basek, 0, 8, [[0, TL]]), op0=ALU.mult, op1=ALU.add),
                     reads=[eidr, B_const], writes=[idf])
                K.do(dve, lambda: V.tensor_copy(idxW.ap[:], idf.ap[:]), reads=[idf], writes=[idxW])
                if "SLOT" in debug:
                    for nm, bf, shp in (("CNT", cnt, [128, NE]), ("TLD", tl, [128, NE]), ("OFFT", offT, [128, NE]), ("ENDT", endT, [128, NE]),
                                        ("RSB", Rsb, [128, NJ, NE]), ("Q1D", Q1, [128, NJ, NE]), ("Q2D", Q2, [128, NJ, NE])):
                        dd = nc.dram_tensor(nm, shp, F32, kind="ExternalOutput").ap()
                        K.dma(sp, [lambda dd=dd, bf=bf: S.dma_start(out=dd, in_=bf.ap[:])], bf, reads=[bf])
                    K.dma(sp, [lambda: S.dma_start(out=DBG["SLOT"], in_=slf.ap[:])], slf, reads=[slf])
                    K.dma(sp, [lambda: S.dma_start(out=DBG["EID"], in_=eidr.ap[:])], eidr, reads=[eidr])
                    K.dma(sp, [lambda: S.dma_start(out=DBG["WSEL"], in_=wsel.ap[:])], wsel, reads=[wsel])
            with scope() as p2:
                xrr = mkring("xrow", [128, D], BF16, 3, p2)
                for j, (sq, r0) in enumerate(tiles):
                    xt = xrr.next()
                    K.dma(sp, [lambda: S.dma_start(out=xt.ap[:], in_=sq["XN2R"][r0:r0 + 128, :])], xt, writes=[xt])
                    K.dma(pool, [lambda k=k: G.indirect_dma_start(out=XS, out_offset=bass.IndirectOffsetOnAxis(ap=slot_i.ap[:, k, j:j + 1], axis=0),
                                                                  in_=xt.ap[:], in_offset=None) for k in range(2)],
                          xt, reads=[xt, slot_i], pwrites=[B_XS])
            with scope() as p3:
                wgr = mkring("wg", [128, 8, DFF], BF16, 2, p3)
                wur = mkring("wu", [128, 8, DFF], BF16, 2, p3)
                wdr = mkring("wd", [128, 4, D], BF16, 2, p3)
                xrr = mkring("xs_rows", [128, 4, D], BF16, 2, p3)
                xTr = mkring("xsT", [128, 8, 512], BF16, 2, p3)
                her = mkring("heT", [128, 4, 512], BF16, 2, p3)
                sr = mkring("sl", [128, 512], BF16, 2, p3)
                ysr = mkring("ysb", [128, D], BF16, 3, p3)
                st = {}

                def load_tile(i):
                    wg, wu, wd, xr = wgr.next(), wur.next(), wdr.next(), xrr.next()
                    K.dma(sp, [lambda: S.dma_start(out=xr.ap[:], in_=XS[i * 512:(i + 1) * 512, :].rearrange("(a p) d -> p a d", p=128))],
                          xr, reads=[B_XS], writes=[xr])
                    for (wt_, src_) in ((wg, WG[l]), (wu, WU[l]), (wd, WD[l])):
                        K.dma(pool, [lambda wt_=wt_, src_=src_: G.indirect_dma_start(
                            out=wt_.ap[:].rearrange("p a b -> p (a b)"), out_offset=None, in_=src_,
                            in_offset=bass.IndirectOffsetOnAxis(ap=idxW.ap[:, i:i + 1], axis=0))],
                              wt_, reads=[idxW, B_expw[l]], writes=[wt_])
                    st[i] = dict(wg=wg, wu=wu, wd=wd, xr=xr)

                def transposes(i):
                    xr = st[i]["xr"]
                    xT = xTr.next()
                    st[i]["xT"] = xT
                    for js in range(4):
                        pb = ps8.next()
                        pv = pb.ap[:].bitcast(BF16)
                        K.do(pe, lambda: [T.transpose(pv[:, kt * 128:(kt + 1) * 128], xr.ap[:, js, kt::8], ident_bf[:])
                                          for kt in range(8)][-1], reads=[xr, B_const], writes=[pb])
                        K.do(act if js % 2 == 0 else dve,
                             (lambda: A.copy(xT.ap[:, :, js * 128:(js + 1) * 128], pv.rearrange("p (k t) -> p k t", k=8))) if js % 2 == 0 else
                             (lambda: V.tensor_copy(xT.ap[:, :, js * 128:(js + 1) * 128], pv.rearrange("p (k t) -> p k t", k=8))),
                             reads=[pb], **(dict(writes=[xT]) if js == 0 else dict(pwrites=[xT])))

                def gateup(i):
                    wg, wu, xT = st[i]["wg"], st[i]["wu"], st[i]["xT"]
                    he = her.next()
                    st[i]["he"] = he
                    for ft in range(4):
                        pgt, put = ps8.next(), ps8.next()
                        mm_group(pgt.ap[:, :], [(wg.ap[:, kt, ft::4], xT.ap[:, kt, :]) for kt in range(8)],
                                 reads=[wg, xT], pbuf=pgt)
                        mm_group(put.ap[:, :], [(wu.ap[:, kt, ft::4], xT.ap[:, kt, :]) for kt in range(8)],
                                 reads=[wu, xT], pbuf=put)
                        sl = sr.next()
                        K.do(act, lambda: A.activation(out=sl.ap[:], in_=pgt.ap[:, :], func=AF.Silu), reads=[pgt], writes=[sl])
                        K.do(dve, lambda: V.tensor_tensor(out=he.ap[:, ft, :], in0=sl.ap[:], in1=put.ap[:, :], op=ALU.mult),
                             reads=[sl, put], **(dict(writes=[he]) if ft == 0 else dict(pwrites=[he])))

                def down(i):
                    wd, he = st[i]["wd"], st[i]["he"]
                    for js in range(4):
                        ys = ysr.next()
                        for half in range(2):
                            po = ps8.next()
                            mm_group(po.ap[:, :], [(he.ap[:, kt, js * 128:(js + 1) * 128], wd.ap[:, kt, half * 512:(half + 1) * 512])
                                                   for kt in range(4)], reads=[he, wd], pbuf=po)
                            hs = slice(half * 512, (half + 1) * 512)
                            kw = dict(writes=[ys]) if half == 0 else dict(pwrites=[ys])
                            if half == 0:
                                K.do(act, lambda: A.copy(ys.ap[:, hs], po.ap[:, :]), reads=[po], **kw)
                            else:
                                K.do(dve, lambda: V.tensor_copy(ys.ap[:, hs], po.ap[:, :]), reads=[po], **kw)
                        r0 = i * 512 + js * 128
                        K.dma(sp, [lambda: S.dma_start(out=YS[r0:r0 + 128, :], in_=ys.ap[:])], ys, reads=[ys], pwrites=[B_YS])

                load_tile(0)
                transposes(0)
                if TL > 1:
                    load_tile(1)
                for i in range(TL):
                    gateup(i)
                    if i + 1 < TL:
                        transposes(i + 1)
                    down(i)
                    if i + 2 < TL:
                        load_tile(i + 2)
                    del st[i]
            with scope() as p4:
                G2 = {}
                for mr in sorted({sq["modrow"] for (sq, _) in tiles}):
                    G2[mr] = Buf(sb(f"g2b{mr}", [128, D], F32, p4))
                    K.dma(sp, [lambda mr=mr: S.dma_start(out=G2[mr].ap[:], in_=MOD[l, mr, 5 * D:6 * D].partition_broadcast(128))],
                          G2[mr], writes=[G2[mr]])
                G3b = None
                if last:
                    G3b = Buf(sb("g3b", [128, D], F32, p4))
                    K.dma(sp, [lambda: S.dma_start(out=G3b.ap[:], in_=final_norm_g.partition_broadcast(128))], G3b, writes=[G3b])
                y1r = mkring("y1", [128, D], BF16, 2, p4)
                y2r = mkring("y2", [128, D], BF16, 2, p4)
                hr = mkring("hin2", [128, D], F32, 2, p4)
                hnr = mkring("hn2", [128, D], F32, 2, p4)
                mr_ = mkring("mix", [128, D], F32, 2, p4)
                tmpr = mkring("tmp3", [128, D], F32, 2, p4)
                ssr = mkring("ss3", [128, 2], F32, 2, p4)
                for j, (sq, r0) in enumerate(tiles):
                    y1, y2, ht, hn, mx = y1r.next(), y2r.next(), hr.next(), hnr.next(), mr_.next()
                    K.dma(pool, [lambda: G.indirect_dma_start(out=y1.ap[:], out_offset=None, in_=YS,
                                                              in_offset=bass.IndirectOffsetOnAxis(ap=slot_i.ap[:, 0, j:j + 1], axis=0))],
                          y1, reads=[slot_i, B_YS], writes=[y1])
                    K.dma(pool, [lambda: G.indirect_dma_start(out=y2.ap[:], out_offset=None, in_=YS,
                                                              in_offset=bass.IndirectOffsetOnAxis(ap=slot_i.ap[:, 1, j:j + 1], axis=0))],
                          y2, reads=[slot_i, B_YS], writes=[y2])
                    K.dma(sp, [lambda: S.dma_start(out=ht.ap[:], in_=sq["HA"][r0:r0 + 128, :])], ht, writes=[ht])
                    K.do(act, lambda: A.activation(out=mx.ap[:], in_=y1.ap[:], func=AF.Copy, scale=wsel.ap[:, 0, j:j + 1]),
                         reads=[y1, wsel], writes=[mx])
                    K.do(dve, lambda: V.scalar_tensor_tensor(out=mx.ap[:], in0=y2.ap[:], scalar=wsel.ap[:, 1, j:j + 1], in1=mx.ap[:],
                                                             op0=ALU.mult, op1=ALU.add), reads=[y2, wsel], writes=[mx])
                    K.do(dve, lambda: V.tensor_tensor(out=mx.ap[:], in0=mx.ap[:], in1=G2[sq["modrow"]].ap[:], op=ALU.mult),
                         reads=[G2[sq["modrow"]]], writes=[mx])
                    K.do(dve, lambda: V.tensor_tensor(out=hn.ap[:], in0=mx.ap[:], in1=ht.ap[:], op=ALU.add), reads=[mx, ht], writes=[hn])
                    if not last:
                        K.dma(sp, [lambda: S.dma_start(out=sq["HB"][r0:r0 + 128, :], in_=hn.ap[:])], hn, reads=[hn])
                    else:
                        ss = ssr.next()
                        tmpb = tmpr.next()
                        rms_mod(hn, G3b, None, None, tmpb, ss)
                        K.dma(sp, [lambda: S.dma_start(out=out_d[sq["b"], r0:r0 + 128, :], in_=tmpb.ap[:])], tmpb, reads=[tmpb])

    def phase_moe(l, seq_list):
        last = (l == DEPTH - 1)
        blocks = []
        ctxs = [sq for sq in seq_list if sq["ctx"]]
        if ctxs and not last:
            blocks.append([(sq, 0, LC) for sq in ctxs])
        for sq in seq_list:
            if not sq["ctx"]:
                for tt in range(LS // 512):
                    blocks.append([(sq, tt * 512, 512)])
        with scope() as ph:
            rw = Buf(sb("rw", [128, 8, NE], BF16, ph))
            rbb = Buf(sb("rbb", [128, 4, NE], F32, ph))
            K.dma(pool, [lambda: G.dma_start(out=rw.ap[:], in_=router_w.rearrange("(kt p) e -> p kt e", p=128))], rw, writes=[rw])
            K.dma(sp, [lambda j=j: S.dma_start(out=rbb.ap[:, j, :], in_=router_b.partition_broadcast(128)) for j in range(4)],
                  rbb, writes=[rbb])
            g2c = Buf(sb("g2c", [128, 3, 8], F32, ph))
            for r in range(3):
                load_T(g2c, g2c.ap[:, r, :], [(0, 8, MOD[l, r, 5 * D:6 * D].rearrange("(k p) -> k p", p=128))], 8, 128,
                       partial=(r > 0))
            G3b = None
            if last:
                G3b = Buf(sb("g3b", [128, D], F32, ph))
                K.dma(sp, [lambda: S.dma_start(out=G3b.ap[:], in_=final_norm_g.partition_broadcast(128))], G3b, writes=[G3b])
            xTr = mkring("xT", [128, 8, 512], BF16, 2, ph)
            heT = sb("heT", [128, 64, 512], BF16, ph)
            B_he = Buf()
            WR = mkring("wstream", [128, 16384], BF16, 2, ph)
            moT = sb("moT", [128, 8, 512], F32, ph)
            B_mo = Buf()
            gbr = mkring("gb", [128, 512], BF16, 2, ph)
            sr = mkring("sl", [128, 512], BF16, 2, ph)
            t1r = mkring("t1", [128, 512], BF16, 2, ph)
            hr = mkring("hin2", [128, D], F32, 2, ph)
            hnr = mkring("hn2", [128, D], F32, 2, ph)
            tmpb = Buf(sb("tmp3", [128, D], F32, ph))
            ssr = mkring("ss3", [128, 2], F32, 2, ph)
            sc = Buf(sb("r_sc", [128, 4, NE], F32, ph))
            bb = Buf(sb("r_b", [128, 4, NE], F32, ph))
            b2 = Buf(sb("r_b2", [128, 4, NE], F32, ph))
            q1 = Buf(sb("r_q1", [128, 4, NE], F32, ph))
            m16 = Buf(sb("r_m16", [128, 16], F32, ph))
            m16b = Buf(sb("r_m16b", [128, 16], F32, ph))
            m4 = Buf(sb("r_m4", [128, 4], F32, ph))
            gts = Buf(sb("r_gts", [128, 4, NE], BF16, ph))
            gT = Buf(sb("r_gT", [16, 512], BF16, ph))

            def bl(ap_t, off, pstride, dims):
                return bass.AP(ap_t, off, [[pstride, 128]] + dims)

            for blk in blocks:
                xT = xTr.next()
                off = 0
                ems = []
                for (sq, t0, n) in blk:
                    ems.append(lambda sq=sq, t0=t0, n=n, off=off: S.dma_start(
                        out=xT.ap[:, :, off:off + n], in_=sq["XN2T"][:, t0:t0 + n].rearrange("(k p) t -> p k t", p=128)))
                    off += n
                K.dma(sp, ems, xT, writes=[xT])
                pr = ps8.next()
                for j in range(4):
                    mm_group(pr.ap[:, j * NE:(j + 1) * NE], [(xT.ap[:, kt, j * 128:(j + 1) * 128], rw.ap[:, kt, :]) for kt in range(8)],
                             reads=[xT, rw], pbuf=pr, pw=(j > 0))
                K.do(act, lambda: A.activation(out=sc.ap[:].rearrange("p j e -> p (j e)"), in_=pr.ap[:, 0:4 * NE], func=AF.Sigmoid),
                     reads=[pr], writes=[sc])

                def vv(fn, rd, wr):
                    K.do(dve, fn, reads=rd, writes=wr)
                flat = lambda t_: t_.ap[:].rearrange("p j e -> p (j e)")
                g44 = lambda t_: t_.ap[:].rearrange("p j (g e) -> p (j g) e", e=4)
                vv(lambda: V.tensor_tensor(out=flat(bb), in0=flat(sc), in1=flat(rbb), op=ALU.add), [sc, rbb], [bb])
                vv(lambda: V.tensor_reduce(out=m16.ap[:], in_=g44(bb), axis=AX.X, op=ALU.max), [bb], [m16])
                m16bc = bl(m16.ap, 0, 16, [[1, 16], [0, 4]])
                vv(lambda: V.tensor_tensor(out=g44(q1), in0=g44(bb), in1=m16bc, op=ALU.is_equal), [bb, m16], [q1])
                vv(lambda: V.scalar_tensor_tensor(out=flat(b2), in0=flat(q1), scalar=-BIG, in1=flat(bb), op0=ALU.mult, op1=ALU.add),
                   [q1, bb], [b2])
                vv(lambda: V.tensor_reduce(out=m16b.ap[:], in_=g44(b2), axis=AX.X, op=ALU.max), [b2], [m16b])
                vv(lambda: V.tensor_tensor(out=m16.ap[:], in0=m16.ap[:], in1=m16b.ap[:], op=ALU.add), [m16b], [m16])
                vv(lambda: V.tensor_reduce(out=m4.ap[:], in_=m16.ap[:].rearrange("p (j g) -> p j g", g=4), axis=AX.X, op=ALU.max),
                   [m16], [m4])
                m4bc = bl(m4.ap, 0, 4, [[1, 4], [0, 4]])
                vv(lambda: V.tensor_tensor(out=m16b.ap[:].rearrange("p (j g) -> p j g", g=4),
                                           in0=m16.ap[:].rearrange("p (j g) -> p j g", g=4), in1=m4bc, op=ALU.is_equal),
                   [m16, m4], [m16b])
                vv(lambda: V.tensor_scalar(m16b.ap[:], m16b.ap[:], -1.0, BIG, ALU.add, ALU.mult), [], [m16b])
                penbc = bl(m16b.ap, 0, 16, [[1, 16], [0, 4]])
                vv(lambda: V.tensor_tensor(out=g44(b2), in0=g44(bb), in1=penbc, op=ALU.add), [bb, m16b], [b2])
                vv(lambda: V.tensor_reduce(out=m4.ap[:], in_=b2.ap[:], axis=AX.X, op=ALU.max), [b2], [m4])
                m4e = bl(m4.ap, 0, 4, [[1, 4], [0, NE]])
                vv(lambda: V.tensor_tensor(out=q1.ap[:], in0=b2.ap[:], in1=m4e, op=ALU.is_equal), [b2, m4], [q1])
                vv(lambda: V.scalar_tensor_tensor(out=flat(b2), in0=flat(q1), scalar=-BIG, in1=flat(b2), op0=ALU.mult, op1=ALU.add),
                   [q1], [b2])
                vv(lambda: V.tensor_reduce(out=m4.ap[:], in_=b2.ap[:], axis=AX.X, op=ALU.max), [b2], [m4])
                vv(lambda: V.tensor_tensor(out=bb.ap[:], in0=b2.ap[:], in1=m4e, op=ALU.is_equal), [b2, m4], [bb])
                vv(lambda: V.tensor_tensor(out=flat(q1), in0=flat(q1), in1=flat(bb), op=ALU.add), [bb], [q1])
                vv(lambda: V.tensor_tensor(out=flat(q1), in0=flat(q1), in1=flat(sc), op=ALU.mult), [sc], [q1])
                vv(lambda: V.tensor_reduce(out=m4.ap[:], in_=q1.ap[:], axis=AX.X, op=ALU.add), [q1], [m4])
                vv(lambda: V.reciprocal(m4.ap[:], m4.ap[:]), [], [m4])
                vv(lambda: V.tensor_tensor(out=gts.ap[:], in0=q1.ap[:], in1=m4e, op=ALU.mult), [q1, m4], [gts])
                if "GATES" in debug:
                    pass
                pg = ps8.next()
                pgv = pg.ap[:].bitcast(BF16)
                K.do(pe, lambda: [T.transpose(pgv[0:NE, j * 128:(j + 1) * 128], gts.ap[:, j, :], ident_bf[:])
                                  for j in range(4)][-1], reads=[gts, B_const], writes=[pg])
                K.do(act, lambda: A.copy(gT.ap[:], pgv[0:NE, 0:512]), reads=[pg], writes=[gT])
                for e in range(NE):
                    ws = WR.next()
                    after = []
                    K.dma(sp, [lambda: S.dma_start(out=ws.ap[:, 0:4096].rearrange("p (k f) -> p k f", k=8),
                                                   in_=WG[l, e].rearrange("(k p) f -> p k f", p=128)),
                               lambda: S.dma_start(out=ws.ap[:, 4096:8192].rearrange("p (k f) -> p k f", k=8),
                                                   in_=WU[l, e].rearrange("(k p) f -> p k f", p=128))],
                          ws, reads=[B_expw[l]], writes=[ws])
                    wgv = ws.ap[:, 0:4096].rearrange("p (k f) -> p k f", k=8)
                    wuv = ws.ap[:, 4096:8192].rearrange("p (k f) -> p k f", k=8)
                    pbc = ps8.next()
                    mm_group(pbc.ap[:, :], [(onesel[:, e, :], gT.ap[:, :])], reads=[gT, B_const], pbuf=pbc)
                    gb = gbr.next()
                    K.do(act, lambda: A.copy(gb.ap[:], pbc.ap[:, :]), reads=[pbc], writes=[gb])
                    for ft in range(4):
                        pgt, put = ps8.next(), ps8.next()
                        mm_group(pgt.ap[:, :], [(wgv[:, kt, ft * 128:(ft + 1) * 128], xT.ap[:, kt, :]) for kt in range(8)],
                                 reads=[ws, xT], pbuf=pgt)
                        mm_group(put.ap[:, :], [(wuv[:, kt, ft * 128:(ft + 1) * 128], xT.ap[:, kt, :]) for kt in range(8)],
                                 reads=[ws, xT], pbuf=put)
                        sl = sr.next()
                        K.do(act, lambda: A.activation(out=sl.ap[:], in_=pgt.ap[:, :], func=AF.Silu), reads=[pgt], writes=[sl])
                        t1 = t1r.next()
                        K.do(dve, lambda: V.tensor_tensor(out=t1.ap[:], in0=sl.ap[:], in1=put.ap[:, :], op=ALU.mult),
                             reads=[sl, put], writes=[t1])
                        first = (e == 0 and ft == 0)
                        K.do(pool, lambda: G.tensor_tensor(out=heT[:, e * 4 + ft, :], in0=t1.ap[:], in1=gb.ap[:], op=ALU.mult),
                             reads=[t1, gb], **(dict(writes=[B_he]) if first else dict(pwrites=[B_he])))
                for dt2 in range(4):
                    ws = WR.next()
                    wdv = ws.ap[:].rearrange("p (i c) -> p i c", c=256)
                    K.dma(sp, [lambda: S.dma_start(out=wdv, in_=WD[l][:, dt2 * 256:(dt2 + 1) * 256].rearrange("(i p) c -> p i c", p=128))],
                          ws, reads=[B_expw[l]], writes=[ws])
                    for dtl in range(2):
                        dt_ = dt2 * 2 + dtl
                        po = ps8.next()
                        mm_group(po.ap[:, :], [(wdv[:, i, dtl * 128:(dtl + 1) * 128], heT[:, i, :]) for i in range(64)],
                                 reads=[ws, B_he], pbuf=po)
                        mr = blk[0][0]["modrow"]
                        if len(blk) == 1:
                            K.do(act, lambda: A.activation(out=moT[:, dt_, :], in_=po.ap[:, :], func=AF.Copy,
                                                           scale=g2c.ap[:, mr, dt_:dt_ + 1]),
                                 reads=[po, g2c], **(dict(writes=[B_mo]) if dt_ == 0 else dict(pwrites=[B_mo])))
                        else:
                            o2 = 0
                            for pi, (sq, t0, n) in enumerate(blk):
                                K.do(act, lambda: A.activation(out=moT[:, dt_, o2:o2 + n], in_=po.ap[:, o2:o2 + n], func=AF.Copy,
                                                               scale=g2c.ap[:, sq["modrow"], dt_:dt_ + 1]),
                                     reads=[po, g2c], **(dict(writes=[B_mo]) if (dt_ == 0 and pi == 0) else dict(pwrites=[B_mo])))
                                o2 += n
                off = 0
                for (sq, t0, n) in blk:
                    for jj in range(n // 128):
                        c0 = off + jj * 128
                        r0 = t0 + jj * 128
                        ht = hr.next()
                        K.dma(sp, [lambda: S.dma_start(out=ht.ap[:], in_=sq["HA"][r0:r0 + 128, :])], ht, writes=[ht])
                        hn = hnr.next()
                        for half in range(2):
                            pt = ps8.next()
                            K.do(pe, lambda: [T.transpose(pt.ap[:, k * 128:(k + 1) * 128], moT[:, half * 4 + k, c0:c0 + 128], ident_f[:])
                                              for k in range(4)][-1], reads=[B_mo, B_const], writes=[pt])
                            hs = slice(half * 512, (half + 1) * 512)
                            K.do(dve, lambda: V.tensor_tensor(out=hn.ap[:, hs], in0=pt.ap[:, :], in1=ht.ap[:, hs], op=ALU.add),
                                 reads=[pt, ht], **(dict(writes=[hn]) if half == 0 else dict(pwrites=[hn])))
                        if not last:
                            K.dma(pool, [lambda: G.dma_start(out=sq["HB"][r0:r0 + 128, :], in_=hn.ap[:])], hn, reads=[hn])
                        else:
                            ss = ssr.next()
                            rms_mod(hn, G3b, None, None, tmpb, ss)
                            K.dma(pool, [lambda: G.dma_start(out=out_d[sq["b"], r0:r0 + 128, :], in_=tmpb.ap[:])], tmpb, reads=[tmpb])
                    off += n

    DBG = {}
    debug_seq = 2
    if "XNT" in debug:
        DBG["XNT"] = nc.dram_tensor("XNT", [D, LS], BF16, kind="ExternalOutput").ap()
    if "SLOT" in debug:
        njd = sum((SEQ[s_]["L"] // 128) for s_ in seqs)
        DBG["SLOT"] = nc.dram_tensor("SLOT", [128, 2, njd], F32, kind="ExternalOutput").ap()
        DBG["WSEL"] = nc.dram_tensor("WSEL", [128, 2, njd], F32, kind="ExternalOutput").ap()
        DBG["EID"] = nc.dram_tensor("EID", [128, (2 * njd * 128 + NE * 511) // 512], F32, kind="ExternalOutput").ap()

    def stop(name):
        return stop_after == name

    def finish():
        K.barrier()
        es.close()
        return nc

    init_consts()
    seq_list = [SEQ[s] for s in seqs]
    for l in range(nlayers):
        convert_experts(l)
    for l in range(nlayers):
        phase_adaln(l)
    if stop("adaln"):
        return finish()
    for l in range(nlayers):
        phase_s5_tables(l)
    if stop("tables"):
        return finish()
    for l in range(nlayers):
        last = (l == DEPTH - 1)
        for sq in seq_list:
            phase_front(l, sq, only_cu=(last and sq["ctx"]))
        if stop(f"front{l}"):
            return finish()
        for sq in seq_list:
            phase_s5(l, sq)
        if stop(f"s5{l}"):
            return finish()
        for sq in seq_list:
            if not (last and sq["ctx"]):
                phase_back(l, sq)
        if stop(f"back{l}"):
            return finish()
        phase_moe_sparse(l, seq_list)
        if stop(f"moe{l}"):
            return finish()
    return finish()


_PROG = {}


def _inputs_for_core(inputs, c):
    m = {}
    b0 = 2 * c
    f = lambda a: np.ascontiguousarray(np.asarray(a, dtype=np.float32))
    m["x"] = f(inputs["x"][b0:b0 + 2])
    m["ctx"] = f(inputs["ctx"][b0:b0 + 2])
    m["cvec"] = f(np.stack([inputs["c"][b0], inputs["c"][b0 + 1], inputs["c_ctx"]], axis=0))
    for k in ("w_mod", "b_mod", "norm1_g", "norm2_g", "w_in", "sgu_norm_g", "sgu_w", "sgu_b", "conv_w",
              "s5_lam_re", "s5_lam_im", "s5_log_dt", "s5_b_re", "s5_b_im", "s5_c_re", "s5_c_im", "s5_d",
              "glu_w", "glu_b", "w_branch", "w_out", "router_w", "router_b", "exp_w_gate", "exp_w_up",
              "exp_w_down", "final_norm_g"):
        m[k] = f(inputs[k])
    return m


def kernel(**inputs):
    if "nc" not in _PROG:
        _PROG["nc"] = build_program()
    nc = _PROG["nc"]
    shared = _inputs_for_core(inputs, 0)
    in_maps = []
    for c in range(NCORES):
        m = dict(shared)
        b0 = 2 * c
        m["x"] = np.ascontiguousarray(np.asarray(inputs["x"][b0:b0 + 2], dtype=np.float32))
        m["ctx"] = np.ascontiguousarray(np.asarray(inputs["ctx"][b0:b0 + 2], dtype=np.float32))
        m["cvec"] = np.ascontiguousarray(np.stack([inputs["c"][b0], inputs["c"][b0 + 1], inputs["c_ctx"]], axis=0).astype(np.float32))
        in_maps.append(m)
    res = run_bass_kernel_spmd(nc, in_maps, core_ids=list(range(NCORES)))
    out = np.concatenate([np.asarray(r["out"]) for r in res.results], axis=0)
    return out.astype(np.float32)
```
